# Optimizing a Trainium2 kernel written in Bass

```python
import math
import jax, jax.numpy as jnp
from jax import lax
import numpy as np

D_MODEL = 2048
BATCH = 2
SEQ = 4096
DEPTH = 2

CHUNK = 64
QBLK = 128
HEAD_DIM = 128
N_HEADS = D_MODEL // HEAD_DIM
N_KV = N_HEADS // 4
GROUP = N_HEADS // N_KV
Q_DIM = N_HEADS * HEAD_DIM
KV_DIM = N_KV * HEAD_DIM
IDX_HEADS = 16
IDX_DIM = 128
IDX_ROPE = 64
IDXQ_DIM = IDX_HEADS * IDX_DIM
IN_A = Q_DIM + 2 * KV_DIM + IDXQ_DIM + IDX_DIM + IDX_HEADS
TOPK_MAX = 256
ROPE_THETA = 10000.0
CONV_W = 31
D_FF = 4 * D_MODEL
PLE_DIM = 256
MAX_OFFSET = 4096
N_A = (DEPTH + 1) // 2
N_B = DEPTH // 2
RMS_EPS = 1e-6
LN_EPS = 1e-5

kernel_name = "hybrid_dsa_conformer_conv_encoder"


def rmsnorm(x, g):
    xf = x.astype(jnp.float32)
    y = xf * lax.rsqrt(jnp.mean(xf * xf, axis=-1, keepdims=True) + RMS_EPS)
    return (y * g.astype(jnp.float32)).astype(x.dtype)


def layernorm(x, g, b):
    xf = x.astype(jnp.float32)
    mu = jnp.mean(xf, axis=-1, keepdims=True)
    xc = xf - mu
    y = xc * lax.rsqrt(jnp.mean(xc * xc, axis=-1, keepdims=True) + LN_EPS)
    return (y * g.astype(jnp.float32) + b.astype(jnp.float32)).astype(x.dtype)


def rope(x, positions, rot_dim):
    half = rot_dim // 2
    inv = ROPE_THETA ** (-jnp.arange(half, dtype=jnp.float32) * (2.0 / rot_dim))
    ang = positions.astype(jnp.float32)[..., None] * inv
    ang = ang.reshape(ang.shape[:2] + (1,) * (x.ndim - 3) + (half,))
    cos, sin = jnp.cos(ang), jnp.sin(ang)
    xr = x[..., :rot_dim].astype(jnp.float32)
    x1, x2 = xr[..., :half], xr[..., half:]
    rot = jnp.concatenate([x1 * cos - x2 * sin, x2 * cos + x1 * sin], axis=-1)
    return jnp.concatenate([rot.astype(x.dtype), x[..., rot_dim:]], axis=-1)


def dsa_mixer(hn, positions, w_in, w_out, kidx_g, kidx_b):
    B, S, _ = hn.shape
    k_top = min(TOPK_MAX, S // 4)
    proj = hn @ w_in
    o0 = Q_DIM
    o1 = o0 + KV_DIM
    o2 = o1 + KV_DIM
    o3 = o2 + IDXQ_DIM
    o4 = o3 + IDX_DIM
    q = proj[..., :o0].reshape(B, S, N_HEADS, HEAD_DIM)
    k = proj[..., o0:o1].reshape(B, S, N_KV, HEAD_DIM)
    v = proj[..., o1:o2].reshape(B, S, N_KV, HEAD_DIM)
    qi = proj[..., o2:o3].reshape(B, S, IDX_HEADS, IDX_DIM)
    ki = proj[..., o3:o4]
    wi = proj[..., o4:].astype(jnp.float32) * (IDX_HEADS ** -0.5 * IDX_DIM ** -0.5)

    q = rope(q, positions, HEAD_DIM)
    k = rope(k, positions, HEAD_DIM)
    ki = layernorm(ki, kidx_g, kidx_b)
    qi = rope(qi, positions, IDX_ROPE)
    ki = rope(ki, positions, IDX_ROPE)

    chunk_id = jnp.arange(S) // CHUNK
    neg = jnp.finfo(jnp.float32).min
    scale = HEAD_DIM ** -0.5

    def block(bi):
        t0 = bi * QBLK
        qb = lax.dynamic_slice_in_dim(q, t0, QBLK, axis=1)
        qib = lax.dynamic_slice_in_dim(qi, t0, QBLK, axis=1)
        wib = lax.dynamic_slice_in_dim(wi, t0, QBLK, axis=1)
        q_chunk = (t0 + jnp.arange(QBLK)) // CHUNK
        dots = jnp.einsum('bqhd,bsd->bqhs', qib, ki)
        score = jnp.einsum('bqhs,bqh->bqs', jax.nn.relu(dots).astype(jnp.float32), wib)
        admissible = chunk_id[None, :] <= q_chunk[:, None]
        score = jnp.where(admissible[None], score, neg)
        _, idx = lax.top_k(score, k_top)
        valid = (idx // CHUNK) <= q_chunk[None, :, None]
        ksel = jax.vmap(lambda kb, ib: kb[ib])(k, idx)
        vsel = jax.vmap(lambda vb, ib: vb[ib])(v, idx)
        qg = qb.reshape(B, QBLK, N_KV, GROUP, HEAD_DIM)
        logits = jnp.einsum('bqgrd,bqkgd->bqgrk', qg, ksel).astype(jnp.float32) * scale
        logits = jnp.where(valid[:, :, None, None, :], logits, neg)
        probs = jax.nn.softmax(logits, axis=-1).astype(vsel.dtype)
        o = jnp.einsum('bqgrk,bqkgd->bqgrd', probs, vsel)
        return o.reshape(B, QBLK, Q_DIM)

    out = lax.map(block, jnp.arange(S // QBLK))
    out = jnp.transpose(out, (1, 0, 2, 3)).reshape(B, S, Q_DIM)
    return out @ w_out


def conformer_conv(hn, w_pw1, b_pw1, w_dw, b_dw, ln_g, ln_b, w_pw2, b_pw2):
    D = hn.shape[-1]
    u = hn @ w_pw1 + b_pw1
    u = u[..., :D] * jax.nn.sigmoid(u[..., D:])
    u = lax.conv_general_dilated(
        u, w_dw[:, None, :], window_strides=(1,), padding=[(CONV_W - 1, 0)],
        dimension_numbers=('NWC', 'WIO', 'NWC'), feature_group_count=D) + b_dw
    u = jax.nn.silu(layernorm(u, ln_g, ln_b))
    return u @ w_pw2 + b_pw2


def sq_relu_mlp(hn, w1, w2):
    a = jax.nn.relu(hn @ w1)
    return (a * a) @ w2


def setup_inputs(seed: int = 0) -> dict:
    key = jax.random.key(seed)
    ks = jax.random.split(key, 24)
    f32 = jnp.float32
    nrm = lambda k, shape, s: jax.random.normal(k, shape, f32) * s
    x = jax.random.normal(ks[0], (BATCH, SEQ, D_MODEL), f32)
    p = jax.random.normal(ks[1], (DEPTH, BATCH, SEQ, PLE_DIM), f32)
    offset = jax.random.randint(ks[2], (BATCH, 1), 0, MAX_OFFSET)
    positions = (offset + jnp.arange(SEQ)[None, :]).astype(jnp.int32)
    return {
        "x": x,
        "p": p,
        "positions": positions,
        "norm_mix_g": 1.0 + nrm(ks[3], (DEPTH, D_MODEL), 0.02),
        "norm_mlp_g": 1.0 + nrm(ks[4], (DEPTH, D_MODEL), 0.02),
        "final_g": 1.0 + nrm(ks[5], (D_MODEL,), 0.02),
        "a_w_in": nrm(ks[6], (N_A, D_MODEL, IN_A), D_MODEL ** -0.5),
        "a_w_out": nrm(ks[7], (N_A, Q_DIM, D_MODEL), Q_DIM ** -0.5),
        "a_kidx_g": 1.0 + nrm(ks[8], (N_A, IDX_DIM), 0.02),
        "a_kidx_b": nrm(ks[9], (N_A, IDX_DIM), 0.02),
        "b_w_pw1": nrm(ks[10], (N_B, D_MODEL, 2 * D_MODEL), D_MODEL ** -0.5),
        "b_b_pw1": nrm(ks[11], (N_B, 2 * D_MODEL), 0.02),
        "b_w_dw": nrm(ks[12], (N_B, CONV_W, D_MODEL), CONV_W ** -0.5),
        "b_b_dw": nrm(ks[13], (N_B, D_MODEL), 0.02),
        "b_ln_g": 1.0 + nrm(ks[14], (N_B, D_MODEL), 0.02),
        "b_ln_b": nrm(ks[15], (N_B, D_MODEL), 0.02),
        "b_w_pw2": nrm(ks[16], (N_B, D_MODEL, D_MODEL), D_MODEL ** -0.5),
        "b_b_pw2": nrm(ks[17], (N_B, D_MODEL), 0.02),
        "mlp_w1": nrm(ks[18], (DEPTH, D_MODEL, D_FF), D_MODEL ** -0.5),
        "mlp_w2": nrm(ks[19], (DEPTH, D_FF, D_MODEL), D_FF ** -0.5),
        "ple_w_proj": nrm(ks[20], (DEPTH, PLE_DIM, D_MODEL), PLE_DIM ** -0.5),
        "ple_w_gate": nrm(ks[21], (DEPTH, D_MODEL, D_MODEL), D_MODEL ** -0.5),
    }


def reference(x, p, positions, norm_mix_g, norm_mlp_g, final_g,
              a_w_in, a_w_out, a_kidx_g, a_kidx_b,
              b_w_pw1, b_b_pw1, b_w_dw, b_b_dw, b_ln_g, b_ln_b, b_w_pw2, b_b_pw2,
              mlp_w1, mlp_w2, ple_w_proj, ple_w_gate):
    h = x
    for i in range(DEPTH):
        hn = rmsnorm(h, norm_mix_g[i])
        j = i // 2
        if i % 2 == 0:
            h = h + dsa_mixer(hn, positions, a_w_in[j], a_w_out[j], a_kidx_g[j], a_kidx_b[j])
        else:
            h = h + conformer_conv(hn, b_w_pw1[j], b_b_pw1[j], b_w_dw[j], b_b_dw[j],
                                   b_ln_g[j], b_ln_b[j], b_w_pw2[j], b_b_pw2[j])
        h = h + sq_relu_mlp(rmsnorm(h, norm_mlp_g[i]), mlp_w1[i], mlp_w2[i])
        h = h + jax.nn.sigmoid(h @ ple_w_gate[i]) * (p[i] @ ple_w_proj[i])
    return rmsnorm(h, final_g)
```

```python
import math
import numpy as np
from contextlib import ExitStack
import concourse.bass as bass
import concourse.mybir as mybir
from concourse.bass_utils import run_bass_kernel_spmd

F32 = mybir.dt.float32
BF16 = mybir.dt.bfloat16
I32 = mybir.dt.int32
AF = mybir.ActivationFunctionType
ALU = mybir.AluOpType
AX = mybir.AxisListType

PE, ACT, DVE, POOL, SP = range(5)
NENG = 5
DSIZE = {F32: 4, BF16: 2, I32: 4}

D = 2048
S = 4096
NT = 9
T0 = NT * 128
KC = 16
DFF = 8192
IN_A = 5264
SB_BYTES = 176 * 1024
TWO_PI = 2.0 * math.pi


class Op:
    __slots__ = ("gid", "eng", "fn", "deps", "is_dma", "signal", "sem", "val")


class Prog:
    PAGE = 64
    NDMA_SEMS = 32

    def __init__(self, nc, sb_bytes, ps_bytes=16384):
        self.nc = nc
        self.ops = []
        self.np_sb = sb_bytes // self.PAGE
        self.np_ps = ps_bytes // self.PAGE
        self.lw = {"sb": np.full(self.np_sb, -1, np.int64), "ps": np.full(self.np_ps, -1, np.int64)}
        self.rd = {
            "sb": np.full((NENG + 1, self.np_sb), -1, np.int64),
            "ps": np.full((NENG + 1, self.np_ps), -1, np.int64),
        }
        self.bases = {}
        self.bank_last = np.full((8, NENG), -1, np.int64)

    def rng(self, ap):
        name = ap.tensor.name
        if name not in self.bases:
            return None
        space, base = self.bases[name]
        pstep = ap.ap[0][0]
        ds = DSIZE[ap.dtype]
        off = ap.offset % pstep if pstep > 0 else ap.offset
        ext = 1
        for st, cnt in ap.ap[1:]:
            ext += abs(st) * (cnt - 1)
        lo = base + off * ds
        hi = lo + ext * ds
        return space, lo // self.PAGE, (hi + self.PAGE - 1) // self.PAGE

    def add(self, eng, fn, reads=(), writes=(), dma=False, extra=()):
        op = Op()
        op.gid = len(self.ops)
        op.eng = eng
        op.fn = fn
        op.is_dma = dma
        op.signal = dma
        op.sem = None
        op.val = 0
        deps = set()
        ridx = NENG if dma else eng
        rr_ = [self.rng(a) for a in reads]
        ww_ = [self.rng(a) for a in writes]
        for r in rr_:
            if r is None:
                continue
            sp, a, b = r
            w = self.lw[sp][a:b]
            deps.update(np.unique(w[w >= 0]).tolist())
        for r in ww_:
            if r is None:
                continue
            sp, a, b = r
            w = self.lw[sp][a:b]
            deps.update(np.unique(w[w >= 0]).tolist())
            rr = self.rd[sp][:, a:b]
            for e in range(NENG + 1):
                if e == eng and not dma and e != NENG:
                    continue
                x = rr[e]
                deps.update(np.unique(x[x >= 0]).tolist())
        for r in rr_:
            if r is None:
                continue
            sp, a, b = r
            self.rd[sp][ridx, a:b] = op.gid
        for r in ww_:
            if r is None:
                continue
            sp, a, b = r
            self.lw[sp][a:b] = op.gid
            self.rd[sp][:, a:b] = -1
        if not dma:
            bpp = 2048 // self.PAGE
            for r in rr_ + ww_:
                if r is None or r[0] != "ps":
                    continue
                for bk in range(r[1] // bpp, (r[2] - 1) // bpp + 1):
                    for e in range(NENG):
                        if e != eng and self.bank_last[bk, e] >= 0:
                            deps.add(int(self.bank_last[bk, e]))
                    self.bank_last[bk, eng] = op.gid
        deps.update(extra)
        deps.discard(op.gid)
        best = {}
        out = set()
        for d in deps:
            o = self.ops[d]
            if o.is_dma:
                out.add(d)
            else:
                if o.eng == PE and eng == PE and not dma:
                    continue
                if o.eng not in best or best[o.eng] < d:
                    best[o.eng] = d
        out.update(best.values())
        op.deps = out
        self.ops.append(op)
        return op.gid

    def wait_for(self, eng, gids):
        op = Op()
        op.gid = len(self.ops)
        op.eng = eng
        op.fn = None
        op.is_dma = False
        op.signal = False
        op.sem = None
        op.val = 0
        op.deps = set(gids)
        self.ops.append(op)
        return op.gid

    def emit(self):
        nc = self.nc
        with ExitStack() as es:
            esems = [es.enter_context(nc.semaphore(f"e{i}")) for i in range(NENG)]
            dsems = [es.enter_context(nc.semaphore(f"d{i}")) for i in range(self.NDMA_SEMS)]
            dcount = [0] * self.NDMA_SEMS
            dlast = [None] * self.NDMA_SEMS
            k = 0
            for op in self.ops:
                if op.is_dma:
                    s = k % self.NDMA_SEMS
                    k += 1
                    if dlast[s] is not None:
                        op.deps.add(dlast[s])
                    dcount[s] += 1
                    op.sem = dsems[s]
                    op.val = 16 * dcount[s]
                    dlast[s] = op.gid
            for op in self.ops:
                for d in op.deps:
                    self.ops[d].signal = True
            ecount = [0] * NENG
            for op in self.ops:
                if op.is_dma or op.fn is None:
                    continue
                if op.signal:
                    ecount[op.eng] += 1
                    op.sem = esems[op.eng]
                    op.val = ecount[op.eng]
            per = [[] for _ in range(NENG)]
            for op in self.ops:
                per[op.eng].append(op)
            ops = self.ops
            self.nwaits = 0

            def run(eng_idx, e):
                waited = {}
                for op in per[eng_idx]:
                    for d in sorted(op.deps):
                        o = ops[d]
                        key = id(o.sem)
                        if waited.get(key, 0) < o.val:
                            e.wait_ge(o.sem, o.val)
                            waited[key] = o.val
                            self.nwaits += 1
                    if op.fn is None:
                        continue
                    ins = op.fn(e)
                    if op.signal:
                        ins.then_inc(op.sem, 16 if op.is_dma else 1)

            with nc.Block() as block:
                @block.tensor
                def _(e):
                    run(PE, e)

                @block.scalar
                def _(e):
                    run(ACT, e)

                @block.vector
                def _(e):
                    run(DVE, e)

                @block.gpsimd
                def _(e):
                    run(POOL, e)

                @block.sync
                def _(e):
                    run(SP, e)


class Arena:
    def __init__(self, ar):
        self.ar = ar
        self.top = 0

    def mark(self):
        return self.top

    def release(self, m):
        self.top = m

    def alloc(self, shape, dt):
        n = 1
        for s in shape:
            n *= s
        nb = (n * DSIZE[dt] + 63) // 64 * 64
        assert self.top + nb <= SB_BYTES, f"SBUF arena overflow {self.top + nb}"
        v = self.ar[:, self.top // 4:(self.top + nb) // 4]
        self.top += nb
        if dt != F32:
            v = v.bitcast(dt)
        v = v[:, :n]
        if len(shape) == 2:
            v = v.rearrange("p (a b) -> p a b", a=shape[0])
        elif len(shape) == 3:
            v = v.rearrange("p (a b c) -> p a b c", a=shape[0], b=shape[1])
        return v


class _Stop(Exception):
    pass


def build_program(dbg=None, stop=None):
    try:
        return _build_program(dbg, stop)
    except _Stop as e:
        return e.args[0]


def _build_program(dbg=None, stop=None):
    nc = bass.Bass("TRN2", target_bir_lowering=False)
    dram = {}

    def din(name, shape, dt=F32):
        dram[name] = nc.dram_tensor(name, list(shape), dt, kind="ExternalInput").ap()
        return dram[name]

    xs = din("xs", [S, D])
    xo = din("xo", [T0, D])
    pos_s = din("pos_s", [1, S], I32)
    pos_o = din("pos_o", [1, T0], I32)
    p0 = din("p0", [T0, 256])
    p1 = din("p1", [1024, 256])
    qinfo = din("qinfo", [128, NT])
    hflag = din("hflag", [128, 1])
    cst = din("cst", [128, 16])
    ident_d = din("ident", [128, 128])
    psw128_d = din("psw128", [128, 128])
    psw64_d = din("psw64", [128, 128])
    chunkid_d = din("chunkid", [128, 64])
    g_mix = din("norm_mix_g", [2, D])
    g_mlp = din("norm_mlp_g", [2, D])
    g_fin = din("final_g", [1, D])
    a_w_in = din("a_w_in", [D, IN_A])
    a_w_out = din("a_w_out", [D, D])
    kidx_g = din("a_kidx_g", [128, 1])
    kidx_b = din("a_kidx_b", [128, 1])
    w_pw1 = din("b_w_pw1", [D, 2 * D])
    b_pw1 = din("b_b_pw1", [1, 2 * D])
    w_dw = din("b_w_dw", [31, D])
    b_dw = din("b_b_dw", [1, D])
    ln_g = din("b_ln_g", [1, D])
    ln_b = din("b_ln_b", [1, D])
    w_pw2 = din("b_w_pw2", [D, D])
    b_pw2 = din("b_b_pw2", [1, D])
    mlp_w1 = din("mlp_w1", [2, D, DFF])
    mlp_w2 = din("mlp_w2", [2, DFF, D])
    ple_wp = din("ple_w_proj", [2, 256, D])
    ple_wg = din("ple_w_gate", [2, D, D])
    y = nc.dram_tensor("y", [1024, D], F32, kind="ExternalOutput").ap()
    dbg_out = None
    if dbg:
        dbg_out = nc.dram_tensor("dbg", [T0, D], F32, kind="ExternalOutput").ap()

    with ExitStack() as es:
        ar = es.enter_context(nc.sbuf_tensor("arena", [128, SB_BYTES // 4], F32))
        ps = es.enter_context(nc.psum_tensor("ps", [128, 4096], F32))
        P = Prog(nc, SB_BYTES)
        P.bases["arena"] = ("sb", 0)
        P.bases["ps"] = ("ps", 0)
        A = Arena(ar)
        es.enter_context(nc.allow_non_contiguous_dma(reason="small per-partition constant loads"))

        def bank(i, n=1):
            return ps[:, i * 512:(i + n) * 512]

        def bankb(i, n=1):
            return ps[:, i * 512:(i + n) * 512].bitcast(BF16)

        def mm(out, lhsT, rhs, start, stop):
            P.add(PE, lambda e: e.matmul(out, lhsT=lhsT, rhs=rhs, start=start, stop=stop),
                  reads=[lhsT, rhs], writes=[out])

        def tr(out, in_, ident):
            P.add(PE, lambda e: e.transpose(out, in_, ident), reads=[in_, ident], writes=[out])

        def act(out, in_, func, bias=None, scale=None, accum=None, eng=ACT):
            kw = {}
            rd = [in_]
            wr = [out]
            if bias is not None:
                kw["bias"] = bias
                if not isinstance(bias, float):
                    rd.append(bias)
            if scale is not None:
                kw["scale"] = scale
                if not isinstance(scale, float):
                    rd.append(scale)
            if accum is not None:
                kw["accum_out"] = accum
                wr.append(accum)
            P.add(ACT, lambda e: e.activation(out=out, in_=in_, func=func, **kw), reads=rd, writes=wr)

        def ts(eng, out, in0, s1, s2, op0, op1=None, accum=None):
            rd = [in0]
            wr = [out]
            if not isinstance(s1, float):
                rd.append(s1)
            if s2 is not None and not isinstance(s2, float):
                rd.append(s2)
            kw = {}
            if op1 is not None:
                kw["op1"] = op1
            if accum is not None:
                kw["accum_out"] = accum
                wr.append(accum)
            P.add(eng, lambda e: e.tensor_scalar(out=out, in0=in0, scalar1=s1, scalar2=s2, op0=op0, **kw),
                  reads=rd, writes=wr)

        def tt(eng, out, in0, in1, op):
            P.add(eng, lambda e: e.tensor_tensor(out=out, in0=in0, in1=in1, op=op), reads=[in0, in1], writes=[out])

        def stt(eng, out, in0, sc, in1, op0, op1):
            rd = [in0, in1]
            if not isinstance(sc, float):
                rd.append(sc)
            P.add(eng, lambda e: e.scalar_tensor_tensor(out=out, in0=in0, scalar=sc, in1=in1, op0=op0, op1=op1),
                  reads=rd, writes=[out])

        def cp(eng, out, in_):
            if eng == ACT:
                act(out, in_, AF.Copy)
            else:
                P.add(eng, lambda e: e.tensor_copy(out=out, in_=in_), reads=[in_], writes=[out])

        def recip(out, in_):
            P.add(DVE, lambda e: e.reciprocal(out=out, in_=in_), reads=[in_], writes=[out])

        def memset(eng, out, v):
            P.add(eng, lambda e: e.memset(out, v), writes=[out])

        def dma(eng, out, in_, extra=()):
            return P.add(eng, lambda e: e.dma_start(out=out, in_=in_), reads=[in_], writes=[out], dma=True, extra=extra)

        def early(tag, views):
            if stop != tag:
                return
            gl = []
            for i, v in enumerate(views):
                ncol = v.shape[-1]
                gl.append(dma(SP, y[i * 128:(i + 1) * 128, 0:ncol], v))
            P.wait_for(SP, gl)
            P.emit()
            build_program.stats = (len(P.ops), P.nwaits)
            raise _Stop(nc)

        ident = A.alloc([128], BF16)
        psw128 = A.alloc([128], BF16)
        psw64 = A.alloc([128], BF16)
        ones_bf = A.alloc([128], BF16)
        ones_f = A.alloc([128], F32)
        ones_d = A.alloc([128], F32)
        cst_sb = A.alloc([16], F32)
        qinfo_sb = A.alloc([NT], F32)
        hflag_sb = A.alloc([1], F32)
        chunkid = A.alloc([64], F32)
        gfm = A.alloc([4, KC], F32)
        kig = A.alloc([2], F32)
        stats = A.alloc([64], F32)
        dma(POOL, ident, ident_d)
        dma(POOL, psw128, psw128_d)
        dma(POOL, psw64, psw64_d)
        dma(SP, cst_sb, cst)
        dma(SP, qinfo_sb, qinfo)
        dma(SP, hflag_sb, hflag)
        dma(SP, chunkid, chunkid_d)
        dma(SP, kig[:, 0:1], kidx_g)
        dma(SP, kig[:, 1:2], kidx_b)
        for i, gsrc in enumerate([g_mix[0:1, :], g_mlp[0:1, :], g_mix[1:2, :], g_mlp[1:2, :]]):
            dma(SP, gfm[:, i, :], gsrc.rearrange("o (kc p) -> p (o kc)", p=128))
        memset(DVE, ones_bf, 1.0)
        memset(DVE, ones_f, 1.0 / 128.0)
        memset(DVE, ones_d, 1.0 / D)

        early('consts', [cst_sb, chunkid, gfm.rearrange('p a b -> p (a b)'), ident.bitcast(F32)])
        NPB = 4
        panels = [A.alloc([8, 256], BF16) for _ in range(NPB)]
        pstate = {"i": 0}

        def load_panel(w2d, r0, nk, c0, ncols=256):
            buf = panels[pstate["i"] % NPB]
            pstate["i"] += 1
            src = w2d[r0:r0 + nk * 128, c0:c0 + ncols].rearrange("(kc p) n -> p kc n", p=128)
            dma(POOL, buf[:, 0:nk, 0:ncols], src)
            return buf

        hn_tm = [A.alloc([D], BF16)] * 2
        nstate = {"i": 0}

        def norm_T(src, dstT, col0, gidx, st_col):
            i = nstate["i"]
            nstate["i"] += 1
            hb = hn_tm[i % 2]
            if gidx is not None:
                ss = stats[:, st_col:st_col + 1]
                memset(DVE, ss, 0.0)
                act(hb, src, AF.Square, accum=ss)
                ts(DVE, stats[:, st_col + 1:st_col + 2], ss, 1.0 / D, 1e-6, ALU.mult, ALU.add)
                recip(stats[:, st_col + 2:st_col + 3], stats[:, st_col + 1:st_col + 2])
                act(stats[:, st_col + 3:st_col + 4], stats[:, st_col + 2:st_col + 3], AF.Sqrt)
                ts(DVE, hb, src, stats[:, st_col + 3:st_col + 4], None, ALU.mult)
            else:
                cp(ACT, hb, src)
            pb = bankb(4 + 2 * (i % 2), 2)
            for kc in range(KC):
                tr(pb[:, kc * 128:(kc + 1) * 128], hb[:, kc * 128:(kc + 1) * 128], ident)
            if gidx is not None:
                for kc in range(KC):
                    o = dstT[:, kc, col0:col0 + 128]
                    i_ = pb[:, kc * 128:(kc + 1) * 128]
                    if kc < 8:
                        act(o, i_, AF.Copy, scale=gfm[:, gidx, kc:kc + 1])
                    else:
                        ts(DVE, o, i_, gfm[:, gidx, kc:kc + 1], None, ALU.mult)
            else:
                o = dstT[:, :, col0:col0 + 128]
                i_ = pb.rearrange("p (kc c) -> p kc c", kc=KC)
                cp(DVE if i % 2 else ACT, o, i_)

        def rope_tables(posb_i, n, tabs, tf):
            posf = tabs[:, 4, :]
            cp(DVE, posf, posb_i)
            x = tf[:, 0, 0:n]
            kf = tf[:, 1, 0:n]
            for (ti, inv_c, ph_c) in ((0, 0, 2), (1, 0, 3), (2, 1, 4), (3, 1, 5)):
                ts(DVE, x, posf, cst_sb[:, inv_c:inv_c + 1], cst_sb[:, ph_c:ph_c + 1], ALU.mult, ALU.add)
                ts(DVE, posb_i, x, 1.0 / TWO_PI, None, ALU.mult)
                cp(DVE, kf, posb_i)
                stt(DVE, x, kf, -TWO_PI, x, ALU.mult, ALU.add)
                ts(DVE, kf, x, math.pi, -TWO_PI, ALU.is_gt, ALU.mult)
                tt(DVE, x, x, kf, ALU.add)
                ts(DVE, x, x, math.pi, -math.pi, ALU.min, ALU.max)
                act(tabs[:, ti, :], x, AF.Sin)

        def rope_apply(dst, src_ps, n, Ct, St, psw, tmpb, tmpf, pbank):
            cp(ACT, tmpb[:, 0:n], src_ps)
            mm(pbank[:, 0:n], psw, tmpb[:, 0:n], True, True)
            tt(POOL, tmpf[:, 0, 0:n], tmpb[:, 0:n], Ct, ALU.mult)
            tt(DVE, tmpf[:, 1, 0:n], pbank[:, 0:n], St, ALU.mult)
            tt(POOL, dst, tmpf[:, 0, 0:n], tmpf[:, 1, 0:n], ALU.add)

        qscr = nc.dram_tensor("qscr", [NT, 128, 2048], BF16).ap()
        qiscr = nc.dram_tensor("qiscr", [NT, 128, 2048], BF16).ap()
        wiscr = nc.dram_tensor("wiscr", [NT, 128, 16], F32).ap()
        m_kv = A.mark()
        KT = A.alloc([4, S], BF16)
        V = A.alloc([32, 512], BF16)
        kiT = A.alloc([S], BF16)
        m_attn = A.mark()
        assert m_attn - m_kv == NT * D * 4
        wkv = A.alloc([KC, 1152], BF16)
        for kb in range(2):
            dma(POOL, wkv[:, kb * 8:(kb + 1) * 8, 0:1024],
                a_w_in[kb * 1024:(kb + 1) * 1024, 2048:3072].rearrange("(kc p) n -> p kc n", p=128))
            dma(POOL, wkv[:, kb * 8:(kb + 1) * 8, 1024:1152],
                a_w_in[kb * 1024:(kb + 1) * 1024, 5120:5248].rearrange("(kc p) n -> p kc n", p=128))
        early('a1_w', [wkv[:, 0, :].bitcast(F32), wkv[:, 15, :].bitcast(F32)])
        G1 = 256
        xt = [A.alloc([D], F32)] * 2
        hnT = A.alloc([KC, G1], BF16)
        tabs = A.alloc([5, G1], F32)
        posb = A.alloc([G1], I32)
        tmpb = A.alloc([G1], BF16)
        tmpf = A.alloc([2, G1], F32)
        lnb = A.alloc([3, G1], F32)
        for sg in range(S // G1):
            s0 = sg * G1
            dma(SP, posb, pos_s[0, s0:s0 + G1].partition_broadcast(128))
            rope_tables(posb, G1, tabs, tmpf)
            if sg == 0:
                early('a1_tab', [tabs.rearrange('p a b -> p (a b)')])
            for ti in range(2):
                xb = xt[ti % 2]
                dma(SP, xb, xs[s0 + ti * 128:s0 + (ti + 1) * 128, :])
                norm_T(xb, hnT, ti * 128, 0, 16 * (ti % 2))
            if sg == 0:
                early('a1_n', [hnT[:, 0:8, :].rearrange('p a b -> p (a b)').bitcast(F32), hnT[:, 8:16, :].rearrange('p a b -> p (a b)').bitcast(F32)])
            for g in range(4):
                pk = bank(g % 2)[:, 0:G1]
                for kc in range(KC):
                    mm(pk, wkv[:, kc, g * 128:(g + 1) * 128], hnT[:, kc, :], kc == 0, kc == KC - 1)
                rope_apply(KT[:, g, s0:s0 + G1], pk, G1, tabs[:, 0, :], tabs[:, 1, :], psw128, tmpb, tmpf, bank(2))
            for ti in range(2):
                pv = bank(ti % 2)
                for kc in range(KC):
                    mm(pv, hnT[:, kc, ti * 128:(ti + 1) * 128], wkv[:, kc, 512:1024], kc == 0, kc == KC - 1)
                cp(ACT if ti % 2 else DVE, V[:, sg * 2 + ti, :], pv)
            pk = bank(3)[:, 0:G1]
            pm_ = bank(2)[:, 0:G1]
            for kc in range(KC):
                mm(pk, wkv[:, kc, 1024:1152], hnT[:, kc, :], kc == 0, kc == KC - 1)
            cp(ACT, lnb[:, 0, :], pk)
            mm(pm_, ones_f, lnb[:, 0, :], True, True)
            tt(DVE, lnb[:, 1, :], lnb[:, 0, :], pm_, ALU.subtract)
            act(lnb[:, 2, :], lnb[:, 1, :], AF.Square)
            mm(pm_, ones_f, lnb[:, 2, :], True, True)
            ts(DVE, lnb[:, 0, :], pm_, 1e-5, None, ALU.add)
            recip(lnb[:, 2, :], lnb[:, 0, :])
            act(lnb[:, 0, :], lnb[:, 2, :], AF.Sqrt)
            tt(DVE, lnb[:, 1, :], lnb[:, 1, :], lnb[:, 0, :], ALU.mult)
            ts(DVE, lnb[:, 2, :], lnb[:, 1, :], kig[:, 0:1], kig[:, 1:2], ALU.mult, ALU.add)
            rope_apply(kiT[:, s0:s0 + G1], lnb[:, 2, :], G1, tabs[:, 2, :], tabs[:, 3, :], psw64, tmpb, tmpf, bank(2))
            if sg == 0:
                early('a1_g0', [KT[:, 0, 0:512].bitcast(F32), KT[:, 3, 0:512].bitcast(F32), kiT[:, 0:512].bitcast(F32), V[:, 0, :].bitcast(F32), V[:, 1, :].bitcast(F32)])
        early('a1', [KT[:, 0, :].bitcast(F32), KT[:, 3, :].bitcast(F32), kiT.bitcast(F32), V[:, 0:8, :].rearrange('p a b -> p (a b)').bitcast(F32), V[:, 24:32, :].rearrange('p a b -> p (a b)').bitcast(F32)])
        A.release(m_attn)

        attnT = A.alloc([KC, T0], BF16)
        m_ws = A.mark()
        hnTg = A.alloc([KC, 384], BF16)
        tabs_o = A.alloc([5, 384], F32)
        posb_o = A.alloc([384], I32)
        tmpb_o = A.alloc([384], BF16)
        tmpf_o = A.alloc([2, 384], F32)
        stage = [A.alloc([384], BF16) for _ in range(2)]
        wwi = A.alloc([KC, 16], BF16)
        wist = A.alloc([3, 16], F32)
        xt2 = [A.alloc([D], F32)] * 2
        for kb in range(2):
            dma(POOL, wwi[:, kb * 8:(kb + 1) * 8, :],
                a_w_in[kb * 1024:(kb + 1) * 1024, 5248:5264].rearrange("(kc p) n -> p kc n", p=128))
        WSCALE = (16 ** -0.5) * (128 ** -0.5)
        ASCALE = 128 ** -0.5
        NBIS = 28
        RANGE = 64.0
        wr_gids = [[] for _ in range(NT)]
        sti = 0
        for grp in range(3):
            t0g = grp * 384
            dma(SP, posb_o, pos_o[0, t0g:t0g + 384].partition_broadcast(128))
            rope_tables(posb_o, 384, tabs_o, tmpf_o)
            for ti in range(3):
                xb = xt2[ti % 2]
                dma(SP, xb, xo[t0g + ti * 128:t0g + (ti + 1) * 128, :])
                norm_T(xb, hnTg, ti * 128, 0, 16 * (ti % 2))
            for (scr, c_base, Ct, St, psw) in ((qscr, 0, 0, 1, psw128), (qiscr, 3072, 2, 3, psw64)):
                for hp in range(8):
                    pA = load_panel(a_w_in, 0, 8, c_base + hp * 256)
                    pB = load_panel(a_w_in, 1024, 8, c_base + hp * 256)
                    for n in range(2):
                        pk = bank(2 * (hp % 2) + n)
                        for kc in range(KC):
                            pan = pA if kc < 8 else pB
                            mm(pk[:, 0:384], pan[:, kc % 8, n * 128:(n + 1) * 128], hnTg[:, kc, :], kc == 0, kc == KC - 1)
                    for n in range(2):
                        hd = hp * 2 + n
                        st_ = stage[sti % 2]
                        sti += 1
                        rope_apply(st_, bank(2 * (hp % 2) + n)[:, 0:384], 384, tabs_o[:, Ct, :], tabs_o[:, St, :],
                                   psw, tmpb_o, tmpf_o, bank(6 + n % 2))
                        gd = dma(SP, scr[grp * 3:(grp + 1) * 3, :, hd * 128:(hd + 1) * 128].rearrange("t p q -> p t q"),
                                 st_.rearrange("p (t q) -> p t q", t=3))
                        for k3 in range(3):
                            wr_gids[grp * 3 + k3].append(gd)
            for ti in range(3):
                pw = bank(ti % 2)
                for kc in range(KC):
                    mm(pw[:, 0:16], hnTg[:, kc, ti * 128:(ti + 1) * 128], wwi[:, kc, :], kc == 0, kc == KC - 1)
                ts(DVE, wist[:, ti, :], pw[:, 0:16], WSCALE, None, ALU.mult)
                gd = dma(SP, wiscr[grp * 3 + ti], wist[:, ti, :])
                wr_gids[grp * 3 + ti].append(gd)
        early('a2', [tabs_o.rearrange('p a b -> p (a b)'), hnTg[:, 0:4, :].rearrange('p a b -> p (a b)').bitcast(F32)])
        A.release(m_ws)

        Qb = A.alloc([16, 128], BF16)
        qib = A.alloc([16, 128], BF16)
        wi_sb = A.alloc([16], F32)
        maskT = A.alloc([32, 128], BF16)
        score = A.alloc([S], F32)
        rbreg = A.alloc([2048], F32)
        rbuf = [rbreg[:, 0:1024], rbreg[:, 1024:2048]]
        maskb = rbreg.bitcast(BF16)
        PT = [score[:, 0:512].bitcast(BF16), score[:, 512:1024].bitcast(BF16)]
        rcp = score[:, 1024:1536]
        for tile_i in range(NT):
            dma(SP, Qb.rearrange("p h q -> p (h q)"), qscr[tile_i], extra=wr_gids[tile_i])
            dma(SP, qib.rearrange("p h q -> p (h q)"), qiscr[tile_i], extra=wr_gids[tile_i])
            dma(SP, wi_sb, wiscr[tile_i], extra=wr_gids[tile_i])
            sc3 = score.rearrange("p (c k) -> p c k", k=64)
            ts(DVE, sc3, chunkid.unsqueeze(2).to_broadcast([128, 64, 64]), qinfo_sb[:, tile_i:tile_i + 1],
               -1e30, ALU.is_gt, ALU.mult)
            it = 0
            for hh in range(16):
                for sc in range(4):
                    pb2 = bank(2 * (it % 2), 2)
                    for half in range(2):
                        mm(pb2[:, half * 512:(half + 1) * 512], qib[:, hh, :],
                           kiT[:, sc * 1024 + half * 512: sc * 1024 + (half + 1) * 512], True, True)
                    rb = rbuf[it % 2]
                    act(rb, pb2, AF.Relu)
                    seg = score[:, sc * 1024:(sc + 1) * 1024]
                    stt(DVE, seg, rb, wi_sb[:, hh:hh + 1], seg, ALU.mult, ALU.add)
                    it += 1
            mid = stats[:, 32:33]
            tcol = stats[:, 33:34]
            cnt = stats[:, 34:35]
            memset(DVE, mid, 0.0)
            w = RANGE
            for itb in range(NBIS):
                memset(POOL, cnt, 0.0)
                ts(DVE, maskb, score, mid, 0.0, ALU.is_ge, ALU.add, accum=cnt)
                ts(DVE, tcol, cnt, 256.0, w, ALU.is_ge, ALU.mult)
                stt(DVE, mid, tcol, -0.5 * w, mid, ALU.add, ALU.add)
                w *= 0.5
            ts(DVE, tcol, mid, -w, None, ALU.add)
            ts(DVE, maskb, score, tcol, None, ALU.is_ge)
            for c4 in range(4):
                pb = bankb(4 + c4 % 2 * 2, 2)[:, 0:1024]
                for cc in range(8):
                    ch = c4 * 8 + cc
                    tr(pb[:, cc * 128:(cc + 1) * 128], maskb[:, ch * 128:(ch + 1) * 128], ident)
                cp(ACT if c4 % 2 else DVE, maskT[:, c4 * 8:(c4 + 1) * 8, :], pb.rearrange("p (c q) -> p c q", c=8))
            for g in range(4):
                po = bank(4 + 2 * (g % 2))
                pd = bank(5 + 2 * (g % 2))
                qsl = Qb[:, 4 * g:4 * g + 4, :].rearrange("p h q -> p (h q)")
                for jp in range(16):
                    pst = bank(2 * (jp % 2), 2)
                    for half in range(2):
                        j = jp * 2 + half
                        mm(pst[:, half * 512:(half + 1) * 512], KT[:, g, j * 128:(j + 1) * 128], qsl, True, True)
                    pt = PT[jp % 2]
                    act(pt, pst, AF.Exp, scale=ASCALE)
                    pt4 = pt.rearrange("p (j h q) -> p j h q", j=2, h=4)
                    mk = maskT[:, jp * 2:jp * 2 + 2, :].unsqueeze(2).to_broadcast([128, 2, 4, 128])
                    tt(DVE, pt4, pt4, mk, ALU.mult)
                    for half in range(2):
                        j = jp * 2 + half
                        mm(po, V[:, j, g * 128:(g + 1) * 128], pt[:, half * 512:(half + 1) * 512], j == 0, j == 31)
                        mm(pd, ones_bf, pt[:, half * 512:(half + 1) * 512], j == 0, j == 31)
                recip(rcp, pd)
                tt(DVE, attnT[:, 4 * g:4 * g + 4, tile_i * 128:(tile_i + 1) * 128],
                   po.rearrange("p (h q) -> p h q", h=4), rcp.rearrange("p (h q) -> p h q", h=4), ALU.mult)
        A.release(m_ws)

        A.release(m_kv)
        h = A.alloc([NT, D], F32)
        assert A.mark() == m_attn
        A.release(m_ws)
        for ti in range(NT):
            dma(SP, h[:, ti, :], xo[ti * 128:(ti + 1) * 128, :])
        bi = 0
        for n in range(8):
            pA = load_panel(a_w_out, 0, 8, n * 256)
            pB = load_panel(a_w_out, 1024, 8, n * 256)
            for ti in range(NT):
                pk = bank(bi % 8)[:, 0:256]
                bi += 1
                for kc in range(KC):
                    pan = pA if kc < 8 else pB
                    mm(pk, attnT[:, kc, ti * 128:(ti + 1) * 128], pan[:, kc % 8, :], kc == 0, kc == KC - 1)
                hs = h[:, ti, n * 256:(n + 1) * 256]
                tt(DVE, hs, pk, hs, ALU.add)
        A.release(m_attn)

        def dump(tag):
            if dbg == tag:
                gl = []
                for ti in range(NT):
                    gl.append(dma(SP, dbg_out[ti * 128:(ti + 1) * 128, :], h[:, ti, :]))
                P.wait_for(SP, gl)

        dump("mix0")

        def mlp_ple(layer, tiles, p_dram, p_row0):
            mk0 = A.mark()
            ntl = len(tiles)
            T = ntl * 128
            hT = A.alloc([KC, T], BF16)
            aT = A.alloc([8, T], BF16)
            rtmp = [A.alloc([512], BF16) for _ in range(2)]
            for k, ti in enumerate(tiles):
                norm_T(h[:, ti, :], hT, k * 128, 1 + 2 * layer, 16 * (k % 2))
            gw = 384 if T % 384 == 0 else 512
            ngr = T // gw
            w1 = mlp_w1[layer]
            w2 = mlp_w2[layer]
            bsel = 0
            ri = 0
            for fb in range(8):
                for pp in range(4):
                    c0 = fb * 1024 + pp * 256
                    pA = load_panel(w1, 0, 8, c0)
                    pB = load_panel(w1, 1024, 8, c0)
                    for gi in range(ngr):
                        base = 2 * (bsel % 4)
                        bsel += 1
                        for n in range(2):
                            pk = bank(base + n)
                            for kc in range(KC):
                                pan = pA if kc < 8 else pB
                                mm(pk[:, 0:gw], pan[:, kc % 8, n * 128:(n + 1) * 128], hT[:, kc, gi * gw:(gi + 1) * gw],
                                   kc == 0, kc == KC - 1)
                        for n in range(2):
                            pk = bank(base + n)
                            rt = rtmp[ri % 2]
                            ri += 1
                            act(rt[:, 0:gw], pk[:, 0:gw], AF.Relu)
                            act(aT[:, pp * 2 + n, gi * gw:(gi + 1) * gw], rt[:, 0:gw], AF.Square)
                for n in range(8):
                    pW = load_panel(w2, fb * 1024, 8, n * 256)
                    for k, ti in enumerate(tiles):
                        pk = bank(bsel % 8)[:, 0:256]
                        bsel += 1
                        for fc in range(8):
                            mm(pk, aT[:, fc, k * 128:(k + 1) * 128], pW[:, fc, :], fc == 0, fc == 7)
                        hs = h[:, ti, n * 256:(n + 1) * 256]
                        tt(DVE, hs, pk, hs, ALU.add)
            dump(f"mlp{layer}")
            for k, ti in enumerate(tiles):
                norm_T(h[:, ti, :], hT, k * 128, None, 0)
            pT = aT
            pld = [A.alloc([256], F32) for _ in range(2)]
            plb = [A.alloc([256], BF16) for _ in range(2)]
            sg = [A.alloc([256], F32) for _ in range(2)]
            for k, ti in enumerate(tiles):
                dma(SP, pld[k % 2], p_dram[p_row0 + k * 128:p_row0 + (k + 1) * 128, :])
                cp(ACT, plb[k % 2], pld[k % 2])
                pb = bankb(4 + 2 * (k % 2), 1)[:, 0:256]
                for kc in range(2):
                    tr(pb[:, kc * 128:(kc + 1) * 128], plb[k % 2][:, kc * 128:(kc + 1) * 128], ident)
                cp(DVE, pT[:, 0:2, k * 128:(k + 1) * 128], pb.rearrange("p (kc c) -> p kc c", kc=2))
            wg = ple_wg[layer]
            wp = ple_wp[layer]
            for n in range(8):
                pA = load_panel(wg, 0, 8, n * 256)
                pB = load_panel(wg, 1024, 8, n * 256)
                pP = load_panel(wp, 0, 2, n * 256)
                for k, ti in enumerate(tiles):
                    pg = bank(2 * (k % 2))[:, 0:256]
                    pe = bank(2 * (k % 2) + 1)[:, 0:256]
                    for kc in range(KC):
                        pan = pA if kc < 8 else pB
                        mm(pg, hT[:, kc, k * 128:(k + 1) * 128], pan[:, kc % 8, :], kc == 0, kc == KC - 1)
                    for kc in range(2):
                        mm(pe, pT[:, kc, k * 128:(k + 1) * 128], pP[:, kc, :], kc == 0, kc == 1)
                    s_ = sg[k % 2]
                    act(s_, pg, AF.Sigmoid)
                    tt(DVE, s_, s_, pe, ALU.mult)
                    hs = h[:, ti, n * 256:(n + 1) * 256]
                    tt(POOL, hs, hs, s_, ALU.add)
            A.release(mk0)

        mlp_ple(0, list(range(NT)), p0, 0)
        dump("l0")

        mk1 = A.mark()
        cb = A.alloc([8, KC], F32)
        dma(SP, cb[:, 0, :], b_pw1[0:1, 0:D].rearrange("o (kc p) -> p (o kc)", p=128))
        dma(SP, cb[:, 1, :], b_pw1[0:1, D:2 * D].rearrange("o (kc p) -> p (o kc)", p=128))
        dma(SP, cb[:, 2, :], b_dw.rearrange("o (kc p) -> p (o kc)", p=128))
        dma(SP, cb[:, 3, :], ln_g.rearrange("o (kc p) -> p (o kc)", p=128))
        dma(SP, cb[:, 4, :], ln_b.rearrange("o (kc p) -> p (o kc)", p=128))
        wdw = A.alloc([KC, 31], F32)
        for c in range(KC):
            dma(SP, wdw[:, c, :], w_dw[:, c * 128:(c + 1) * 128].rearrange("j p -> p j"))
        hTh = A.alloc([KC, 640], BF16)
        b2row = A.alloc([D], BF16)
        dma(POOL, b2row[0:1, :], b_pw2)
        vbuf = A.alloc([KC, 512], F32)
        ubuf = [A.alloc([640], F32) for _ in range(2)]
        sgb = [A.alloc([320], F32) for _ in range(2)]
        sqb = [A.alloc([512], F32) for _ in range(2)]
        lnm = A.alloc([3, 512], F32)
        for half in (1, 0):
            tiles = [half * 4 + k for k in range(5)]
            for k, ti in enumerate(tiles):
                norm_T(h[:, ti, :], hTh, k * 128, 2, 16 * (k % 2))
            for cq in range(8):
                pans = []
                for part in range(2):
                    pans.append((load_panel(w_pw1, 0, 8, part * D + cq * 256),
                                 load_panel(w_pw1, 1024, 8, part * D + cq * 256)))
                for cc in range(2):
                    c = cq * 2 + cc
                    ub = ubuf[c % 2]
                    for gi in range(2):
                        pa = bank(2 * gi)
                        pg = bank(2 * gi + 1)
                        for part, pk in ((0, pa), (1, pg)):
                            pA, pB = pans[part]
                            for kc in range(KC):
                                pan = pA if kc < 8 else pB
                                mm(pk[:, 0:320], pan[:, kc % 8, cc * 128:(cc + 1) * 128], hTh[:, kc, gi * 320:(gi + 1) * 320],
                                   kc == 0, kc == KC - 1)
                        s_ = sgb[gi]
                        act(s_, pg[:, 0:320], AF.Sigmoid, bias=cb[:, 1, c:c + 1])
                        stt(DVE, ub[:, gi * 320:(gi + 1) * 320], pa[:, 0:320], cb[:, 0, c:c + 1], s_, ALU.add, ALU.mult)
                    if half == 0:
                        ts(DVE, ub[:, 0:128], ub[:, 0:128], hflag_sb[:, 0:1], None, ALU.mult)
                    eng = DVE
                    acc = vbuf[:, c, :]
                    ts(eng, acc, ub[:, 98:98 + 512], wdw[:, c, 0:1], cb[:, 2, c:c + 1], ALU.mult, ALU.add)
                    for j in range(1, 31):
                        stt(eng, acc, ub[:, 98 + j:98 + j + 512], wdw[:, c, j:j + 1], acc, ALU.mult, ALU.add)
            pm = bank(4)
            pq = bank(5)
            for c in range(KC):
                mm(pm, ones_d, vbuf[:, c, :], c == 0, c == KC - 1)
            for c in range(KC):
                sq = sqb[c % 2]
                act(sq, vbuf[:, c, :], AF.Square)
                mm(pq, ones_d, sq, c == 0, c == KC - 1)
            cp(ACT, lnm[:, 0, :], pm)
            tt(DVE, lnm[:, 1, :], lnm[:, 0, :], lnm[:, 0, :], ALU.mult)
            tt(DVE, lnm[:, 1, :], pq, lnm[:, 1, :], ALU.subtract)
            ts(DVE, lnm[:, 1, :], lnm[:, 1, :], 1e-5, None, ALU.add)
            recip(lnm[:, 2, :], lnm[:, 1, :])
            act(lnm[:, 1, :], lnm[:, 2, :], AF.Sqrt)
            zT = hTh
            for c in range(KC):
                sq = sqb[c % 2]
                eng = DVE if c % 2 == 0 else POOL
                tt(eng, sq, vbuf[:, c, :], lnm[:, 0, :], ALU.subtract)
                tt(eng, sq, sq, lnm[:, 1, :], ALU.mult)
                act(zT[:, c, 0:512], sq, AF.Silu, bias=cb[:, 4, c:c + 1], scale=cb[:, 3, c:c + 1])
            bi = 0
            for n in range(8):
                pA = load_panel(w_pw2, 0, 8, n * 256)
                pB = load_panel(w_pw2, 1024, 8, n * 256)
                for k in range(4):
                    ti = 1 + half * 4 + k
                    pk = bank(bi % 4)[:, 0:256]
                    bi += 1
                    for kc in range(KC):
                        pan = pA if kc < 8 else pB
                        mm(pk, zT[:, kc, k * 128:(k + 1) * 128], pan[:, kc % 8, :], kc == 0, False)
                    mm(pk, ones_bf[0:1, :], b2row[0:1, n * 256:(n + 1) * 256], False, True)
                    hs = h[:, ti, n * 256:(n + 1) * 256]
                    tt(DVE, hs, pk, hs, ALU.add)
        A.release(mk1)
        dump("mix1")

        mlp_ple(1, list(range(1, NT)), p1, 0)
        dump("l1")

        gbc = A.alloc([D], F32)
        dma(SP, gbc, g_fin[0].partition_broadcast(128))
        ob = [A.alloc([D], F32) for _ in range(2)]
        outs = []
        for k in range(8):
            ti = 1 + k
            o = ob[k % 2]
            c0 = 48 + 4 * (k % 2)
            memset(DVE, stats[:, c0:c0 + 1], 0.0)
            act(o, h[:, ti, :], AF.Square, accum=stats[:, c0:c0 + 1])
            ts(DVE, stats[:, c0 + 1:c0 + 2], stats[:, c0:c0 + 1], 1.0 / D, 1e-6, ALU.mult, ALU.add)
            recip(stats[:, c0 + 2:c0 + 3], stats[:, c0 + 1:c0 + 2])
            act(stats[:, c0 + 3:c0 + 4], stats[:, c0 + 2:c0 + 3], AF.Sqrt)
            stt(DVE, o, h[:, ti, :], stats[:, c0 + 3:c0 + 4], gbc, ALU.mult, ALU.mult)
            outs.append(dma(SP, y[k * 128:(k + 1) * 128, :], o))
        P.wait_for(SP, outs)
        P.emit()
        build_program.stats = (len(P.ops), P.nwaits)
    return nc


def _consts():
    d = np.arange(128)
    cst = np.zeros((128, 16), np.float32)
    inv128 = (10000.0 ** (-(d % 64).astype(np.float32) * np.float32(2.0 / 128))).astype(np.float32)
    inv64 = (10000.0 ** (-(d % 32).astype(np.float32) * np.float32(2.0 / 64))).astype(np.float32)
    inv64[64:] = 0.0
    cst[:, 0] = inv128
    cst[:, 1] = inv64
    cst[:, 2] = math.pi / 2
    cst[:, 3] = np.where(d < 64, math.pi, 0.0)
    cst[:, 4] = math.pi / 2
    cst[:, 5] = np.where(d < 32, math.pi, 0.0)
    ident = np.eye(128, dtype=np.float32)
    psw128 = np.zeros((128, 128), np.float32)
    for m in range(128):
        psw128[(m + 64) % 128, m] = 1.0
    psw64 = np.zeros((128, 128), np.float32)
    for m in range(64):
        psw64[(m + 32) % 64, m] = 1.0
    chunkid = np.tile(np.arange(64, dtype=np.float32)[None, :], (128, 1))
    return cst, ident, psw128, psw64, chunkid


_CACHE = {}


def make_in_maps(inputs):
    x = np.ascontiguousarray(inputs["x"], dtype=np.float32)
    p = np.ascontiguousarray(inputs["p"], dtype=np.float32)
    pos = np.ascontiguousarray(inputs["positions"], dtype=np.int32)
    cst, ident, psw128, psw64, chunkid = _consts()
    shared = {
        "cst": cst, "ident": ident, "psw128": psw128, "psw64": psw64, "chunkid": chunkid,
        "norm_mix_g": np.ascontiguousarray(inputs["norm_mix_g"], np.float32),
        "norm_mlp_g": np.ascontiguousarray(inputs["norm_mlp_g"], np.float32),
        "final_g": np.ascontiguousarray(inputs["final_g"], np.float32).reshape(1, D),
        "a_w_in": np.ascontiguousarray(inputs["a_w_in"][0], np.float32),
        "a_w_out": np.ascontiguousarray(inputs["a_w_out"][0], np.float32),
        "a_kidx_g": np.ascontiguousarray(inputs["a_kidx_g"][0], np.float32).reshape(128, 1),
        "a_kidx_b": np.ascontiguousarray(inputs["a_kidx_b"][0], np.float32).reshape(128, 1),
        "b_w_pw1": np.ascontiguousarray(inputs["b_w_pw1"][0], np.float32),
        "b_b_pw1": np.ascontiguousarray(inputs["b_b_pw1"][0], np.float32).reshape(1, 2 * D),
        "b_w_dw": np.ascontiguousarray(inputs["b_w_dw"][0], np.float32),
        "b_b_dw": np.ascontiguousarray(inputs["b_b_dw"][0], np.float32).reshape(1, D),
        "b_ln_g": np.ascontiguousarray(inputs["b_ln_g"][0], np.float32).reshape(1, D),
        "b_ln_b": np.ascontiguousarray(inputs["b_ln_b"][0], np.float32).reshape(1, D),
        "b_w_pw2": np.ascontiguousarray(inputs["b_w_pw2"][0], np.float32),
        "b_b_pw2": np.ascontiguousarray(inputs["b_b_pw2"][0], np.float32).reshape(1, D),
        "mlp_w1": np.ascontiguousarray(inputs["mlp_w1"], np.float32),
        "mlp_w2": np.ascontiguousarray(inputs["mlp_w2"], np.float32),
        "ple_w_proj": np.ascontiguousarray(inputs["ple_w_proj"], np.float32),
        "ple_w_gate": np.ascontiguousarray(inputs["ple_w_gate"], np.float32),
    }
    maps = []
    for c in range(8):
        b = c // 4
        t0 = (c % 4) * 1024
        first = (c % 4) == 0
        rows = np.arange(t0 - 128, t0 + 1024)
        if first:
            rows[:128] = np.arange(0, 128)
        m = dict(shared)
        m["xs"] = x[b]
        m["xo"] = np.ascontiguousarray(x[b][rows])
        m["pos_s"] = pos[b].reshape(1, S)
        m["pos_o"] = np.ascontiguousarray(pos[b][rows]).reshape(1, T0)
        m["p0"] = np.ascontiguousarray(p[0, b][rows])
        m["p1"] = np.ascontiguousarray(p[1, b][t0:t0 + 1024])
        m["qinfo"] = np.ascontiguousarray((rows // 64).astype(np.float32).reshape(NT, 128).T)
        m["hflag"] = np.full((128, 1), 0.0 if first else 1.0, np.float32)
        maps.append(m)
    return maps


def kernel(**inputs):
    if "nc" not in _CACHE:
        _CACHE["nc"] = build_program()
    nc = _CACHE["nc"]
    maps = make_in_maps(inputs)
    res = run_bass_kernel_spmd(nc, maps, core_ids=list(range(8)))
    out = np.empty((2, S, D), np.float32)
    for c in range(8):
        b = c // 4
        t0 = (c % 4) * 1024
        out[b, t0:t0 + 1024] = res.results[c]["y"]
    return out
```

```python
import math
import numpy as np
from contextlib import ExitStack
import concourse.bass as bass
import concourse.mybir as mybir
from concourse.bass_utils import run_bass_kernel_spmd

F32 = mybir.dt.float32
BF16 = mybir.dt.bfloat16
I32 = mybir.dt.int32
AF = mybir.ActivationFunctionType
ALU = mybir.AluOpType
AX = mybir.AxisListType

PE, ACT, DVE, POOL, SP = range(5)
NENG = 5
DSIZE = {F32: 4, BF16: 2, I32: 4}

D = 2048
S = 4096
NT = 9
T0 = NT * 128
KC = 16
DFF = 8192
IN_A = 5264
SB_BYTES = 176 * 1024
TWO_PI = 2.0 * math.pi


class Op:
    __slots__ = ("gid", "eng", "fn", "deps", "is_dma", "signal", "sem", "val")


class Prog:
    PAGE = 64
    NDMA_SEMS = 32

    def __init__(self, nc, sb_bytes, ps_bytes=16384):
        self.nc = nc
        self.ops = []
        self.np_sb = sb_bytes // self.PAGE
        self.np_ps = ps_bytes // self.PAGE
        self.lw = {"sb": np.full(self.np_sb, -1, np.int64), "ps": np.full(self.np_ps, -1, np.int64)}
        self.rd = {
            "sb": np.full((NENG + 1, self.np_sb), -1, np.int64),
            "ps": np.full((NENG + 1, self.np_ps), -1, np.int64),
        }
        self.bases = {}
        self.bank_last = np.full((8, NENG), -1, np.int64)

    def rng(self, ap):
        name = ap.tensor.name
        if name not in self.bases:
            return None
        space, base = self.bases[name]
        pstep = ap.ap[0][0]
        ds = DSIZE[ap.dtype]
        off = ap.offset % pstep if pstep > 0 else ap.offset
        ext = 1
        for st, cnt in ap.ap[1:]:
            ext += abs(st) * (cnt - 1)
        lo = base + off * ds
        hi = lo + ext * ds
        return space, lo // self.PAGE, (hi + self.PAGE - 1) // self.PAGE

    def add(self, eng, fn, reads=(), writes=(), dma=False, extra=()):
        op = Op()
        op.gid = len(self.ops)
        op.eng = eng
        op.fn = fn
        op.is_dma = dma
        op.signal = dma
        op.sem = None
        op.val = 0
        deps = set()
        ridx = NENG if dma else eng
        rr_ = [self.rng(a) for a in reads]
        ww_ = [self.rng(a) for a in writes]
        for r in rr_:
            if r is None:
                continue
            sp, a, b = r
            w = self.lw[sp][a:b]
            deps.update(np.unique(w[w >= 0]).tolist())
        for r in ww_:
            if r is None:
                continue
            sp, a, b = r
            w = self.lw[sp][a:b]
            deps.update(np.unique(w[w >= 0]).tolist())
            rr = self.rd[sp][:, a:b]
            for e in range(NENG + 1):
                if e == eng and not dma and e != NENG:
                    continue
                x = rr[e]
                deps.update(np.unique(x[x >= 0]).tolist())
        for r in rr_:
            if r is None:
                continue
            sp, a, b = r
            self.rd[sp][ridx, a:b] = op.gid
        for r in ww_:
            if r is None:
                continue
            sp, a, b = r
            self.lw[sp][a:b] = op.gid
            self.rd[sp][:, a:b] = -1
        if not dma:
            bpp = 2048 // self.PAGE
            for r in rr_ + ww_:
                if r is None or r[0] != "ps":
                    continue
                for bk in range(r[1] // bpp, (r[2] - 1) // bpp + 1):
                    for e in range(NENG):
                        if e != eng and self.bank_last[bk, e] >= 0:
                            deps.add(int(self.bank_last[bk, e]))
                    self.bank_last[bk, eng] = op.gid
        deps.update(extra)
        deps.discard(op.gid)
        best = {}
        out = set()
        for d in deps:
            o = self.ops[d]
            if o.is_dma:
                out.add(d)
            else:
                if o.eng == PE and eng == PE and not dma:
                    continue
                if o.eng not in best or best[o.eng] < d:
                    best[o.eng] = d
        out.update(best.values())
        op.deps = out
        self.ops.append(op)
        return op.gid

    def wait_for(self, eng, gids):
        op = Op()
        op.gid = len(self.ops)
        op.eng = eng
        op.fn = None
        op.is_dma = False
        op.signal = False
        op.sem = None
        op.val = 0
        op.deps = set(gids)
        self.ops.append(op)
        return op.gid

    def emit(self):
        nc = self.nc
        with ExitStack() as es:
            esems = [es.enter_context(nc.semaphore(f"e{i}")) for i in range(NENG)]
            dsems = [es.enter_context(nc.semaphore(f"d{i}")) for i in range(self.NDMA_SEMS)]
            dcount = [0] * self.NDMA_SEMS
            dlast = [None] * self.NDMA_SEMS
            k = 0
            for op in self.ops:
                if op.is_dma:
                    s = k % self.NDMA_SEMS
                    k += 1
                    if dlast[s] is not None:
                        op.deps.add(dlast[s])
                    dcount[s] += 1
                    op.sem = dsems[s]
                    op.val = 16 * dcount[s]
                    dlast[s] = op.gid
            for op in self.ops:
                for d in op.deps:
                    self.ops[d].signal = True
            ecount = [0] * NENG
            for op in self.ops:
                if op.is_dma or op.fn is None:
                    continue
                if op.signal:
                    ecount[op.eng] += 1
                    op.sem = esems[op.eng]
                    op.val = ecount[op.eng]
            per = [[] for _ in range(NENG)]
            for op in self.ops:
                per[op.eng].append(op)
            ops = self.ops
            self.nwaits = 0

            def run(eng_idx, e):
                waited = {}
                for op in per[eng_idx]:
                    for d in sorted(op.deps):
                        o = ops[d]
                        key = id(o.sem)
                        if waited.get(key, 0) < o.val:
                            e.wait_ge(o.sem, o.val)
                            waited[key] = o.val
                            self.nwaits += 1
                    if op.fn is None:
                        continue
                    ins = op.fn(e)
                    if op.signal:
                        ins.then_inc(op.sem, 16 if op.is_dma else 1)

            with nc.Block() as block:
                @block.tensor
                def _(e):
                    run(PE, e)

                @block.scalar
                def _(e):
                    run(ACT, e)

                @block.vector
                def _(e):
                    run(DVE, e)

                @block.gpsimd
                def _(e):
                    run(POOL, e)

                @block.sync
                def _(e):
                    run(SP, e)


class Arena:
    def __init__(self, ar):
        self.ar = ar
        self.top = 0

    def mark(self):
        return self.top

    def release(self, m):
        self.top = m

    def alloc(self, shape, dt):
        n = 1
        for s in shape:
            n *= s
        nb = (n * DSIZE[dt] + 63) // 64 * 64
        assert self.top + nb <= SB_BYTES, f"SBUF arena overflow {self.top + nb}"
        v = self.ar[:, self.top // 4:(self.top + nb) // 4]
        self.top += nb
        if dt != F32:
            v = v.bitcast(dt)
        v = v[:, :n]
        if len(shape) == 2:
            v = v.rearrange("p (a b) -> p a b", a=shape[0])
        elif len(shape) == 3:
            v = v.rearrange("p (a b c) -> p a b c", a=shape[0], b=shape[1])
        return v


class _Stop(Exception):
    pass


def build_program(dbg=None, stop=None):
    try:
        return _build_program(dbg, stop)
    except _Stop as e:
        return e.args[0]


def _build_program(dbg=None, stop=None):
    nc = bass.Bass("TRN2", target_bir_lowering=False)
    dram = {}

    def din(name, shape, dt=F32):
        dram[name] = nc.dram_tensor(name, list(shape), dt, kind="ExternalInput").ap()
        return dram[name]

    xs = din("xs", [S, D])
    xo = din("xo", [T0, D])
    pos_s = din("pos_s", [1, S], I32)
    pos_o = din("pos_o", [1, T0], I32)
    p0 = din("p0", [T0, 256])
    p1 = din("p1", [1024, 256])
    qinfo = din("qinfo", [128, NT])
    hflag = din("hflag", [128, 1])
    cst = din("cst", [128, 16])
    ident_d = din("ident", [128, 128])
    psw128_d = din("psw128", [128, 128])
    psw64_d = din("psw64", [128, 128])
    chunkid_d = din("chunkid", [128, 64])
    g_mix = din("norm_mix_g", [2, D])
    g_mlp = din("norm_mlp_g", [2, D])
    g_fin = din("final_g", [1, D])
    a_w_in = din("a_w_in", [D, IN_A])
    a_w_out = din("a_w_out", [D, D])
    kidx_g = din("a_kidx_g", [128, 1])
    kidx_b = din("a_kidx_b", [128, 1])
    w_pw1 = din("b_w_pw1", [D, 2 * D])
    b_pw1 = din("b_b_pw1", [1, 2 * D])
    w_dw = din("b_w_dw", [31, D])
    b_dw = din("b_b_dw", [1, D])
    ln_g = din("b_ln_g", [1, D])
    ln_b = din("b_ln_b", [1, D])
    w_pw2 = din("b_w_pw2", [D, D])
    b_pw2 = din("b_b_pw2", [1, D])
    mlp_w1 = din("mlp_w1", [2, D, DFF])
    mlp_w2 = din("mlp_w2", [2, DFF, D])
    ple_wp = din("ple_w_proj", [2, 256, D])
    ple_wg = din("ple_w_gate", [2, D, D])
    y = nc.dram_tensor("y", [1024, D], F32, kind="ExternalOutput").ap()
    dbg_out = None
    if dbg:
        dbg_out = nc.dram_tensor("dbg", [T0, D], F32, kind="ExternalOutput").ap()

    with ExitStack() as es:
        ar = es.enter_context(nc.sbuf_tensor("arena", [128, SB_BYTES // 4], F32))
        ps = es.enter_context(nc.psum_tensor("ps", [128, 4096], F32))
        P = Prog(nc, SB_BYTES)
        P.bases["arena"] = ("sb", 0)
        P.bases["ps"] = ("ps", 0)
        A = Arena(ar)
        es.enter_context(nc.allow_non_contiguous_dma(reason="small per-partition constant loads"))

        def bank(i, n=1):
            return ps[:, i * 512:(i + n) * 512]

        def bankb(i, n=1):
            return ps[:, i * 512:(i + n) * 512].bitcast(BF16)

        def mm(out, lhsT, rhs, start, stop):
            P.add(PE, lambda e: e.matmul(out, lhsT=lhsT, rhs=rhs, start=start, stop=stop),
                  reads=[lhsT, rhs], writes=[out])

        def tr(out, in_, ident):
            P.add(PE, lambda e: e.transpose(out, in_, ident), reads=[in_, ident], writes=[out])

        def act(out, in_, func, bias=None, scale=None, accum=None, eng=ACT):
            kw = {}
            rd = [in_]
            wr = [out]
            if bias is not None:
                kw["bias"] = bias
                if not isinstance(bias, float):
                    rd.append(bias)
            if scale is not None:
                kw["scale"] = scale
                if not isinstance(scale, float):
                    rd.append(scale)
            if accum is not None:
                kw["accum_out"] = accum
                wr.append(accum)
            P.add(ACT, lambda e: e.activation(out=out, in_=in_, func=func, **kw), reads=rd, writes=wr)

        def ts(eng, out, in0, s1, s2, op0, op1=None, accum=None):
            rd = [in0]
            wr = [out]
            if not isinstance(s1, float):
                rd.append(s1)
            if s2 is not None and not isinstance(s2, float):
                rd.append(s2)
            kw = {}
            if op1 is not None:
                kw["op1"] = op1
            if accum is not None:
                kw["accum_out"] = accum
                wr.append(accum)
            P.add(eng, lambda e: e.tensor_scalar(out=out, in0=in0, scalar1=s1, scalar2=s2, op0=op0, **kw),
                  reads=rd, writes=wr)

        def tt(eng, out, in0, in1, op):
            P.add(eng, lambda e: e.tensor_tensor(out=out, in0=in0, in1=in1, op=op), reads=[in0, in1], writes=[out])

        def stt(eng, out, in0, sc, in1, op0, op1):
            rd = [in0, in1]
            if not isinstance(sc, float):
                rd.append(sc)
            P.add(eng, lambda e: e.scalar_tensor_tensor(out=out, in0=in0, scalar=sc, in1=in1, op0=op0, op1=op1),
                  reads=rd, writes=[out])

        def cp(eng, out, in_):
            if eng == ACT:
                act(out, in_, AF.Copy)
            else:
                P.add(eng, lambda e: e.tensor_copy(out=out, in_=in_), reads=[in_], writes=[out])

        def recip(out, in_):
            P.add(DVE, lambda e: e.reciprocal(out=out, in_=in_), reads=[in_], writes=[out])

        def memset(eng, out, v):
            P.add(eng, lambda e: e.memset(out, v), writes=[out])

        def dma(eng, out, in_, extra=()):
            return P.add(eng, lambda e: e.dma_start(out=out, in_=in_), reads=[in_], writes=[out], dma=True, extra=extra)

        def early(tag, views):
            if stop != tag:
                return
            gl = []
            for i, v in enumerate(views):
                ncol = v.shape[-1]
                gl.append(dma(SP, y[i * 128:(i + 1) * 128, 0:ncol], v))
            P.wait_for(SP, gl)
            P.emit()
            build_program.stats = (len(P.ops), P.nwaits)
            raise _Stop(nc)

        ident = A.alloc([128], BF16)
        psw128 = A.alloc([128], BF16)
        psw64 = A.alloc([128], BF16)
        ones_bf = A.alloc([128], BF16)
        ones_f = A.alloc([128], F32)
        ones_d = A.alloc([128], F32)
        cst_sb = A.alloc([16], F32)
        qinfo_sb = A.alloc([NT], F32)
        hflag_sb = A.alloc([1], F32)
        chunkid = A.alloc([64], F32)
        gfm = A.alloc([4, KC], F32)
        kig = A.alloc([2], F32)
        stats = A.alloc([64], F32)
        dma(POOL, ident, ident_d)
        dma(POOL, psw128, psw128_d)
        dma(POOL, psw64, psw64_d)
        dma(SP, cst_sb, cst)
        dma(SP, qinfo_sb, qinfo)
        dma(SP, hflag_sb, hflag)
        dma(SP, chunkid, chunkid_d)
        dma(SP, kig[:, 0:1], kidx_g)
        dma(SP, kig[:, 1:2], kidx_b)
        for i, gsrc in enumerate([g_mix[0:1, :], g_mlp[0:1, :], g_mix[1:2, :], g_mlp[1:2, :]]):
            dma(SP, gfm[:, i, :], gsrc.rearrange("o (kc p) -> p (o kc)", p=128))
        memset(DVE, ones_bf, 1.0)
        memset(DVE, ones_f, 1.0 / 128.0)
        memset(DVE, ones_d, 1.0 / D)

        early('consts', [cst_sb, chunkid, gfm.rearrange('p a b -> p (a b)'), ident.bitcast(F32)])
        NPB = 4
        panels = [A.alloc([8, 256], BF16) for _ in range(NPB)]
        pstate = {"i": 0}

        def load_panel(w2d, r0, nk, c0, ncols=256):
            buf = panels[pstate["i"] % NPB]
            pstate["i"] += 1
            src = w2d[r0:r0 + nk * 128, c0:c0 + ncols].rearrange("(kc p) n -> p kc n", p=128)
            dma(POOL, buf[:, 0:nk, 0:ncols], src)
            return buf

        hn_tm = [A.alloc([D], BF16)] * 2
        nstate = {"i": 0}

        def norm_T(src, dstT, col0, gidx, st_col):
            i = nstate["i"]
            nstate["i"] += 1
            hb = hn_tm[i % 2]
            if gidx is not None:
                ss = stats[:, st_col:st_col + 1]
                memset(DVE, ss, 0.0)
                act(hb, src, AF.Square, accum=ss)
                ts(DVE, stats[:, st_col + 1:st_col + 2], ss, 1.0 / D, 1e-6, ALU.mult, ALU.add)
                recip(stats[:, st_col + 2:st_col + 3], stats[:, st_col + 1:st_col + 2])
                act(stats[:, st_col + 3:st_col + 4], stats[:, st_col + 2:st_col + 3], AF.Sqrt)
                ts(DVE, hb, src, stats[:, st_col + 3:st_col + 4], None, ALU.mult)
            else:
                cp(ACT, hb, src)
            pb = bankb(4 + 2 * (i % 2), 2)
            for kc in range(KC):
                tr(pb[:, kc * 128:(kc + 1) * 128], hb[:, kc * 128:(kc + 1) * 128], ident)
            if gidx is not None:
                for kc in range(KC):
                    o = dstT[:, kc, col0:col0 + 128]
                    i_ = pb[:, kc * 128:(kc + 1) * 128]
                    if kc < 8:
                        act(o, i_, AF.Copy, scale=gfm[:, gidx, kc:kc + 1])
                    else:
                        ts(DVE, o, i_, gfm[:, gidx, kc:kc + 1], None, ALU.mult)
            else:
                o = dstT[:, :, col0:col0 + 128]
                i_ = pb.rearrange("p (kc c) -> p kc c", kc=KC)
                cp(DVE if i % 2 else ACT, o, i_)

        def rope_tables(posb_i, n, tabs, tf):
            posf = tabs[:, 4, :]
            cp(DVE, posf, posb_i)
            x = tf[:, 0, 0:n]
            kf = tf[:, 1, 0:n]
            for (ti, inv_c, ph_c) in ((0, 0, 2), (1, 0, 3), (2, 1, 4), (3, 1, 5)):
                ts(DVE, x, posf, cst_sb[:, inv_c:inv_c + 1], cst_sb[:, ph_c:ph_c + 1], ALU.mult, ALU.add)
                ts(DVE, posb_i, x, 1.0 / TWO_PI, None, ALU.mult)
                cp(DVE, kf, posb_i)
                stt(DVE, x, kf, -TWO_PI, x, ALU.mult, ALU.add)
                ts(DVE, kf, x, math.pi, -TWO_PI, ALU.is_gt, ALU.mult)
                tt(DVE, x, x, kf, ALU.add)
                ts(DVE, x, x, math.pi, -math.pi, ALU.min, ALU.max)
                act(tabs[:, ti, :], x, AF.Sin)

        rstate = {"i": 0}

        def rope_apply(dst, src_ps, n, Ct, St, psw, tmpbs, tmpfs, pbanks):
            i = rstate["i"]
            rstate["i"] += 1
            tmpb = tmpbs[i % len(tmpbs)]
            tmpf = tmpfs[i % len(tmpfs)]
            pbank = pbanks[i % len(pbanks)]
            cp(ACT, tmpb[:, 0:n], src_ps)
            mm(pbank[:, 0:n], psw, tmpb[:, 0:n], True, True)
            tt(POOL, tmpf[:, 0, 0:n], tmpb[:, 0:n], Ct, ALU.mult)
            tt(DVE, tmpf[:, 1, 0:n], pbank[:, 0:n], St, ALU.mult)
            tt(POOL, dst, tmpf[:, 0, 0:n], tmpf[:, 1, 0:n], ALU.add)

        qscr = nc.dram_tensor("qscr", [NT, 128, 2048], BF16).ap()
        qiscr = nc.dram_tensor("qiscr", [NT, 128, 2048], BF16).ap()
        wiscr = nc.dram_tensor("wiscr", [NT, 128, 16], F32).ap()
        m_kv = A.mark()
        KT = A.alloc([4, S], BF16)
        V = A.alloc([32, 512], BF16)
        kiT = A.alloc([S], BF16)
        m_attn = A.mark()
        assert m_attn - m_kv == NT * D * 4
        wkv = A.alloc([KC, 1152], BF16)
        for kb in range(2):
            dma(POOL, wkv[:, kb * 8:(kb + 1) * 8, 0:1024],
                a_w_in[kb * 1024:(kb + 1) * 1024, 2048:3072].rearrange("(kc p) n -> p kc n", p=128))
            dma(POOL, wkv[:, kb * 8:(kb + 1) * 8, 1024:1152],
                a_w_in[kb * 1024:(kb + 1) * 1024, 5120:5248].rearrange("(kc p) n -> p kc n", p=128))
        early('a1_w', [wkv[:, 0, :].bitcast(F32), wkv[:, 15, :].bitcast(F32)])
        G1 = 256
        xt = [A.alloc([D], F32)] * 2
        hnT2 = [A.alloc([KC, G1], BF16) for _ in range(2)]
        tabs2 = [A.alloc([5, G1], F32) for _ in range(2)]
        posb2 = [A.alloc([G1], I32) for _ in range(2)]
        tmpbs = [A.alloc([G1], BF16) for _ in range(2)]
        tmpfs = [A.alloc([2, G1], F32) for _ in range(2)]
        lnb = A.alloc([3, G1], F32)
        for sg in range(S // G1):
            s0 = sg * G1
            hnT = hnT2[sg % 2]
            tabs = tabs2[sg % 2]
            posb = posb2[sg % 2]
            tmpf = tmpfs[sg % 2]
            dma(SP, posb, pos_s[0, s0:s0 + G1].partition_broadcast(128))
            rope_tables(posb, G1, tabs, tmpf)
            if sg == 0:
                early('a1_tab', [tabs.rearrange('p a b -> p (a b)')])
            for ti in range(2):
                xb = xt[ti % 2]
                dma(SP, xb, xs[s0 + ti * 128:s0 + (ti + 1) * 128, :])
                norm_T(xb, hnT, ti * 128, 0, 16 * (ti % 2))
            if sg == 0:
                early('a1_n', [hnT[:, 0:8, :].rearrange('p a b -> p (a b)').bitcast(F32), hnT[:, 8:16, :].rearrange('p a b -> p (a b)').bitcast(F32)])
            for g in range(4):
                pk = bank(g % 2)[:, 0:G1]
                for kc in range(KC):
                    mm(pk, wkv[:, kc, g * 128:(g + 1) * 128], hnT[:, kc, :], kc == 0, kc == KC - 1)
                rope_apply(KT[:, g, s0:s0 + G1], pk, G1, tabs[:, 0, :], tabs[:, 1, :], psw128, tmpbs, tmpfs, [bank(2), bank(4)])
            for ti in range(2):
                pv = bank(ti % 2)
                for kc in range(KC):
                    mm(pv, hnT[:, kc, ti * 128:(ti + 1) * 128], wkv[:, kc, 512:1024], kc == 0, kc == KC - 1)
                cp(ACT if ti % 2 else DVE, V[:, sg * 2 + ti, :], pv)
            pk = bank(3)[:, 0:G1]
            pm_ = bank(5)[:, 0:G1]
            for kc in range(KC):
                mm(pk, wkv[:, kc, 1024:1152], hnT[:, kc, :], kc == 0, kc == KC - 1)
            cp(ACT, lnb[:, 0, :], pk)
            mm(pm_, ones_f, lnb[:, 0, :], True, True)
            tt(DVE, lnb[:, 1, :], lnb[:, 0, :], pm_, ALU.subtract)
            act(lnb[:, 2, :], lnb[:, 1, :], AF.Square)
            mm(pm_, ones_f, lnb[:, 2, :], True, True)
            ts(DVE, lnb[:, 0, :], pm_, 1e-5, None, ALU.add)
            recip(lnb[:, 2, :], lnb[:, 0, :])
            act(lnb[:, 0, :], lnb[:, 2, :], AF.Sqrt)
            tt(DVE, lnb[:, 1, :], lnb[:, 1, :], lnb[:, 0, :], ALU.mult)
            ts(DVE, lnb[:, 2, :], lnb[:, 1, :], kig[:, 0:1], kig[:, 1:2], ALU.mult, ALU.add)
            rope_apply(kiT[:, s0:s0 + G1], lnb[:, 2, :], G1, tabs[:, 2, :], tabs[:, 3, :], psw64, tmpbs, tmpfs, [bank(2), bank(4)])
            if sg == 0:
                early('a1_g0', [KT[:, 0, 0:512].bitcast(F32), KT[:, 3, 0:512].bitcast(F32), kiT[:, 0:512].bitcast(F32), V[:, 0, :].bitcast(F32), V[:, 1, :].bitcast(F32)])
        early('a1', [KT[:, 0, :].bitcast(F32), KT[:, 3, :].bitcast(F32), kiT.bitcast(F32), V[:, 0:8, :].rearrange('p a b -> p (a b)').bitcast(F32), V[:, 24:32, :].rearrange('p a b -> p (a b)').bitcast(F32)])
        A.release(m_attn)

        attnT = A.alloc([KC, T0], BF16)
        m_ws = A.mark()
        hnTg = A.alloc([KC, 384], BF16)
        tabs_o = A.alloc([5, 384], F32)
        posb_o = A.alloc([384], I32)
        tmpb_os = [A.alloc([384], BF16) for _ in range(2)]
        tmpf_os = [A.alloc([2, 384], F32) for _ in range(2)]
        tmpf_o = tmpf_os[0]
        stage = [A.alloc([384], BF16) for _ in range(2)]
        wwi = A.alloc([KC, 16], BF16)
        wist = A.alloc([3, 16], F32)
        xt2 = [A.alloc([D], F32)] * 2
        for kb in range(2):
            dma(POOL, wwi[:, kb * 8:(kb + 1) * 8, :],
                a_w_in[kb * 1024:(kb + 1) * 1024, 5248:5264].rearrange("(kc p) n -> p kc n", p=128))
        WSCALE = (16 ** -0.5) * (128 ** -0.5)
        ASCALE = 128 ** -0.5
        NBIS = 26
        RANGE = 64.0
        wr_gids = [[] for _ in range(NT)]
        sti = 0
        for grp in range(3):
            t0g = grp * 384
            dma(SP, posb_o, pos_o[0, t0g:t0g + 384].partition_broadcast(128))
            rope_tables(posb_o, 384, tabs_o, tmpf_o)
            for ti in range(3):
                xb = xt2[ti % 2]
                dma(SP, xb, xo[t0g + ti * 128:t0g + (ti + 1) * 128, :])
                norm_T(xb, hnTg, ti * 128, 0, 16 * (ti % 2))
            for (scr, c_base, Ct, St, psw) in ((qscr, 0, 0, 1, psw128), (qiscr, 3072, 2, 3, psw64)):
                for hp in range(8):
                    pA = load_panel(a_w_in, 0, 8, c_base + hp * 256)
                    pB = load_panel(a_w_in, 1024, 8, c_base + hp * 256)
                    for n in range(2):
                        pk = bank(2 * (hp % 2) + n)
                        for kc in range(KC):
                            pan = pA if kc < 8 else pB
                            mm(pk[:, 0:384], pan[:, kc % 8, n * 128:(n + 1) * 128], hnTg[:, kc, :], kc == 0, kc == KC - 1)
                    for n in range(2):
                        hd = hp * 2 + n
                        st_ = stage[sti % 2]
                        sti += 1
                        rope_apply(st_, bank(2 * (hp % 2) + n)[:, 0:384], 384, tabs_o[:, Ct, :], tabs_o[:, St, :],
                                   psw, tmpb_os, tmpf_os, [bank(6), bank(7)])
                        gd = dma(SP, scr[grp * 3:(grp + 1) * 3, :, hd * 128:(hd + 1) * 128].rearrange("t p q -> p t q"),
                                 st_.rearrange("p (t q) -> p t q", t=3))
                        for k3 in range(3):
                            wr_gids[grp * 3 + k3].append(gd)
            for ti in range(3):
                pw = bank(ti % 2)
                for kc in range(KC):
                    mm(pw[:, 0:16], hnTg[:, kc, ti * 128:(ti + 1) * 128], wwi[:, kc, :], kc == 0, kc == KC - 1)
                ts(DVE, wist[:, ti, :], pw[:, 0:16], WSCALE, None, ALU.mult)
                gd = dma(SP, wiscr[grp * 3 + ti], wist[:, ti, :])
                wr_gids[grp * 3 + ti].append(gd)
        early('a2', [tabs_o.rearrange('p a b -> p (a b)'), hnTg[:, 0:4, :].rearrange('p a b -> p (a b)').bitcast(F32)])
        A.release(m_ws)

        Qb = A.alloc([16, 128], BF16)
        qib = A.alloc([16, 128], BF16)
        wi_sb = A.alloc([16], F32)
        cntb = A.alloc([32], F32)
        maskT = A.alloc([32, 128], BF16)
        score = A.alloc([S], F32)
        rbreg = A.alloc([2048], F32)
        rbuf = [rbreg[:, 0:1024], rbreg[:, 1024:2048]]
        maskb = rbreg.bitcast(BF16)
        PT = [score[:, 0:512].bitcast(BF16), score[:, 512:1024].bitcast(BF16)]
        rcp = score[:, 1024:1536]
        for tile_i in range(NT):
            dma(SP, Qb.rearrange("p h q -> p (h q)"), qscr[tile_i], extra=wr_gids[tile_i])
            dma(SP, qib.rearrange("p h q -> p (h q)"), qiscr[tile_i], extra=wr_gids[tile_i])
            dma(SP, wi_sb, wiscr[tile_i], extra=wr_gids[tile_i])
            sc3 = score.rearrange("p (c k) -> p c k", k=64)
            ts(DVE, sc3, chunkid.unsqueeze(2).to_broadcast([128, 64, 64]), qinfo_sb[:, tile_i:tile_i + 1],
               -1e30, ALU.is_gt, ALU.mult)
            it = 0
            for hh in range(16):
                for sc in range(4):
                    pb2 = bank(2 * (it % 2), 2)
                    for half in range(2):
                        mm(pb2[:, half * 512:(half + 1) * 512], qib[:, hh, :],
                           kiT[:, sc * 1024 + half * 512: sc * 1024 + (half + 1) * 512], True, True)
                    rb = rbuf[it % 2]
                    act(rb, pb2, AF.Relu)
                    seg = score[:, sc * 1024:(sc + 1) * 1024]
                    stt(DVE, seg, rb, wi_sb[:, hh:hh + 1], seg, ALU.mult, ALU.add)
                    it += 1
            mid = stats[:, 32:33]
            tcol = stats[:, 33:34]
            memset(DVE, mid, 0.0)
            memset(POOL, cntb, 0.0)
            w = RANGE
            for itb in range(NBIS):
                cnt = cntb[:, itb:itb + 1]
                ts(DVE, maskb, score, mid, 0.0, ALU.is_ge, ALU.add, accum=cnt)
                ts(DVE, tcol, cnt, 256.0, w, ALU.is_ge, ALU.mult)
                stt(DVE, mid, tcol, -0.5 * w, mid, ALU.add, ALU.add)
                w *= 0.5
            ts(DVE, tcol, mid, -w, None, ALU.add)
            ts(DVE, maskb, score, tcol, None, ALU.is_ge)
            for c4 in range(4):
                pb = bankb(4 + c4 % 2 * 2, 2)[:, 0:1024]
                for cc in range(8):
                    ch = c4 * 8 + cc
                    tr(pb[:, cc * 128:(cc + 1) * 128], maskb[:, ch * 128:(ch + 1) * 128], ident)
                cp(ACT if c4 % 2 else DVE, maskT[:, c4 * 8:(c4 + 1) * 8, :], pb.rearrange("p (c q) -> p c q", c=8))
            for g in range(4):
                po = bank(4 + 2 * (g % 2))
                pd = bank(5 + 2 * (g % 2))
                qsl = Qb[:, 4 * g:4 * g + 4, :].rearrange("p h q -> p (h q)")
                for jp in range(16):
                    pst = bank(2 * (jp % 2), 2)
                    for half in range(2):
                        j = jp * 2 + half
                        mm(pst[:, half * 512:(half + 1) * 512], KT[:, g, j * 128:(j + 1) * 128], qsl, True, True)
                    pt = PT[jp % 2]
                    act(pt, pst, AF.Exp, scale=ASCALE)
                    pt4 = pt.rearrange("p (j h q) -> p j h q", j=2, h=4)
                    mk = maskT[:, jp * 2:jp * 2 + 2, :].unsqueeze(2).to_broadcast([128, 2, 4, 128])
                    tt(DVE, pt4, pt4, mk, ALU.mult)
                    for half in range(2):
                        j = jp * 2 + half
                        mm(po, V[:, j, g * 128:(g + 1) * 128], pt[:, half * 512:(half + 1) * 512], j == 0, j == 31)
                        mm(pd, ones_bf, pt[:, half * 512:(half + 1) * 512], j == 0, j == 31)
                recip(rcp, pd)
                tt(DVE, attnT[:, 4 * g:4 * g + 4, tile_i * 128:(tile_i + 1) * 128],
                   po.rearrange("p (h q) -> p h q", h=4), rcp.rearrange("p (h q) -> p h q", h=4), ALU.mult)
        A.release(m_ws)

        A.release(m_kv)
        h = A.alloc([NT, D], F32)
        assert A.mark() == m_attn
        A.release(m_ws)
        for ti in range(NT):
            dma(SP, h[:, ti, :], xo[ti * 128:(ti + 1) * 128, :])
        bi = 0
        for n in range(8):
            pA = load_panel(a_w_out, 0, 8, n * 256)
            pB = load_panel(a_w_out, 1024, 8, n * 256)
            for ti in range(NT):
                pk = bank(bi % 8)[:, 0:256]
                bi += 1
                for kc in range(KC):
                    pan = pA if kc < 8 else pB
                    mm(pk, attnT[:, kc, ti * 128:(ti + 1) * 128], pan[:, kc % 8, :], kc == 0, kc == KC - 1)
                hs = h[:, ti, n * 256:(n + 1) * 256]
                tt(DVE, hs, pk, hs, ALU.add)
        A.release(m_attn)

        def dump(tag):
            if dbg == tag:
                gl = []
                for ti in range(NT):
                    gl.append(dma(SP, dbg_out[ti * 128:(ti + 1) * 128, :], h[:, ti, :]))
                P.wait_for(SP, gl)

        dump("mix0")

        def mlp_ple(layer, tiles, p_dram, p_row0):
            mk0 = A.mark()
            ntl = len(tiles)
            T = ntl * 128
            hT = A.alloc([KC, T], BF16)
            aT = A.alloc([8, T], BF16)
            rtmp = [A.alloc([512], BF16) for _ in range(2)]
            for k, ti in enumerate(tiles):
                norm_T(h[:, ti, :], hT, k * 128, 1 + 2 * layer, 16 * (k % 2))
            gw = 384 if T % 384 == 0 else 512
            ngr = T // gw
            w1 = mlp_w1[layer]
            w2 = mlp_w2[layer]
            bsel = 0
            ri = 0
            for fb in range(8):
                for pp in range(4):
                    c0 = fb * 1024 + pp * 256
                    pA = load_panel(w1, 0, 8, c0)
                    pB = load_panel(w1, 1024, 8, c0)
                    for gi in range(ngr):
                        base = 2 * (bsel % 4)
                        bsel += 1
                        for n in range(2):
                            pk = bank(base + n)
                            for kc in range(KC):
                                pan = pA if kc < 8 else pB
                                mm(pk[:, 0:gw], pan[:, kc % 8, n * 128:(n + 1) * 128], hT[:, kc, gi * gw:(gi + 1) * gw],
                                   kc == 0, kc == KC - 1)
                        for n in range(2):
                            pk = bank(base + n)
                            rt = rtmp[ri % 2]
                            ri += 1
                            act(rt[:, 0:gw], pk[:, 0:gw], AF.Relu)
                            act(aT[:, pp * 2 + n, gi * gw:(gi + 1) * gw], rt[:, 0:gw], AF.Square)
                for n in range(8):
                    pW = load_panel(w2, fb * 1024, 8, n * 256)
                    for k, ti in enumerate(tiles):
                        pk = bank(bsel % 8)[:, 0:256]
                        bsel += 1
                        for fc in range(8):
                            mm(pk, aT[:, fc, k * 128:(k + 1) * 128], pW[:, fc, :], fc == 0, fc == 7)
                        hs = h[:, ti, n * 256:(n + 1) * 256]
                        tt(DVE, hs, pk, hs, ALU.add)
            dump(f"mlp{layer}")
            for k, ti in enumerate(tiles):
                norm_T(h[:, ti, :], hT, k * 128, None, 0)
            pT = aT
            pld = [A.alloc([256], F32) for _ in range(2)]
            plb = [A.alloc([256], BF16) for _ in range(2)]
            sg = [A.alloc([256], F32) for _ in range(2)]
            for k, ti in enumerate(tiles):
                dma(SP, pld[k % 2], p_dram[p_row0 + k * 128:p_row0 + (k + 1) * 128, :])
                cp(ACT, plb[k % 2], pld[k % 2])
                pb = bankb(4 + 2 * (k % 2), 1)[:, 0:256]
                for kc in range(2):
                    tr(pb[:, kc * 128:(kc + 1) * 128], plb[k % 2][:, kc * 128:(kc + 1) * 128], ident)
                cp(DVE, pT[:, 0:2, k * 128:(k + 1) * 128], pb.rearrange("p (kc c) -> p kc c", kc=2))
            wg = ple_wg[layer]
            wp = ple_wp[layer]
            for n in range(8):
                pA = load_panel(wg, 0, 8, n * 256)
                pB = load_panel(wg, 1024, 8, n * 256)
                pP = load_panel(wp, 0, 2, n * 256)
                for k, ti in enumerate(tiles):
                    pg = bank(2 * (k % 2))[:, 0:256]
                    pe = bank(2 * (k % 2) + 1)[:, 0:256]
                    for kc in range(KC):
                        pan = pA if kc < 8 else pB
                        mm(pg, hT[:, kc, k * 128:(k + 1) * 128], pan[:, kc % 8, :], kc == 0, kc == KC - 1)
                    for kc in range(2):
                        mm(pe, pT[:, kc, k * 128:(k + 1) * 128], pP[:, kc, :], kc == 0, kc == 1)
                    s_ = sg[k % 2]
                    act(s_, pg, AF.Sigmoid)
                    tt(DVE, s_, s_, pe, ALU.mult)
                    hs = h[:, ti, n * 256:(n + 1) * 256]
                    tt(POOL, hs, hs, s_, ALU.add)
            A.release(mk0)

        mlp_ple(0, list(range(NT)), p0, 0)
        dump("l0")

        mk1 = A.mark()
        cb = A.alloc([8, KC], F32)
        dma(SP, cb[:, 0, :], b_pw1[0:1, 0:D].rearrange("o (kc p) -> p (o kc)", p=128))
        dma(SP, cb[:, 1, :], b_pw1[0:1, D:2 * D].rearrange("o (kc p) -> p (o kc)", p=128))
        dma(SP, cb[:, 2, :], b_dw.rearrange("o (kc p) -> p (o kc)", p=128))
        dma(SP, cb[:, 3, :], ln_g.rearrange("o (kc p) -> p (o kc)", p=128))
        dma(SP, cb[:, 4, :], ln_b.rearrange("o (kc p) -> p (o kc)", p=128))
        wdw = A.alloc([KC, 31], F32)
        for c in range(KC):
            dma(SP, wdw[:, c, :], w_dw[:, c * 128:(c + 1) * 128].rearrange("j p -> p j"))
        hTh = A.alloc([KC, 640], BF16)
        b2row = A.alloc([D], BF16)
        dma(POOL, b2row[0:1, :], b_pw2)
        vbuf = A.alloc([KC, 512], F32)
        ubuf = [A.alloc([640], BF16) for _ in range(2)]
        sgb = [A.alloc([320], F32) for _ in range(2)]
        sqb = [A.alloc([512], F32)] * 2
        dgb = [A.alloc([16, 128], BF16) for _ in range(2)]
        dgi = 0
        lnm = A.alloc([3, 512], F32)
        for half in (1, 0):
            tiles = [half * 4 + k for k in range(5)]
            for k, ti in enumerate(tiles):
                norm_T(h[:, ti, :], hTh, k * 128, 2, 16 * (k % 2))
            for cq in range(8):
                pans = []
                for part in range(2):
                    pans.append((load_panel(w_pw1, 0, 8, part * D + cq * 256),
                                 load_panel(w_pw1, 1024, 8, part * D + cq * 256)))
                for cc in range(2):
                    c = cq * 2 + cc
                    ub = ubuf[c % 2]
                    for gi in range(2):
                        pa = bank(2 * gi)
                        pg = bank(2 * gi + 1)
                        for part, pk in ((0, pa), (1, pg)):
                            pA, pB = pans[part]
                            for kc in range(KC):
                                pan = pA if kc < 8 else pB
                                mm(pk[:, 0:320], pan[:, kc % 8, cc * 128:(cc + 1) * 128], hTh[:, kc, gi * 320:(gi + 1) * 320],
                                   kc == 0, kc == KC - 1)
                        s_ = sgb[gi]
                        act(s_, pg[:, 0:320], AF.Sigmoid, bias=cb[:, 1, c:c + 1])
                        stt(DVE, ub[:, gi * 320:(gi + 1) * 320], pa[:, 0:320], cb[:, 0, c:c + 1], s_, ALU.add, ALU.mult)
                    if half == 0:
                        ts(DVE, ub[:, 0:128], ub[:, 0:128], hflag_sb[:, 0:1], None, ALU.mult)
                    pacc = bank(4 + c % 2)
                    for (j0, nj) in ((0, 16), (16, 15)):
                        dg = dgb[dgi % 2]
                        dgi += 1
                        tt(POOL, dg[:, 0:nj, :], ident.unsqueeze(1).to_broadcast([128, nj, 128]),
                           wdw[:, c, j0:j0 + nj].unsqueeze(2).to_broadcast([128, nj, 128]), ALU.mult)
                        for jj in range(nj):
                            j = j0 + jj
                            mm(pacc, dg[:, jj, :], ub[:, 98 + j:98 + j + 512], j == 0, j == 30)
                    act(vbuf[:, c, :], pacc, AF.Identity, bias=cb[:, 2, c:c + 1])
            pm = bank(4)
            pq = bank(5)
            for c in range(KC):
                mm(pm, ones_d, vbuf[:, c, :], c == 0, c == KC - 1)
            for c in range(KC):
                sq = sqb[c % 2]
                act(sq, vbuf[:, c, :], AF.Square)
                mm(pq, ones_d, sq, c == 0, c == KC - 1)
            cp(ACT, lnm[:, 0, :], pm)
            tt(DVE, lnm[:, 1, :], lnm[:, 0, :], lnm[:, 0, :], ALU.mult)
            tt(DVE, lnm[:, 1, :], pq, lnm[:, 1, :], ALU.subtract)
            ts(DVE, lnm[:, 1, :], lnm[:, 1, :], 1e-5, None, ALU.add)
            recip(lnm[:, 2, :], lnm[:, 1, :])
            act(lnm[:, 1, :], lnm[:, 2, :], AF.Sqrt)
            zT = hTh
            for c in range(KC):
                sq = sqb[c % 2]
                eng = DVE if c % 2 == 0 else POOL
                tt(eng, sq, vbuf[:, c, :], lnm[:, 0, :], ALU.subtract)
                tt(eng, sq, sq, lnm[:, 1, :], ALU.mult)
                act(zT[:, c, 0:512], sq, AF.Silu, bias=cb[:, 4, c:c + 1], scale=cb[:, 3, c:c + 1])
            bi = 0
            for n in range(8):
                pA = load_panel(w_pw2, 0, 8, n * 256)
                pB = load_panel(w_pw2, 1024, 8, n * 256)
                for k in range(4):
                    ti = 1 + half * 4 + k
                    pk = bank(bi % 4)[:, 0:256]
                    bi += 1
                    for kc in range(KC):
                        pan = pA if kc < 8 else pB
                        mm(pk, zT[:, kc, k * 128:(k + 1) * 128], pan[:, kc % 8, :], kc == 0, False)
                    mm(pk, ones_bf[0:1, :], b2row[0:1, n * 256:(n + 1) * 256], False, True)
                    hs = h[:, ti, n * 256:(n + 1) * 256]
                    tt(DVE, hs, pk, hs, ALU.add)
        A.release(mk1)
        dump("mix1")

        mlp_ple(1, list(range(1, NT)), p1, 0)
        dump("l1")

        gbc = A.alloc([D], F32)
        dma(SP, gbc, g_fin[0].partition_broadcast(128))
        ob = [A.alloc([D], F32) for _ in range(2)]
        outs = []
        for k in range(8):
            ti = 1 + k
            o = ob[k % 2]
            c0 = 48 + 4 * (k % 2)
            memset(DVE, stats[:, c0:c0 + 1], 0.0)
            act(o, h[:, ti, :], AF.Square, accum=stats[:, c0:c0 + 1])
            ts(DVE, stats[:, c0 + 1:c0 + 2], stats[:, c0:c0 + 1], 1.0 / D, 1e-6, ALU.mult, ALU.add)
            recip(stats[:, c0 + 2:c0 + 3], stats[:, c0 + 1:c0 + 2])
            act(stats[:, c0 + 3:c0 + 4], stats[:, c0 + 2:c0 + 3], AF.Sqrt)
            stt(DVE, o, h[:, ti, :], stats[:, c0 + 3:c0 + 4], gbc, ALU.mult, ALU.mult)
            outs.append(dma(SP, y[k * 128:(k + 1) * 128, :], o))
        P.wait_for(SP, outs)
        P.emit()
        build_program.stats = (len(P.ops), P.nwaits)
    return nc


def _consts():
    d = np.arange(128)
    cst = np.zeros((128, 16), np.float32)
    inv128 = (10000.0 ** (-(d % 64).astype(np.float32) * np.float32(2.0 / 128))).astype(np.float32)
    inv64 = (10000.0 ** (-(d % 32).astype(np.float32) * np.float32(2.0 / 64))).astype(np.float32)
    inv64[64:] = 0.0
    cst[:, 0] = inv128
    cst[:, 1] = inv64
    cst[:, 2] = math.pi / 2
    cst[:, 3] = np.where(d < 64, math.pi, 0.0)
    cst[:, 4] = math.pi / 2
    cst[:, 5] = np.where(d < 32, math.pi, 0.0)
    ident = np.eye(128, dtype=np.float32)
    psw128 = np.zeros((128, 128), np.float32)
    for m in range(128):
        psw128[(m + 64) % 128, m] = 1.0
    psw64 = np.zeros((128, 128), np.float32)
    for m in range(64):
        psw64[(m + 32) % 64, m] = 1.0
    chunkid = np.tile(np.arange(64, dtype=np.float32)[None, :], (128, 1))
    return cst, ident, psw128, psw64, chunkid


_CACHE = {}


def make_in_maps(inputs):
    x = np.ascontiguousarray(inputs["x"], dtype=np.float32)
    p = np.ascontiguousarray(inputs["p"], dtype=np.float32)
    pos = np.ascontiguousarray(inputs["positions"], dtype=np.int32)
    cst, ident, psw128, psw64, chunkid = _consts()
    shared = {
        "cst": cst, "ident": ident, "psw128": psw128, "psw64": psw64, "chunkid": chunkid,
        "norm_mix_g": np.ascontiguousarray(inputs["norm_mix_g"], np.float32),
        "norm_mlp_g": np.ascontiguousarray(inputs["norm_mlp_g"], np.float32),
        "final_g": np.ascontiguousarray(inputs["final_g"], np.float32).reshape(1, D),
        "a_w_in": np.ascontiguousarray(inputs["a_w_in"][0], np.float32),
        "a_w_out": np.ascontiguousarray(inputs["a_w_out"][0], np.float32),
        "a_kidx_g": np.ascontiguousarray(inputs["a_kidx_g"][0], np.float32).reshape(128, 1),
        "a_kidx_b": np.ascontiguousarray(inputs["a_kidx_b"][0], np.float32).reshape(128, 1),
        "b_w_pw1": np.ascontiguousarray(inputs["b_w_pw1"][0], np.float32),
        "b_b_pw1": np.ascontiguousarray(inputs["b_b_pw1"][0], np.float32).reshape(1, 2 * D),
        "b_w_dw": np.ascontiguousarray(inputs["b_w_dw"][0], np.float32),
        "b_b_dw": np.ascontiguousarray(inputs["b_b_dw"][0], np.float32).reshape(1, D),
        "b_ln_g": np.ascontiguousarray(inputs["b_ln_g"][0], np.float32).reshape(1, D),
        "b_ln_b": np.ascontiguousarray(inputs["b_ln_b"][0], np.float32).reshape(1, D),
        "b_w_pw2": np.ascontiguousarray(inputs["b_w_pw2"][0], np.float32),
        "b_b_pw2": np.ascontiguousarray(inputs["b_b_pw2"][0], np.float32).reshape(1, D),
        "mlp_w1": np.ascontiguousarray(inputs["mlp_w1"], np.float32),
        "mlp_w2": np.ascontiguousarray(inputs["mlp_w2"], np.float32),
        "ple_w_proj": np.ascontiguousarray(inputs["ple_w_proj"], np.float32),
        "ple_w_gate": np.ascontiguousarray(inputs["ple_w_gate"], np.float32),
    }
    maps = []
    for c in range(8):
        b = c // 4
        t0 = (c % 4) * 1024
        first = (c % 4) == 0
        rows = np.arange(t0 - 128, t0 + 1024)
        if first:
            rows[:128] = np.arange(0, 128)
        m = dict(shared)
        m["xs"] = x[b]
        m["xo"] = np.ascontiguousarray(x[b][rows])
        m["pos_s"] = pos[b].reshape(1, S)
        m["pos_o"] = np.ascontiguousarray(pos[b][rows]).reshape(1, T0)
        m["p0"] = np.ascontiguousarray(p[0, b][rows])
        m["p1"] = np.ascontiguousarray(p[1, b][t0:t0 + 1024])
        m["qinfo"] = np.ascontiguousarray((rows // 64).astype(np.float32).reshape(NT, 128).T)
        m["hflag"] = np.full((128, 1), 0.0 if first else 1.0, np.float32)
        maps.append(m)
    return maps


def kernel(**inputs):
    if "nc" not in _CACHE:
        _CACHE["nc"] = build_program()
    nc = _CACHE["nc"]
    maps = make_in_maps(inputs)
    res = run_bass_kernel_spmd(nc, maps, core_ids=list(range(8)))
    out = np.empty((2, S, D), np.float32)
    for c in range(8):
        b = c // 4
        t0 = (c % 4) * 1024
        out[b, t0:t0 + 1024] = res.results[c]["y"]
    return out
```

```python
import math
import numpy as np
from contextlib import ExitStack
import concourse.bass as bass
import concourse.mybir as mybir
from concourse.bass_utils import run_bass_kernel_spmd

F32 = mybir.dt.float32
BF16 = mybir.dt.bfloat16
I32 = mybir.dt.int32
AF = mybir.ActivationFunctionType
ALU = mybir.AluOpType
AX = mybir.AxisListType

PE, ACT, DVE, POOL, SP = range(5)
NENG = 5
DSIZE = {F32: 4, BF16: 2, I32: 4}

D = 2048
S = 4096
NT = 9
T0 = NT * 128
KC = 16
DFF = 8192
IN_A = 5264
SB_BYTES = 176 * 1024
TWO_PI = 2.0 * math.pi


class Op:
    __slots__ = ("gid", "eng", "fn", "deps", "is_dma", "signal", "sem", "val")


class Prog:
    PAGE = 64
    NDMA_SEMS = 32

    def __init__(self, nc, sb_bytes, ps_bytes=16384):
        self.nc = nc
        self.ops = []
        self.np_sb = sb_bytes // self.PAGE
        self.np_ps = ps_bytes // self.PAGE
        self.lw = {"sb": np.full(self.np_sb, -1, np.int64), "ps": np.full(self.np_ps, -1, np.int64)}
        self.rd = {
            "sb": np.full((NENG + 1, self.np_sb), -1, np.int64),
            "ps": np.full((NENG + 1, self.np_ps), -1, np.int64),
        }
        self.bases = {}
        self.bank_last = np.full((8, NENG), -1, np.int64)

    def rng(self, ap):
        name = ap.tensor.name
        if name not in self.bases:
            return None
        space, base = self.bases[name]
        pstep = ap.ap[0][0]
        ds = DSIZE[ap.dtype]
        off = ap.offset % pstep if pstep > 0 else ap.offset
        ext = 1
        for st, cnt in ap.ap[1:]:
            ext += abs(st) * (cnt - 1)
        lo = base + off * ds
        hi = lo + ext * ds
        return space, lo // self.PAGE, (hi + self.PAGE - 1) // self.PAGE

    def add(self, eng, fn, reads=(), writes=(), dma=False, extra=()):
        op = Op()
        op.gid = len(self.ops)
        op.eng = eng
        op.fn = fn
        op.is_dma = dma
        op.signal = dma
        op.sem = None
        op.val = 0
        deps = set()
        ridx = NENG if dma else eng
        rr_ = [self.rng(a) for a in reads]
        ww_ = [self.rng(a) for a in writes]
        for r in rr_:
            if r is None:
                continue
            sp, a, b = r
            w = self.lw[sp][a:b]
            deps.update(np.unique(w[w >= 0]).tolist())
        for r in ww_:
            if r is None:
                continue
            sp, a, b = r
            w = self.lw[sp][a:b]
            deps.update(np.unique(w[w >= 0]).tolist())
            rr = self.rd[sp][:, a:b]
            for e in range(NENG + 1):
                if e == eng and not dma and e != NENG:
                    continue
                x = rr[e]
                deps.update(np.unique(x[x >= 0]).tolist())
        for r in rr_:
            if r is None:
                continue
            sp, a, b = r
            self.rd[sp][ridx, a:b] = op.gid
        for r in ww_:
            if r is None:
                continue
            sp, a, b = r
            self.lw[sp][a:b] = op.gid
            self.rd[sp][:, a:b] = -1
        if not dma:
            bpp = 2048 // self.PAGE
            for r in rr_ + ww_:
                if r is None or r[0] != "ps":
                    continue
                for bk in range(r[1] // bpp, (r[2] - 1) // bpp + 1):
                    for e in range(NENG):
                        if e != eng and self.bank_last[bk, e] >= 0:
                            deps.add(int(self.bank_last[bk, e]))
                    self.bank_last[bk, eng] = op.gid
        deps.update(extra)
        deps.discard(op.gid)
        best = {}
        out = set()
        for d in deps:
            o = self.ops[d]
            if o.is_dma:
                out.add(d)
            else:
                if o.eng == PE and eng == PE and not dma:
                    continue
                if o.eng not in best or best[o.eng] < d:
                    best[o.eng] = d
        out.update(best.values())
        op.deps = out
        self.ops.append(op)
        return op.gid

    def wait_for(self, eng, gids):
        op = Op()
        op.gid = len(self.ops)
        op.eng = eng
        op.fn = None
        op.is_dma = False
        op.signal = False
        op.sem = None
        op.val = 0
        op.deps = set(gids)
        self.ops.append(op)
        return op.gid

    def emit(self):
        nc = self.nc
        with ExitStack() as es:
            esems = [es.enter_context(nc.semaphore(f"e{i}")) for i in range(NENG)]
            dsems = [es.enter_context(nc.semaphore(f"d{i}")) for i in range(self.NDMA_SEMS)]
            dcount = [0] * self.NDMA_SEMS
            dlast = [None] * self.NDMA_SEMS
            k = 0
            for op in self.ops:
                if op.is_dma:
                    s = k % self.NDMA_SEMS
                    k += 1
                    if dlast[s] is not None:
                        op.deps.add(dlast[s])
                    dcount[s] += 1
                    op.sem = dsems[s]
                    op.val = 16 * dcount[s]
                    dlast[s] = op.gid
            for op in self.ops:
                for d in op.deps:
                    self.ops[d].signal = True
            ecount = [0] * NENG
            for op in self.ops:
                if op.is_dma or op.fn is None:
                    continue
                if op.signal:
                    ecount[op.eng] += 1
                    op.sem = esems[op.eng]
                    op.val = ecount[op.eng]
            per = [[] for _ in range(NENG)]
            for op in self.ops:
                per[op.eng].append(op)
            ops = self.ops
            self.nwaits = 0

            def run(eng_idx, e):
                waited = {}
                for op in per[eng_idx]:
                    for d in sorted(op.deps):
                        o = ops[d]
                        key = id(o.sem)
                        if waited.get(key, 0) < o.val:
                            e.wait_ge(o.sem, o.val)
                            waited[key] = o.val
                            self.nwaits += 1
                    if op.fn is None:
                        continue
                    ins = op.fn(e)
                    if op.signal:
                        ins.then_inc(op.sem, 16 if op.is_dma else 1)

            with nc.Block() as block:
                @block.tensor
                def _(e):
                    run(PE, e)

                @block.scalar
                def _(e):
                    run(ACT, e)

                @block.vector
                def _(e):
                    run(DVE, e)

                @block.gpsimd
                def _(e):
                    run(POOL, e)

                @block.sync
                def _(e):
                    run(SP, e)


class Arena:
    def __init__(self, ar):
        self.ar = ar
        self.top = 0

    def mark(self):
        return self.top

    def release(self, m):
        self.top = m

    def alloc(self, shape, dt):
        n = 1
        for s in shape:
            n *= s
        nb = (n * DSIZE[dt] + 63) // 64 * 64
        assert self.top + nb <= SB_BYTES, f"SBUF arena overflow {self.top + nb}"
        v = self.ar[:, self.top // 4:(self.top + nb) // 4]
        self.top += nb
        if dt != F32:
            v = v.bitcast(dt)
        v = v[:, :n]
        if len(shape) == 2:
            v = v.rearrange("p (a b) -> p a b", a=shape[0])
        elif len(shape) == 3:
            v = v.rearrange("p (a b c) -> p a b c", a=shape[0], b=shape[1])
        return v


class _Stop(Exception):
    pass


def build_program(dbg=None, stop=None):
    try:
        return _build_program(dbg, stop)
    except _Stop as e:
        return e.args[0]


def _build_program(dbg=None, stop=None):
    nc = bass.Bass("TRN2", target_bir_lowering=False)
    dram = {}

    def din(name, shape, dt=F32):
        dram[name] = nc.dram_tensor(name, list(shape), dt, kind="ExternalInput").ap()
        return dram[name]

    xs = din("xs", [S, D])
    xo = din("xo", [T0, D])
    pos_s = din("pos_s", [1, S], I32)
    pos_o = din("pos_o", [1, T0], I32)
    p0 = din("p0", [T0, 256])
    p1 = din("p1", [1024, 256])
    qinfo = din("qinfo", [128, NT])
    hflag = din("hflag", [128, 1])
    cst = din("cst", [128, 16])
    ident_d = din("ident", [128, 128])
    psw128_d = din("psw128", [128, 128])
    psw64_d = din("psw64", [128, 128])
    chunkid_d = din("chunkid", [128, 64])
    g_mix = din("norm_mix_g", [2, D])
    g_mlp = din("norm_mlp_g", [2, D])
    g_fin = din("final_g", [1, D])
    a_w_in = din("a_w_in", [D, IN_A])
    a_w_out = din("a_w_out", [D, D])
    kidx_g = din("a_kidx_g", [128, 1])
    kidx_b = din("a_kidx_b", [128, 1])
    w_pw1 = din("b_w_pw1", [D, 2 * D])
    b_pw1 = din("b_b_pw1", [1, 2 * D])
    w_dw = din("b_w_dw", [31, D])
    b_dw = din("b_b_dw", [1, D])
    ln_g = din("b_ln_g", [1, D])
    ln_b = din("b_ln_b", [1, D])
    w_pw2 = din("b_w_pw2", [D, D])
    b_pw2 = din("b_b_pw2", [1, D])
    mlp_w1 = din("mlp_w1", [2, D, DFF])
    mlp_w2 = din("mlp_w2", [2, DFF, D])
    ple_wp = din("ple_w_proj", [2, 256, D])
    ple_wg = din("ple_w_gate", [2, D, D])
    y = nc.dram_tensor("y", [1024, D], F32, kind="ExternalOutput").ap()
    dbg_out = None
    if dbg:
        dbg_out = nc.dram_tensor("dbg", [T0, D], F32, kind="ExternalOutput").ap()

    with ExitStack() as es:
        ar = es.enter_context(nc.sbuf_tensor("arena", [128, SB_BYTES // 4], F32))
        ps = es.enter_context(nc.psum_tensor("ps", [128, 4096], F32))
        P = Prog(nc, SB_BYTES)
        P.bases["arena"] = ("sb", 0)
        P.bases["ps"] = ("ps", 0)
        A = Arena(ar)
        es.enter_context(nc.allow_non_contiguous_dma(reason="small per-partition constant loads"))

        def bank(i, n=1):
            return ps[:, i * 512:(i + n) * 512]

        def bankb(i, n=1):
            return ps[:, i * 512:(i + n) * 512].bitcast(BF16)

        def mm(out, lhsT, rhs, start, stop):
            P.add(PE, lambda e: e.matmul(out, lhsT=lhsT, rhs=rhs, start=start, stop=stop),
                  reads=[lhsT, rhs], writes=[out])

        def tr(out, in_, ident):
            P.add(PE, lambda e: e.transpose(out, in_, ident), reads=[in_, ident], writes=[out])

        def act(out, in_, func, bias=None, scale=None, accum=None, eng=ACT):
            kw = {}
            rd = [in_]
            wr = [out]
            if bias is not None:
                kw["bias"] = bias
                if not isinstance(bias, float):
                    rd.append(bias)
            if scale is not None:
                kw["scale"] = scale
                if not isinstance(scale, float):
                    rd.append(scale)
            if accum is not None:
                kw["accum_out"] = accum
                wr.append(accum)
            P.add(ACT, lambda e: e.activation(out=out, in_=in_, func=func, **kw), reads=rd, writes=wr)

        def ts(eng, out, in0, s1, s2, op0, op1=None, accum=None):
            rd = [in0]
            wr = [out]
            if not isinstance(s1, float):
                rd.append(s1)
            if s2 is not None and not isinstance(s2, float):
                rd.append(s2)
            kw = {}
            if op1 is not None:
                kw["op1"] = op1
            if accum is not None:
                kw["accum_out"] = accum
                wr.append(accum)
            P.add(eng, lambda e: e.tensor_scalar(out=out, in0=in0, scalar1=s1, scalar2=s2, op0=op0, **kw),
                  reads=rd, writes=wr)

        def tt(eng, out, in0, in1, op):
            P.add(eng, lambda e: e.tensor_tensor(out=out, in0=in0, in1=in1, op=op), reads=[in0, in1], writes=[out])

        def stt(eng, out, in0, sc, in1, op0, op1):
            rd = [in0, in1]
            if not isinstance(sc, float):
                rd.append(sc)
            P.add(eng, lambda e: e.scalar_tensor_tensor(out=out, in0=in0, scalar=sc, in1=in1, op0=op0, op1=op1),
                  reads=rd, writes=[out])

        def cp(eng, out, in_):
            if eng == ACT:
                act(out, in_, AF.Copy)
            else:
                P.add(eng, lambda e: e.tensor_copy(out=out, in_=in_), reads=[in_], writes=[out])

        def recip(out, in_):
            P.add(DVE, lambda e: e.reciprocal(out=out, in_=in_), reads=[in_], writes=[out])

        def memset(eng, out, v):
            P.add(eng, lambda e: e.memset(out, v), writes=[out])

        def dma(eng, out, in_, extra=()):
            return P.add(eng, lambda e: e.dma_start(out=out, in_=in_), reads=[in_], writes=[out], dma=True, extra=extra)

        def early(tag, views):
            if stop != tag:
                return
            gl = []
            for i, v in enumerate(views):
                ncol = v.shape[-1]
                gl.append(dma(SP, y[i * 128:(i + 1) * 128, 0:ncol], v))
            P.wait_for(SP, gl)
            P.emit()
            build_program.stats = (len(P.ops), P.nwaits)
            raise _Stop(nc)

        ident = A.alloc([128], BF16)
        psw128 = A.alloc([128], BF16)
        psw64 = A.alloc([128], BF16)
        ones_bf = A.alloc([128], BF16)
        ones_f = A.alloc([128], F32)
        ones_d = A.alloc([128], F32)
        cst_sb = A.alloc([16], F32)
        qinfo_sb = A.alloc([NT], F32)
        hflag_sb = A.alloc([1], F32)
        chunkid = A.alloc([64], F32)
        gfm = A.alloc([4, KC], F32)
        kig = A.alloc([2], F32)
        stats = A.alloc([64], F32)
        dma(POOL, ident, ident_d)
        dma(POOL, psw128, psw128_d)
        dma(POOL, psw64, psw64_d)
        dma(SP, cst_sb, cst)
        dma(SP, qinfo_sb, qinfo)
        dma(SP, hflag_sb, hflag)
        dma(SP, chunkid, chunkid_d)
        dma(SP, kig[:, 0:1], kidx_g)
        dma(SP, kig[:, 1:2], kidx_b)
        for i, gsrc in enumerate([g_mix[0:1, :], g_mlp[0:1, :], g_mix[1:2, :], g_mlp[1:2, :]]):
            dma(SP, gfm[:, i, :], gsrc.rearrange("o (kc p) -> p (o kc)", p=128))
        memset(DVE, ones_bf, 1.0)
        memset(DVE, ones_f, 1.0 / 128.0)
        memset(DVE, ones_d, 1.0 / D)

        early('consts', [cst_sb, chunkid, gfm.rearrange('p a b -> p (a b)'), ident.bitcast(F32)])
        NPB = 4
        panels = [A.alloc([8, 256], BF16) for _ in range(NPB)]
        pstate = {"i": 0}

        def load_panel(w2d, r0, nk, c0, ncols=256):
            buf = panels[pstate["i"] % NPB]
            pstate["i"] += 1
            src = w2d[r0:r0 + nk * 128, c0:c0 + ncols].rearrange("(kc p) n -> p kc n", p=128)
            dma(POOL, buf[:, 0:nk, 0:ncols], src)
            return buf

        hn_tm = [A.alloc([D], BF16)] * 2
        nstate = {"i": 0}

        def norm_T(src, dstT, col0, gidx, st_col):
            i = nstate["i"]
            nstate["i"] += 1
            hb = hn_tm[i % 2]
            if gidx is not None:
                ss = stats[:, st_col:st_col + 1]
                memset(DVE, ss, 0.0)
                act(hb, src, AF.Square, accum=ss)
                ts(DVE, stats[:, st_col + 1:st_col + 2], ss, 1.0 / D, 1e-6, ALU.mult, ALU.add)
                recip(stats[:, st_col + 2:st_col + 3], stats[:, st_col + 1:st_col + 2])
                act(stats[:, st_col + 3:st_col + 4], stats[:, st_col + 2:st_col + 3], AF.Sqrt)
                ts(DVE, hb, src, stats[:, st_col + 3:st_col + 4], None, ALU.mult)
            else:
                cp(ACT, hb, src)
            pb = bankb(4 + 2 * (i % 2), 2)
            for kc in range(KC):
                tr(pb[:, kc * 128:(kc + 1) * 128], hb[:, kc * 128:(kc + 1) * 128], ident)
            if gidx is not None:
                for kc in range(KC):
                    o = dstT[:, kc, col0:col0 + 128]
                    i_ = pb[:, kc * 128:(kc + 1) * 128]
                    if kc < 8:
                        act(o, i_, AF.Copy, scale=gfm[:, gidx, kc:kc + 1])
                    else:
                        ts(DVE, o, i_, gfm[:, gidx, kc:kc + 1], None, ALU.mult)
            else:
                o = dstT[:, :, col0:col0 + 128]
                i_ = pb.rearrange("p (kc c) -> p kc c", kc=KC)
                cp(DVE if i % 2 else ACT, o, i_)

        def rope_tables(posb_i, n, tabs, tf):
            posf = tabs[:, 4, :]
            cp(DVE, posf, posb_i)
            x = tf[:, 0, 0:n]
            kf = tf[:, 1, 0:n]
            for (ti, inv_c, ph_c) in ((0, 0, 2), (1, 0, 3), (2, 1, 4), (3, 1, 5)):
                ts(DVE, x, posf, cst_sb[:, inv_c:inv_c + 1], cst_sb[:, ph_c:ph_c + 1], ALU.mult, ALU.add)
                ts(DVE, posb_i, x, 1.0 / TWO_PI, None, ALU.mult)
                cp(DVE, kf, posb_i)
                stt(DVE, x, kf, -TWO_PI, x, ALU.mult, ALU.add)
                ts(DVE, kf, x, math.pi, -TWO_PI, ALU.is_gt, ALU.mult)
                tt(DVE, x, x, kf, ALU.add)
                ts(DVE, x, x, math.pi, -math.pi, ALU.min, ALU.max)
                act(tabs[:, ti, :], x, AF.Sin)

        rstate = {"i": 0}

        def rope_apply(dst, src_ps, n, Ct, St, psw, tmpbs, tmpfs, pbanks):
            i = rstate["i"]
            rstate["i"] += 1
            tmpb = tmpbs[i % len(tmpbs)]
            tmpf = tmpfs[i % len(tmpfs)]
            pbank = pbanks[i % len(pbanks)]
            cp(ACT, tmpb[:, 0:n], src_ps)
            mm(pbank[:, 0:n], psw, tmpb[:, 0:n], True, True)
            tt(POOL, tmpf[:, 0, 0:n], tmpb[:, 0:n], Ct, ALU.mult)
            tt(DVE, tmpf[:, 1, 0:n], pbank[:, 0:n], St, ALU.mult)
            tt(POOL, dst, tmpf[:, 0, 0:n], tmpf[:, 1, 0:n], ALU.add)

        qscr = nc.dram_tensor("qscr", [NT, 128, 2048], BF16).ap()
        qiscr = nc.dram_tensor("qiscr", [NT, 128, 2048], BF16).ap()
        wiscr = nc.dram_tensor("wiscr", [NT, 128, 16], F32).ap()
        m_kv = A.mark()
        KT = A.alloc([4, S], BF16)
        V = A.alloc([32, 512], BF16)
        kiT = A.alloc([S], BF16)
        m_attn = A.mark()
        assert m_attn - m_kv == NT * D * 4
        wkv = A.alloc([KC, 1152], BF16)
        for kb in range(2):
            dma(POOL, wkv[:, kb * 8:(kb + 1) * 8, 0:1024],
                a_w_in[kb * 1024:(kb + 1) * 1024, 2048:3072].rearrange("(kc p) n -> p kc n", p=128))
            dma(POOL, wkv[:, kb * 8:(kb + 1) * 8, 1024:1152],
                a_w_in[kb * 1024:(kb + 1) * 1024, 5120:5248].rearrange("(kc p) n -> p kc n", p=128))
        early('a1_w', [wkv[:, 0, :].bitcast(F32), wkv[:, 15, :].bitcast(F32)])
        G1 = 256
        xt = [A.alloc([D], F32)] * 2
        hnT2 = [A.alloc([KC, G1], BF16) for _ in range(2)]
        tabs2 = [A.alloc([5, G1], F32) for _ in range(2)]
        posb2 = [A.alloc([G1], I32) for _ in range(2)]
        tmpbs = [A.alloc([G1], BF16) for _ in range(2)]
        tmpfs = [A.alloc([2, G1], F32) for _ in range(2)]
        lnb = A.alloc([3, G1], F32)
        for sg in range(S // G1):
            s0 = sg * G1
            hnT = hnT2[sg % 2]
            tabs = tabs2[sg % 2]
            posb = posb2[sg % 2]
            tmpf = tmpfs[sg % 2]
            dma(SP, posb, pos_s[0, s0:s0 + G1].partition_broadcast(128))
            rope_tables(posb, G1, tabs, tmpf)
            if sg == 0:
                early('a1_tab', [tabs.rearrange('p a b -> p (a b)')])
            for ti in range(2):
                xb = xt[ti % 2]
                dma(SP, xb, xs[s0 + ti * 128:s0 + (ti + 1) * 128, :])
                norm_T(xb, hnT, ti * 128, 0, 16 * (ti % 2))
            if sg == 0:
                early('a1_n', [hnT[:, 0:8, :].rearrange('p a b -> p (a b)').bitcast(F32), hnT[:, 8:16, :].rearrange('p a b -> p (a b)').bitcast(F32)])
            for g in range(4):
                pk = bank(g % 2)[:, 0:G1]
                for kc in range(KC):
                    mm(pk, wkv[:, kc, g * 128:(g + 1) * 128], hnT[:, kc, :], kc == 0, kc == KC - 1)
                rope_apply(KT[:, g, s0:s0 + G1], pk, G1, tabs[:, 0, :], tabs[:, 1, :], psw128, tmpbs, tmpfs, [bank(2), bank(4)])
            for ti in range(2):
                pv = bank(ti % 2)
                for kc in range(KC):
                    mm(pv, hnT[:, kc, ti * 128:(ti + 1) * 128], wkv[:, kc, 512:1024], kc == 0, kc == KC - 1)
                cp(ACT if ti % 2 else DVE, V[:, sg * 2 + ti, :], pv)
            pk = bank(3)[:, 0:G1]
            pm_ = bank(5)[:, 0:G1]
            for kc in range(KC):
                mm(pk, wkv[:, kc, 1024:1152], hnT[:, kc, :], kc == 0, kc == KC - 1)
            cp(ACT, lnb[:, 0, :], pk)
            mm(pm_, ones_f, lnb[:, 0, :], True, True)
            tt(DVE, lnb[:, 1, :], lnb[:, 0, :], pm_, ALU.subtract)
            act(lnb[:, 2, :], lnb[:, 1, :], AF.Square)
            mm(pm_, ones_f, lnb[:, 2, :], True, True)
            ts(DVE, lnb[:, 0, :], pm_, 1e-5, None, ALU.add)
            recip(lnb[:, 2, :], lnb[:, 0, :])
            act(lnb[:, 0, :], lnb[:, 2, :], AF.Sqrt)
            tt(DVE, lnb[:, 1, :], lnb[:, 1, :], lnb[:, 0, :], ALU.mult)
            ts(DVE, lnb[:, 2, :], lnb[:, 1, :], kig[:, 0:1], kig[:, 1:2], ALU.mult, ALU.add)
            rope_apply(kiT[:, s0:s0 + G1], lnb[:, 2, :], G1, tabs[:, 2, :], tabs[:, 3, :], psw64, tmpbs, tmpfs, [bank(2), bank(4)])
            if sg == 0:
                early('a1_g0', [KT[:, 0, 0:512].bitcast(F32), KT[:, 3, 0:512].bitcast(F32), kiT[:, 0:512].bitcast(F32), V[:, 0, :].bitcast(F32), V[:, 1, :].bitcast(F32)])
        early('a1', [KT[:, 0, :].bitcast(F32), KT[:, 3, :].bitcast(F32), kiT.bitcast(F32), V[:, 0:8, :].rearrange('p a b -> p (a b)').bitcast(F32), V[:, 24:32, :].rearrange('p a b -> p (a b)').bitcast(F32)])
        A.release(m_attn)

        attnT = A.alloc([KC, T0], BF16)
        m_ws = A.mark()
        hnTg = A.alloc([KC, 384], BF16)
        tabs_o = A.alloc([5, 384], F32)
        posb_o = A.alloc([384], I32)
        tmpb_os = [A.alloc([384], BF16) for _ in range(2)]
        tmpf_os = [A.alloc([2, 384], F32) for _ in range(2)]
        tmpf_o = tmpf_os[0]
        stage = [A.alloc([384], BF16) for _ in range(2)]
        wwi = A.alloc([KC, 16], BF16)
        wist = A.alloc([3, 16], F32)
        xt2 = [A.alloc([D], F32)] * 2
        for kb in range(2):
            dma(POOL, wwi[:, kb * 8:(kb + 1) * 8, :],
                a_w_in[kb * 1024:(kb + 1) * 1024, 5248:5264].rearrange("(kc p) n -> p kc n", p=128))
        WSCALE = (16 ** -0.5) * (128 ** -0.5)
        ASCALE = 128 ** -0.5
        NBIS = 26
        RANGE = 64.0
        wr_gids = [[] for _ in range(NT)]
        sti = 0
        for grp in range(3):
            t0g = grp * 384
            dma(SP, posb_o, pos_o[0, t0g:t0g + 384].partition_broadcast(128))
            rope_tables(posb_o, 384, tabs_o, tmpf_o)
            for ti in range(3):
                xb = xt2[ti % 2]
                dma(SP, xb, xo[t0g + ti * 128:t0g + (ti + 1) * 128, :])
                norm_T(xb, hnTg, ti * 128, 0, 16 * (ti % 2))
            for (scr, c_base, Ct, St, psw) in ((qscr, 0, 0, 1, psw128), (qiscr, 3072, 2, 3, psw64)):
                for hp in range(8):
                    pA = load_panel(a_w_in, 0, 8, c_base + hp * 256)
                    pB = load_panel(a_w_in, 1024, 8, c_base + hp * 256)
                    for n in range(2):
                        pk = bank(2 * (hp % 2) + n)
                        for kc in range(KC):
                            pan = pA if kc < 8 else pB
                            mm(pk[:, 0:384], pan[:, kc % 8, n * 128:(n + 1) * 128], hnTg[:, kc, :], kc == 0, kc == KC - 1)
                    for n in range(2):
                        hd = hp * 2 + n
                        st_ = stage[sti % 2]
                        sti += 1
                        rope_apply(st_, bank(2 * (hp % 2) + n)[:, 0:384], 384, tabs_o[:, Ct, :], tabs_o[:, St, :],
                                   psw, tmpb_os, tmpf_os, [bank(6), bank(7)])
                        gd = dma(SP, scr[grp * 3:(grp + 1) * 3, :, hd * 128:(hd + 1) * 128].rearrange("t p q -> p t q"),
                                 st_.rearrange("p (t q) -> p t q", t=3))
                        for k3 in range(3):
                            wr_gids[grp * 3 + k3].append(gd)
            for ti in range(3):
                pw = bank(ti % 2)
                for kc in range(KC):
                    mm(pw[:, 0:16], hnTg[:, kc, ti * 128:(ti + 1) * 128], wwi[:, kc, :], kc == 0, kc == KC - 1)
                ts(DVE, wist[:, ti, :], pw[:, 0:16], WSCALE, None, ALU.mult)
                gd = dma(SP, wiscr[grp * 3 + ti], wist[:, ti, :])
                wr_gids[grp * 3 + ti].append(gd)
        early('a2', [tabs_o.rearrange('p a b -> p (a b)'), hnTg[:, 0:4, :].rearrange('p a b -> p (a b)').bitcast(F32)])
        A.release(m_ws)

        Qb = A.alloc([16, 128], BF16)
        qib = A.alloc([16, 128], BF16)
        wi_sb = A.alloc([16], F32)
        cntb = A.alloc([32], F32)
        maskT = A.alloc([32, 128], BF16)
        score = A.alloc([S], F32)
        rbreg = A.alloc([2048], F32)
        rbuf = [rbreg[:, 0:1024], rbreg[:, 1024:2048]]
        maskb = rbreg.bitcast(BF16)
        PT = [score[:, k * 512:(k + 1) * 512].bitcast(BF16) for k in range(3)]
        rcp = score[:, 1536:2048]
        sgnb = A.alloc([32], F32)
        zcol = stats[:, 35:36]
        for tile_i in range(NT):
            nch = 24 + tile_i
            NK = nch * 128
            dma(SP, Qb.rearrange("p h q -> p (h q)"), qscr[tile_i], extra=wr_gids[tile_i])
            dma(SP, qib.rearrange("p h q -> p (h q)"), qiscr[tile_i], extra=wr_gids[tile_i])
            dma(SP, wi_sb, wiscr[tile_i], extra=wr_gids[tile_i])
            sc3 = score[:, 0:NK].rearrange("p (c k) -> p c k", k=64)
            ts(DVE, sc3, chunkid[:, 0:2 * nch].unsqueeze(2).to_broadcast([128, 2 * nch, 64]),
               qinfo_sb[:, tile_i:tile_i + 1], -1e30, ALU.is_gt, ALU.mult)
            it = 0
            for hh in range(16):
                for sc in range((NK + 1023) // 1024):
                    wd = min(1024, NK - sc * 1024)
                    pb2 = bank(2 * (it % 2), 2)
                    for sub in range(0, wd, 512):
                        w_ = min(512, wd - sub)
                        mm(pb2[:, sub:sub + w_], qib[:, hh, :], kiT[:, sc * 1024 + sub: sc * 1024 + sub + w_], True, True)
                    rb = rbuf[it % 2]
                    act(rb[:, 0:wd], pb2[:, 0:wd], AF.Relu)
                    seg = score[:, sc * 1024:sc * 1024 + wd]
                    stt(DVE, seg, rb[:, 0:wd], wi_sb[:, hh:hh + 1], seg, ALU.mult, ALU.add)
                    it += 1
            mid = stats[:, 32:33]
            tcol = stats[:, 33:34]
            nd = ((NK // 2) // 128) * 128
            n_act = NK - nd
            memset(DVE, mid, 0.0)
            memset(POOL, cntb, 0.0)
            memset(POOL, sgnb, 0.0)
            w = RANGE
            for itb in range(NBIS):
                cnt = cntb[:, itb:itb + 1]
                sgc = sgnb[:, itb:itb + 1]
                ts(DVE, maskb[:, 0:nd], score[:, 0:nd], mid, 0.0, ALU.is_ge, ALU.add, accum=cnt)
                act(maskb[:, nd:NK], score[:, nd:NK], AF.Sign, bias=mid, scale=-1.0, accum=sgc)
                stt(DVE, zcol, cnt, 2.0, sgc, ALU.mult, ALU.subtract)
                ts(DVE, tcol, zcol, 512.0 - n_act, w, ALU.is_ge, ALU.mult)
                stt(DVE, mid, tcol, -0.5 * w, mid, ALU.add, ALU.add)
                w *= 0.5
            ts(DVE, tcol, mid, -w, None, ALU.add)
            ts(DVE, maskb[:, 0:NK], score[:, 0:NK], tcol, None, ALU.is_ge)
            for c4 in range((nch + 7) // 8):
                ncc = min(8, nch - c4 * 8)
                pb = bankb(4 + c4 % 2 * 2, 2)[:, 0:ncc * 128]
                for cc in range(ncc):
                    ch = c4 * 8 + cc
                    tr(pb[:, cc * 128:(cc + 1) * 128], maskb[:, ch * 128:(ch + 1) * 128], ident)
                cp(ACT if c4 % 2 else DVE, maskT[:, c4 * 8:c4 * 8 + ncc, :], pb.rearrange("p (c q) -> p c q", c=ncc))
            njp = (nch + 1) // 2
            for g in range(4):
                po = bank(6)
                pd = bank(7)
                qsl = Qb[:, 4 * g:4 * g + 4, :].rearrange("p h q -> p (h q)")

                def produce(jp, g=g, qsl=qsl):
                    nh = min(2, nch - 2 * jp)
                    pst = bank(2 * (jp % 3), 2)
                    for half in range(nh):
                        j = jp * 2 + half
                        mm(pst[:, half * 512:(half + 1) * 512], KT[:, g, j * 128:(j + 1) * 128], qsl, True, True)
                    pt = PT[jp % 3]
                    act(pt[:, 0:nh * 512], pst[:, 0:nh * 512], AF.Exp, scale=ASCALE)
                    pt4 = pt[:, 0:nh * 512].rearrange("p (j h q) -> p j h q", j=nh, h=4)
                    mk = maskT[:, jp * 2:jp * 2 + nh, :].unsqueeze(2).to_broadcast([128, nh, 4, 128])
                    tt(DVE, pt4, pt4, mk, ALU.mult)

                def consume(jp, g=g, po=po, pd=pd):
                    nh = min(2, nch - 2 * jp)
                    pt = PT[jp % 3]
                    for half in range(nh):
                        j = jp * 2 + half
                        mm(po, V[:, j, g * 128:(g + 1) * 128], pt[:, half * 512:(half + 1) * 512], j == 0, j == nch - 1)
                        mm(pd, ones_bf, pt[:, half * 512:(half + 1) * 512], j == 0, j == nch - 1)

                produce(0)
                produce(1)
                for jp in range(njp):
                    if jp + 2 < njp:
                        produce(jp + 2)
                    consume(jp)
                recip(rcp, pd)
                tt(DVE, attnT[:, 4 * g:4 * g + 4, tile_i * 128:(tile_i + 1) * 128],
                   po.rearrange("p (h q) -> p h q", h=4), rcp.rearrange("p (h q) -> p h q", h=4), ALU.mult)
        A.release(m_ws)

        A.release(m_kv)
        h = A.alloc([NT, D], F32)
        assert A.mark() == m_attn
        A.release(m_ws)
        for ti in range(NT):
            dma(SP, h[:, ti, :], xo[ti * 128:(ti + 1) * 128, :])
        bi = 0
        for n in range(8):
            pA = load_panel(a_w_out, 0, 8, n * 256)
            pB = load_panel(a_w_out, 1024, 8, n * 256)
            for ti in range(NT):
                pk = bank(bi % 8)[:, 0:256]
                bi += 1
                for kc in range(KC):
                    pan = pA if kc < 8 else pB
                    mm(pk, attnT[:, kc, ti * 128:(ti + 1) * 128], pan[:, kc % 8, :], kc == 0, kc == KC - 1)
                hs = h[:, ti, n * 256:(n + 1) * 256]
                tt(DVE, hs, pk, hs, ALU.add)
        A.release(m_attn)

        def dump(tag):
            if dbg == tag:
                gl = []
                for ti in range(NT):
                    gl.append(dma(SP, dbg_out[ti * 128:(ti + 1) * 128, :], h[:, ti, :]))
                P.wait_for(SP, gl)

        dump("mix0")

        def mlp_ple(layer, tiles, p_dram, p_row0):
            mk0 = A.mark()
            ntl = len(tiles)
            T = ntl * 128
            hT = A.alloc([KC, T], BF16)
            aT = A.alloc([8, T], BF16)
            rtmp = [A.alloc([512], BF16) for _ in range(2)]
            for k, ti in enumerate(tiles):
                norm_T(h[:, ti, :], hT, k * 128, 1 + 2 * layer, 16 * (k % 2))
            gw = 384 if T % 384 == 0 else 512
            ngr = T // gw
            w1 = mlp_w1[layer]
            w2 = mlp_w2[layer]
            bsel = 0
            ri = 0
            for fb in range(8):
                for pp in range(4):
                    c0 = fb * 1024 + pp * 256
                    pA = load_panel(w1, 0, 8, c0)
                    pB = load_panel(w1, 1024, 8, c0)
                    for gi in range(ngr):
                        base = 2 * (bsel % 4)
                        bsel += 1
                        for n in range(2):
                            pk = bank(base + n)
                            for kc in range(KC):
                                pan = pA if kc < 8 else pB
                                mm(pk[:, 0:gw], pan[:, kc % 8, n * 128:(n + 1) * 128], hT[:, kc, gi * gw:(gi + 1) * gw],
                                   kc == 0, kc == KC - 1)
                        for n in range(2):
                            pk = bank(base + n)
                            rt = rtmp[ri % 2]
                            ri += 1
                            act(rt[:, 0:gw], pk[:, 0:gw], AF.Relu)
                            act(aT[:, pp * 2 + n, gi * gw:(gi + 1) * gw], rt[:, 0:gw], AF.Square)
                for n in range(8):
                    pW = load_panel(w2, fb * 1024, 8, n * 256)
                    for k, ti in enumerate(tiles):
                        pk = bank(bsel % 8)[:, 0:256]
                        bsel += 1
                        for fc in range(8):
                            mm(pk, aT[:, fc, k * 128:(k + 1) * 128], pW[:, fc, :], fc == 0, fc == 7)
                        hs = h[:, ti, n * 256:(n + 1) * 256]
                        tt(DVE, hs, pk, hs, ALU.add)
            dump(f"mlp{layer}")
            for k, ti in enumerate(tiles):
                norm_T(h[:, ti, :], hT, k * 128, None, 0)
            pT = aT
            pld = [A.alloc([256], F32) for _ in range(2)]
            plb = [A.alloc([256], BF16) for _ in range(2)]
            sg = [A.alloc([256], F32) for _ in range(2)]
            for k, ti in enumerate(tiles):
                dma(SP, pld[k % 2], p_dram[p_row0 + k * 128:p_row0 + (k + 1) * 128, :])
                cp(ACT, plb[k % 2], pld[k % 2])
                pb = bankb(4 + 2 * (k % 2), 1)[:, 0:256]
                for kc in range(2):
                    tr(pb[:, kc * 128:(kc + 1) * 128], plb[k % 2][:, kc * 128:(kc + 1) * 128], ident)
                cp(DVE, pT[:, 0:2, k * 128:(k + 1) * 128], pb.rearrange("p (kc c) -> p kc c", kc=2))
            wg = ple_wg[layer]
            wp = ple_wp[layer]
            for n in range(8):
                pA = load_panel(wg, 0, 8, n * 256)
                pB = load_panel(wg, 1024, 8, n * 256)
                pP = load_panel(wp, 0, 2, n * 256)
                for k, ti in enumerate(tiles):
                    pg = bank(2 * (k % 2))[:, 0:256]
                    pe = bank(2 * (k % 2) + 1)[:, 0:256]
                    for kc in range(KC):
                        pan = pA if kc < 8 else pB
                        mm(pg, hT[:, kc, k * 128:(k + 1) * 128], pan[:, kc % 8, :], kc == 0, kc == KC - 1)
                    for kc in range(2):
                        mm(pe, pT[:, kc, k * 128:(k + 1) * 128], pP[:, kc, :], kc == 0, kc == 1)
                    s_ = sg[k % 2]
                    act(s_, pg, AF.Sigmoid)
                    tt(DVE, s_, s_, pe, ALU.mult)
                    hs = h[:, ti, n * 256:(n + 1) * 256]
                    tt(POOL, hs, hs, s_, ALU.add)
            A.release(mk0)

        mlp_ple(0, list(range(NT)), p0, 0)
        dump("l0")

        mk1 = A.mark()
        cb = A.alloc([8, KC], F32)
        dma(SP, cb[:, 0, :], b_pw1[0:1, 0:D].rearrange("o (kc p) -> p (o kc)", p=128))
        dma(SP, cb[:, 1, :], b_pw1[0:1, D:2 * D].rearrange("o (kc p) -> p (o kc)", p=128))
        dma(SP, cb[:, 2, :], b_dw.rearrange("o (kc p) -> p (o kc)", p=128))
        dma(SP, cb[:, 3, :], ln_g.rearrange("o (kc p) -> p (o kc)", p=128))
        dma(SP, cb[:, 4, :], ln_b.rearrange("o (kc p) -> p (o kc)", p=128))
        wdw = A.alloc([KC, 31], F32)
        for c in range(KC):
            dma(SP, wdw[:, c, :], w_dw[:, c * 128:(c + 1) * 128].rearrange("j p -> p j"))
        hTh = A.alloc([KC, 640], BF16)
        b2row = A.alloc([D], BF16)
        dma(POOL, b2row[0:1, :], b_pw2)
        vbuf = A.alloc([KC, 512], F32)
        ubuf = [A.alloc([640], BF16) for _ in range(2)]
        sgb = [A.alloc([320], F32) for _ in range(2)]
        sqb = [A.alloc([512], F32)] * 2
        dgb = [A.alloc([16, 128], BF16) for _ in range(2)]
        dgi = 0
        lnm = A.alloc([3, 512], F32)
        for half in (1, 0):
            tiles = [half * 4 + k for k in range(5)]
            for k, ti in enumerate(tiles):
                norm_T(h[:, ti, :], hTh, k * 128, 2, 16 * (k % 2))
            for cq in range(8):
                pans = []
                for part in range(2):
                    pans.append((load_panel(w_pw1, 0, 8, part * D + cq * 256),
                                 load_panel(w_pw1, 1024, 8, part * D + cq * 256)))
                for cc in range(2):
                    c = cq * 2 + cc
                    ub = ubuf[c % 2]
                    for gi in range(2):
                        pa = bank(2 * gi)
                        pg = bank(2 * gi + 1)
                        for part, pk in ((0, pa), (1, pg)):
                            pA, pB = pans[part]
                            for kc in range(KC):
                                pan = pA if kc < 8 else pB
                                mm(pk[:, 0:320], pan[:, kc % 8, cc * 128:(cc + 1) * 128], hTh[:, kc, gi * 320:(gi + 1) * 320],
                                   kc == 0, kc == KC - 1)
                        s_ = sgb[gi]
                        act(s_, pg[:, 0:320], AF.Sigmoid, bias=cb[:, 1, c:c + 1])
                        stt(DVE, ub[:, gi * 320:(gi + 1) * 320], pa[:, 0:320], cb[:, 0, c:c + 1], s_, ALU.add, ALU.mult)
                    if half == 0:
                        ts(DVE, ub[:, 0:128], ub[:, 0:128], hflag_sb[:, 0:1], None, ALU.mult)
                    pacc = bank(4 + c % 2)
                    for (j0, nj) in ((0, 16), (16, 15)):
                        dg = dgb[dgi % 2]
                        dgi += 1
                        tt(POOL, dg[:, 0:nj, :], ident.unsqueeze(1).to_broadcast([128, nj, 128]),
                           wdw[:, c, j0:j0 + nj].unsqueeze(2).to_broadcast([128, nj, 128]), ALU.mult)
                        for jj in range(nj):
                            j = j0 + jj
                            mm(pacc, dg[:, jj, :], ub[:, 98 + j:98 + j + 512], j == 0, j == 30)
                    act(vbuf[:, c, :], pacc, AF.Identity, bias=cb[:, 2, c:c + 1])
            pm = bank(4)
            pq = bank(5)
            for c in range(KC):
                mm(pm, ones_d, vbuf[:, c, :], c == 0, c == KC - 1)
            for c in range(KC):
                sq = sqb[c % 2]
                act(sq, vbuf[:, c, :], AF.Square)
                mm(pq, ones_d, sq, c == 0, c == KC - 1)
            cp(ACT, lnm[:, 0, :], pm)
            tt(DVE, lnm[:, 1, :], lnm[:, 0, :], lnm[:, 0, :], ALU.mult)
            tt(DVE, lnm[:, 1, :], pq, lnm[:, 1, :], ALU.subtract)
            ts(DVE, lnm[:, 1, :], lnm[:, 1, :], 1e-5, None, ALU.add)
            recip(lnm[:, 2, :], lnm[:, 1, :])
            act(lnm[:, 1, :], lnm[:, 2, :], AF.Sqrt)
            zT = hTh
            for c in range(KC):
                sq = sqb[c % 2]
                eng = DVE if c % 2 == 0 else POOL
                tt(eng, sq, vbuf[:, c, :], lnm[:, 0, :], ALU.subtract)
                tt(eng, sq, sq, lnm[:, 1, :], ALU.mult)
                act(zT[:, c, 0:512], sq, AF.Silu, bias=cb[:, 4, c:c + 1], scale=cb[:, 3, c:c + 1])
            bi = 0
            for n in range(8):
                pA = load_panel(w_pw2, 0, 8, n * 256)
                pB = load_panel(w_pw2, 1024, 8, n * 256)
                for k in range(4):
                    ti = 1 + half * 4 + k
                    pk = bank(bi % 4)[:, 0:256]
                    bi += 1
                    for kc in range(KC):
                        pan = pA if kc < 8 else pB
                        mm(pk, zT[:, kc, k * 128:(k + 1) * 128], pan[:, kc % 8, :], kc == 0, False)
                    mm(pk, ones_bf[0:1, :], b2row[0:1, n * 256:(n + 1) * 256], False, True)
                    hs = h[:, ti, n * 256:(n + 1) * 256]
                    tt(DVE, hs, pk, hs, ALU.add)
        A.release(mk1)
        dump("mix1")

        mlp_ple(1, list(range(1, NT)), p1, 0)
        dump("l1")

        gbc = A.alloc([D], F32)
        dma(SP, gbc, g_fin[0].partition_broadcast(128))
        ob = [A.alloc([D], F32) for _ in range(2)]
        outs = []
        for k in range(8):
            ti = 1 + k
            o = ob[k % 2]
            c0 = 48 + 4 * (k % 2)
            memset(DVE, stats[:, c0:c0 + 1], 0.0)
            act(o, h[:, ti, :], AF.Square, accum=stats[:, c0:c0 + 1])
            ts(DVE, stats[:, c0 + 1:c0 + 2], stats[:, c0:c0 + 1], 1.0 / D, 1e-6, ALU.mult, ALU.add)
            recip(stats[:, c0 + 2:c0 + 3], stats[:, c0 + 1:c0 + 2])
            act(stats[:, c0 + 3:c0 + 4], stats[:, c0 + 2:c0 + 3], AF.Sqrt)
            stt(DVE, o, h[:, ti, :], stats[:, c0 + 3:c0 + 4], gbc, ALU.mult, ALU.mult)
            outs.append(dma(SP, y[k * 128:(k + 1) * 128, :], o))
        P.wait_for(SP, outs)
        P.emit()
        build_program.stats = (len(P.ops), P.nwaits)
    return nc


def _consts():
    d = np.arange(128)
    cst = np.zeros((128, 16), np.float32)
    inv128 = (10000.0 ** (-(d % 64).astype(np.float32) * np.float32(2.0 / 128))).astype(np.float32)
    inv64 = (10000.0 ** (-(d % 32).astype(np.float32) * np.float32(2.0 / 64))).astype(np.float32)
    inv64[64:] = 0.0
    cst[:, 0] = inv128
    cst[:, 1] = inv64
    cst[:, 2] = math.pi / 2
    cst[:, 3] = np.where(d < 64, math.pi, 0.0)
    cst[:, 4] = math.pi / 2
    cst[:, 5] = np.where(d < 32, math.pi, 0.0)
    ident = np.eye(128, dtype=np.float32)
    psw128 = np.zeros((128, 128), np.float32)
    for m in range(128):
        psw128[(m + 64) % 128, m] = 1.0
    psw64 = np.zeros((128, 128), np.float32)
    for m in range(64):
        psw64[(m + 32) % 64, m] = 1.0
    chunkid = np.tile(np.arange(64, dtype=np.float32)[None, :], (128, 1))
    return cst, ident, psw128, psw64, chunkid


_CACHE = {}


def make_in_maps(inputs):
    x = np.ascontiguousarray(inputs["x"], dtype=np.float32)
    p = np.ascontiguousarray(inputs["p"], dtype=np.float32)
    pos = np.ascontiguousarray(inputs["positions"], dtype=np.int32)
    cst, ident, psw128, psw64, chunkid = _consts()
    shared = {
        "cst": cst, "ident": ident, "psw128": psw128, "psw64": psw64, "chunkid": chunkid,
        "norm_mix_g": np.ascontiguousarray(inputs["norm_mix_g"], np.float32),
        "norm_mlp_g": np.ascontiguousarray(inputs["norm_mlp_g"], np.float32),
        "final_g": np.ascontiguousarray(inputs["final_g"], np.float32).reshape(1, D),
        "a_w_in": np.ascontiguousarray(inputs["a_w_in"][0], np.float32),
        "a_w_out": np.ascontiguousarray(inputs["a_w_out"][0], np.float32),
        "a_kidx_g": np.ascontiguousarray(inputs["a_kidx_g"][0], np.float32).reshape(128, 1),
        "a_kidx_b": np.ascontiguousarray(inputs["a_kidx_b"][0], np.float32).reshape(128, 1),
        "b_w_pw1": np.ascontiguousarray(inputs["b_w_pw1"][0], np.float32),
        "b_b_pw1": np.ascontiguousarray(inputs["b_b_pw1"][0], np.float32).reshape(1, 2 * D),
        "b_w_dw": np.ascontiguousarray(inputs["b_w_dw"][0], np.float32),
        "b_b_dw": np.ascontiguousarray(inputs["b_b_dw"][0], np.float32).reshape(1, D),
        "b_ln_g": np.ascontiguousarray(inputs["b_ln_g"][0], np.float32).reshape(1, D),
        "b_ln_b": np.ascontiguousarray(inputs["b_ln_b"][0], np.float32).reshape(1, D),
        "b_w_pw2": np.ascontiguousarray(inputs["b_w_pw2"][0], np.float32),
        "b_b_pw2": np.ascontiguousarray(inputs["b_b_pw2"][0], np.float32).reshape(1, D),
        "mlp_w1": np.ascontiguousarray(inputs["mlp_w1"], np.float32),
        "mlp_w2": np.ascontiguousarray(inputs["mlp_w2"], np.float32),
        "ple_w_proj": np.ascontiguousarray(inputs["ple_w_proj"], np.float32),
        "ple_w_gate": np.ascontiguousarray(inputs["ple_w_gate"], np.float32),
    }
    maps = []
    for c in range(8):
        b = c // 4
        t0 = (c % 4) * 1024
        first = (c % 4) == 0
        rows = np.arange(t0 - 128, t0 + 1024)
        if first:
            rows[:128] = np.arange(0, 128)
        m = dict(shared)
        m["xs"] = x[b]
        m["xo"] = np.ascontiguousarray(x[b][rows])
        m["pos_s"] = pos[b].reshape(1, S)
        m["pos_o"] = np.ascontiguousarray(pos[b][rows]).reshape(1, T0)
        m["p0"] = np.ascontiguousarray(p[0, b][rows])
        m["p1"] = np.ascontiguousarray(p[1, b][t0:t0 + 1024])
        m["qinfo"] = np.ascontiguousarray((rows // 64).astype(np.float32).reshape(NT, 128).T)
        m["hflag"] = np.full((128, 1), 0.0 if first else 1.0, np.float32)
        maps.append(m)
    return maps


def kernel(**inputs):
    if "nc" not in _CACHE:
        _CACHE["nc"] = build_program()
    nc = _CACHE["nc"]
    maps = make_in_maps(inputs)
    res = run_bass_kernel_spmd(nc, maps, core_ids=list(range(8)))
    out = np.empty((2, S, D), np.float32)
    for c in range(8):
        b = c // 4
        t0 = (c % 4) * 1024
        out[b, t0:t0 + 1024] = res.results[c]["y"]
    return out
```

```python
import math
import numpy as np
from contextlib import ExitStack
import concourse.bass as bass
import concourse.mybir as mybir
from concourse.bass_utils import run_bass_kernel_spmd

F32 = mybir.dt.float32
BF16 = mybir.dt.bfloat16
I32 = mybir.dt.int32
AF = mybir.ActivationFunctionType
ALU = mybir.AluOpType
AX = mybir.AxisListType

PE, ACT, DVE, POOL, SP = range(5)
NENG = 5
DSIZE = {F32: 4, BF16: 2, I32: 4}

D = 2048
S = 4096
NT = 9
T0 = NT * 128
KC = 16
DFF = 8192
IN_A = 5264
SB_BYTES = 176 * 1024
TWO_PI = 2.0 * math.pi


class Op:
    __slots__ = ("gid", "eng", "fn", "deps", "is_dma", "signal", "sem", "val")


class Prog:
    PAGE = 64
    NDMA_SEMS = 32

    def __init__(self, nc, sb_bytes, ps_bytes=16384):
        self.nc = nc
        self.ops = []
        self.np_sb = sb_bytes // self.PAGE
        self.np_ps = ps_bytes // self.PAGE
        self.lw = {"sb": np.full(self.np_sb, -1, np.int64), "ps": np.full(self.np_ps, -1, np.int64)}
        self.rd = {
            "sb": np.full((NENG + 1, self.np_sb), -1, np.int64),
            "ps": np.full((NENG + 1, self.np_ps), -1, np.int64),
        }
        self.bases = {}
        self.bank_last = np.full((8, NENG), -1, np.int64)

    def rng(self, ap):
        name = ap.tensor.name
        if name not in self.bases:
            return None
        space, base = self.bases[name]
        pstep = ap.ap[0][0]
        ds = DSIZE[ap.dtype]
        off = ap.offset % pstep if pstep > 0 else ap.offset
        ext = 1
        for st, cnt in ap.ap[1:]:
            ext += abs(st) * (cnt - 1)
        lo = base + off * ds
        hi = lo + ext * ds
        return space, lo // self.PAGE, (hi + self.PAGE - 1) // self.PAGE

    def add(self, eng, fn, reads=(), writes=(), dma=False, extra=()):
        op = Op()
        op.gid = len(self.ops)
        op.eng = eng
        op.fn = fn
        op.is_dma = dma
        op.signal = dma
        op.sem = None
        op.val = 0
        deps = set()
        ridx = NENG if dma else eng
        rr_ = [self.rng(a) for a in reads]
        ww_ = [self.rng(a) for a in writes]
        for r in rr_:
            if r is None:
                continue
            sp, a, b = r
            w = self.lw[sp][a:b]
            deps.update(np.unique(w[w >= 0]).tolist())
        for r in ww_:
            if r is None:
                continue
            sp, a, b = r
            w = self.lw[sp][a:b]
            deps.update(np.unique(w[w >= 0]).tolist())
            rr = self.rd[sp][:, a:b]
            for e in range(NENG + 1):
                if e == eng and not dma and e != NENG:
                    continue
                x = rr[e]
                deps.update(np.unique(x[x >= 0]).tolist())
        for r in rr_:
            if r is None:
                continue
            sp, a, b = r
            self.rd[sp][ridx, a:b] = op.gid
        for r in ww_:
            if r is None:
                continue
            sp, a, b = r
            self.lw[sp][a:b] = op.gid
            self.rd[sp][:, a:b] = -1
        if not dma:
            bpp = 2048 // self.PAGE
            for r in rr_ + ww_:
                if r is None or r[0] != "ps":
                    continue
                for bk in range(r[1] // bpp, (r[2] - 1) // bpp + 1):
                    for e in range(NENG):
                        if e != eng and self.bank_last[bk, e] >= 0:
                            deps.add(int(self.bank_last[bk, e]))
                    self.bank_last[bk, eng] = op.gid
        deps.update(extra)
        deps.discard(op.gid)
        best = {}
        out = set()
        for d in deps:
            o = self.ops[d]
            if o.is_dma:
                out.add(d)
            else:
                if o.eng == PE and eng == PE and not dma:
                    continue
                if o.eng not in best or best[o.eng] < d:
                    best[o.eng] = d
        out.update(best.values())
        op.deps = out
        self.ops.append(op)
        return op.gid

    def wait_for(self, eng, gids):
        op = Op()
        op.gid = len(self.ops)
        op.eng = eng
        op.fn = None
        op.is_dma = False
        op.signal = False
        op.sem = None
        op.val = 0
        op.deps = set(gids)
        self.ops.append(op)
        return op.gid

    def emit(self):
        nc = self.nc
        with ExitStack() as es:
            esems = [es.enter_context(nc.semaphore(f"e{i}")) for i in range(NENG)]
            dsems = [es.enter_context(nc.semaphore(f"d{i}")) for i in range(self.NDMA_SEMS)]
            dcount = [0] * self.NDMA_SEMS
            dlast = [None] * self.NDMA_SEMS
            k = 0
            for op in self.ops:
                if op.is_dma:
                    s = k % self.NDMA_SEMS
                    k += 1
                    if dlast[s] is not None:
                        op.deps.add(dlast[s])
                    dcount[s] += 1
                    op.sem = dsems[s]
                    op.val = 16 * dcount[s]
                    dlast[s] = op.gid
            for op in self.ops:
                for d in op.deps:
                    self.ops[d].signal = True
            ecount = [0] * NENG
            for op in self.ops:
                if op.is_dma or op.fn is None:
                    continue
                if op.signal:
                    ecount[op.eng] += 1
                    op.sem = esems[op.eng]
                    op.val = ecount[op.eng]
            per = [[] for _ in range(NENG)]
            for op in self.ops:
                per[op.eng].append(op)
            ops = self.ops
            self.nwaits = 0

            def run(eng_idx, e):
                waited = {}
                for op in per[eng_idx]:
                    for d in sorted(op.deps):
                        o = ops[d]
                        key = id(o.sem)
                        if waited.get(key, 0) < o.val:
                            e.wait_ge(o.sem, o.val)
                            waited[key] = o.val
                            self.nwaits += 1
                    if op.fn is None:
                        continue
                    ins = op.fn(e)
                    if op.signal:
                        ins.then_inc(op.sem, 16 if op.is_dma else 1)

            with nc.Block() as block:
                @block.tensor
                def _(e):
                    run(PE, e)

                @block.scalar
                def _(e):
                    run(ACT, e)

                @block.vector
                def _(e):
                    run(DVE, e)

                @block.gpsimd
                def _(e):
                    run(POOL, e)

                @block.sync
                def _(e):
                    run(SP, e)


class Arena:
    def __init__(self, ar):
        self.ar = ar
        self.top = 0

    def mark(self):
        return self.top

    def release(self, m):
        self.top = m

    def alloc(self, shape, dt):
        n = 1
        for s in shape:
            n *= s
        nb = (n * DSIZE[dt] + 63) // 64 * 64
        assert self.top + nb <= SB_BYTES, f"SBUF arena overflow {self.top + nb}"
        v = self.ar[:, self.top // 4:(self.top + nb) // 4]
        self.top += nb
        if dt != F32:
            v = v.bitcast(dt)
        v = v[:, :n]
        if len(shape) == 2:
            v = v.rearrange("p (a b) -> p a b", a=shape[0])
        elif len(shape) == 3:
            v = v.rearrange("p (a b c) -> p a b c", a=shape[0], b=shape[1])
        return v


class _Stop(Exception):
    pass


def build_program(dbg=None, stop=None):
    try:
        return _build_program(dbg, stop)
    except _Stop as e:
        return e.args[0]


def _build_program(dbg=None, stop=None):
    nc = bass.Bass("TRN2", target_bir_lowering=False)
    dram = {}

    def din(name, shape, dt=F32):
        dram[name] = nc.dram_tensor(name, list(shape), dt, kind="ExternalInput").ap()
        return dram[name]

    xs = din("xs", [S, D])
    xo = din("xo", [T0, D])
    pos_s = din("pos_s", [1, S], I32)
    pos_o = din("pos_o", [1, T0], I32)
    p0 = din("p0", [T0, 256])
    p1 = din("p1", [1024, 256])
    qinfo = din("qinfo", [128, NT])
    hflag = din("hflag", [128, 1])
    cst = din("cst", [128, 16])
    ident_d = din("ident", [128, 128])
    psw128_d = din("psw128", [128, 128])
    psw64_d = din("psw64", [128, 128])
    chunkid_d = din("chunkid", [128, 64])
    g_mix = din("norm_mix_g", [2, D])
    g_mlp = din("norm_mlp_g", [2, D])
    g_fin = din("final_g", [1, D])
    a_w_in = din("a_w_in", [D, IN_A])
    a_w_out = din("a_w_out", [D, D])
    kidx_g = din("a_kidx_g", [128, 1])
    kidx_b = din("a_kidx_b", [128, 1])
    w_pw1 = din("b_w_pw1", [D, 2 * D])
    b_pw1 = din("b_b_pw1", [1, 2 * D])
    w_dw = din("b_w_dw", [31, D])
    b_dw = din("b_b_dw", [1, D])
    ln_g = din("b_ln_g", [1, D])
    ln_b = din("b_ln_b", [1, D])
    w_pw2 = din("b_w_pw2", [D, D])
    b_pw2 = din("b_b_pw2", [1, D])
    mlp_w1 = din("mlp_w1", [2, D, DFF])
    mlp_w2 = din("mlp_w2", [2, DFF, D])
    ple_wp = din("ple_w_proj", [2, 256, D])
    ple_wg = din("ple_w_gate", [2, D, D])
    y = nc.dram_tensor("y", [1024, D], F32, kind="ExternalOutput").ap()
    dbg_out = None
    if dbg:
        dbg_out = nc.dram_tensor("dbg", [T0, D], F32, kind="ExternalOutput").ap()

    with ExitStack() as es:
        ar = es.enter_context(nc.sbuf_tensor("arena", [128, SB_BYTES // 4], F32))
        ps = es.enter_context(nc.psum_tensor("ps", [128, 4096], F32))
        P = Prog(nc, SB_BYTES)
        P.bases["arena"] = ("sb", 0)
        P.bases["ps"] = ("ps", 0)
        A = Arena(ar)
        es.enter_context(nc.allow_non_contiguous_dma(reason="small per-partition constant loads"))

        def bank(i, n=1):
            return ps[:, i * 512:(i + n) * 512]

        def bankb(i, n=1):
            return ps[:, i * 512:(i + n) * 512].bitcast(BF16)

        def mm(out, lhsT, rhs, start, stop):
            P.add(PE, lambda e: e.matmul(out, lhsT=lhsT, rhs=rhs, start=start, stop=stop),
                  reads=[lhsT, rhs], writes=[out])

        def tr(out, in_, ident):
            P.add(PE, lambda e: e.transpose(out, in_, ident), reads=[in_, ident], writes=[out])

        def act(out, in_, func, bias=None, scale=None, accum=None, eng=ACT):
            kw = {}
            rd = [in_]
            wr = [out]
            if bias is not None:
                kw["bias"] = bias
                if not isinstance(bias, float):
                    rd.append(bias)
            if scale is not None:
                kw["scale"] = scale
                if not isinstance(scale, float):
                    rd.append(scale)
            if accum is not None:
                kw["accum_out"] = accum
                wr.append(accum)
            P.add(ACT, lambda e: e.activation(out=out, in_=in_, func=func, **kw), reads=rd, writes=wr)

        def ts(eng, out, in0, s1, s2, op0, op1=None, accum=None):
            rd = [in0]
            wr = [out]
            if not isinstance(s1, float):
                rd.append(s1)
            if s2 is not None and not isinstance(s2, float):
                rd.append(s2)
            kw = {}
            if op1 is not None:
                kw["op1"] = op1
            if accum is not None:
                kw["accum_out"] = accum
                wr.append(accum)
            P.add(eng, lambda e: e.tensor_scalar(out=out, in0=in0, scalar1=s1, scalar2=s2, op0=op0, **kw),
                  reads=rd, writes=wr)

        def tt(eng, out, in0, in1, op):
            P.add(eng, lambda e: e.tensor_tensor(out=out, in0=in0, in1=in1, op=op), reads=[in0, in1], writes=[out])

        def stt(eng, out, in0, sc, in1, op0, op1):
            rd = [in0, in1]
            if not isinstance(sc, float):
                rd.append(sc)
            P.add(eng, lambda e: e.scalar_tensor_tensor(out=out, in0=in0, scalar=sc, in1=in1, op0=op0, op1=op1),
                  reads=rd, writes=[out])

        def cp(eng, out, in_):
            if eng == ACT:
                act(out, in_, AF.Copy)
            else:
                P.add(eng, lambda e: e.tensor_copy(out=out, in_=in_), reads=[in_], writes=[out])

        def recip(out, in_):
            P.add(DVE, lambda e: e.reciprocal(out=out, in_=in_), reads=[in_], writes=[out])

        def memset(eng, out, v):
            P.add(eng, lambda e: e.memset(out, v), writes=[out])

        def dma(eng, out, in_, extra=()):
            return P.add(eng, lambda e: e.dma_start(out=out, in_=in_), reads=[in_], writes=[out], dma=True, extra=extra)

        def early(tag, views):
            if stop != tag:
                return
            gl = []
            for i, v in enumerate(views):
                ncol = v.shape[-1]
                gl.append(dma(SP, y[i * 128:(i + 1) * 128, 0:ncol], v))
            P.wait_for(SP, gl)
            P.emit()
            build_program.stats = (len(P.ops), P.nwaits)
            raise _Stop(nc)

        ident = A.alloc([128], BF16)
        psw128 = A.alloc([128], BF16)
        psw64 = A.alloc([128], BF16)
        ones_bf = A.alloc([128], BF16)
        ones_f = A.alloc([128], F32)
        ones_d = A.alloc([128], F32)
        cst_sb = A.alloc([16], F32)
        qinfo_sb = A.alloc([NT], F32)
        hflag_sb = A.alloc([1], F32)
        chunkid = A.alloc([64], F32)
        gfm = A.alloc([4, KC], F32)
        kig = A.alloc([2], F32)
        stats = A.alloc([64], F32)
        dma(POOL, ident, ident_d)
        dma(POOL, psw128, psw128_d)
        dma(POOL, psw64, psw64_d)
        dma(SP, cst_sb, cst)
        dma(SP, qinfo_sb, qinfo)
        dma(SP, hflag_sb, hflag)
        dma(SP, chunkid, chunkid_d)
        dma(SP, kig[:, 0:1], kidx_g)
        dma(SP, kig[:, 1:2], kidx_b)
        for i, gsrc in enumerate([g_mix[0:1, :], g_mlp[0:1, :], g_mix[1:2, :], g_mlp[1:2, :]]):
            dma(SP, gfm[:, i, :], gsrc.rearrange("o (kc p) -> p (o kc)", p=128))
        memset(DVE, ones_bf, 1.0)
        memset(DVE, ones_f, 1.0 / 128.0)
        memset(DVE, ones_d, 1.0 / D)

        early('consts', [cst_sb, chunkid, gfm.rearrange('p a b -> p (a b)'), ident.bitcast(F32)])
        NPB = 4
        panels = [A.alloc([8, 256], BF16) for _ in range(NPB)]
        pstate = {"i": 0}

        def load_panel(w2d, r0, nk, c0, ncols=256):
            buf = panels[pstate["i"] % NPB]
            pstate["i"] += 1
            src = w2d[r0:r0 + nk * 128, c0:c0 + ncols].rearrange("(kc p) n -> p kc n", p=128)
            dma(POOL, buf[:, 0:nk, 0:ncols], src)
            return buf

        hn_tm = [A.alloc([D], BF16)] * 2
        nstate = {"i": 0}

        def norm_T(src, dstT, col0, gidx, st_col):
            i = nstate["i"]
            nstate["i"] += 1
            hb = hn_tm[i % 2]
            if gidx is not None:
                ss = stats[:, st_col:st_col + 1]
                memset(DVE, ss, 0.0)
                act(hb, src, AF.Square, accum=ss)
                ts(DVE, stats[:, st_col + 1:st_col + 2], ss, 1.0 / D, 1e-6, ALU.mult, ALU.add)
                recip(stats[:, st_col + 2:st_col + 3], stats[:, st_col + 1:st_col + 2])
                act(stats[:, st_col + 3:st_col + 4], stats[:, st_col + 2:st_col + 3], AF.Sqrt)
                ts(DVE, hb, src, stats[:, st_col + 3:st_col + 4], None, ALU.mult)
            else:
                cp(ACT, hb, src)
            pb = bankb(4 + 2 * (i % 2), 2)
            for kc in range(KC):
                tr(pb[:, kc * 128:(kc + 1) * 128], hb[:, kc * 128:(kc + 1) * 128], ident)
            if gidx is not None:
                for kc in range(KC):
                    o = dstT[:, kc, col0:col0 + 128]
                    i_ = pb[:, kc * 128:(kc + 1) * 128]
                    if kc < 8:
                        act(o, i_, AF.Copy, scale=gfm[:, gidx, kc:kc + 1])
                    else:
                        ts(DVE, o, i_, gfm[:, gidx, kc:kc + 1], None, ALU.mult)
            else:
                o = dstT[:, :, col0:col0 + 128]
                i_ = pb.rearrange("p (kc c) -> p kc c", kc=KC)
                cp(DVE if i % 2 else ACT, o, i_)

        def rope_tables(posb_i, n, tabs, tf):
            posf = tabs[:, 4, :]
            cp(DVE, posf, posb_i)
            x = tf[:, 0, 0:n]
            kf = tf[:, 1, 0:n]
            for (ti, inv_c, ph_c) in ((0, 0, 2), (1, 0, 3), (2, 1, 4), (3, 1, 5)):
                ts(DVE, x, posf, cst_sb[:, inv_c:inv_c + 1], cst_sb[:, ph_c:ph_c + 1], ALU.mult, ALU.add)
                ts(DVE, posb_i, x, 1.0 / TWO_PI, None, ALU.mult)
                cp(DVE, kf, posb_i)
                stt(DVE, x, kf, -TWO_PI, x, ALU.mult, ALU.add)
                ts(DVE, kf, x, math.pi, -TWO_PI, ALU.is_gt, ALU.mult)
                tt(DVE, x, x, kf, ALU.add)
                ts(DVE, x, x, math.pi, -math.pi, ALU.min, ALU.max)
                act(tabs[:, ti, :], x, AF.Sin)

        rstate = {"i": 0}

        def rope_apply(dst, src_ps, n, Ct, St, psw, tmpbs, tmpfs, pbanks):
            i = rstate["i"]
            rstate["i"] += 1
            tmpb = tmpbs[i % len(tmpbs)]
            tmpf = tmpfs[i % len(tmpfs)]
            pbank = pbanks[i % len(pbanks)]
            cp(ACT, tmpb[:, 0:n], src_ps)
            mm(pbank[:, 0:n], psw, tmpb[:, 0:n], True, True)
            tt(DVE, tmpf[:, 0, 0:n], tmpb[:, 0:n], Ct, ALU.mult)
            tt(DVE, tmpf[:, 1, 0:n], pbank[:, 0:n], St, ALU.mult)
            tt(DVE, dst, tmpf[:, 0, 0:n], tmpf[:, 1, 0:n], ALU.add)

        qscr = nc.dram_tensor("qscr", [NT, 128, 2048], BF16).ap()
        qiscr = nc.dram_tensor("qiscr", [NT, 128, 2048], BF16).ap()
        wiscr = nc.dram_tensor("wiscr", [NT, 128, 16], F32).ap()
        m_kv = A.mark()
        KT = A.alloc([4, S], BF16)
        V = A.alloc([32, 512], BF16)
        kiT = A.alloc([S], BF16)
        m_attn = A.mark()
        assert m_attn - m_kv == NT * D * 4
        wkv = A.alloc([KC, 1152], BF16)
        for kb in range(2):
            dma(POOL, wkv[:, kb * 8:(kb + 1) * 8, 0:1024],
                a_w_in[kb * 1024:(kb + 1) * 1024, 2048:3072].rearrange("(kc p) n -> p kc n", p=128))
            dma(POOL, wkv[:, kb * 8:(kb + 1) * 8, 1024:1152],
                a_w_in[kb * 1024:(kb + 1) * 1024, 5120:5248].rearrange("(kc p) n -> p kc n", p=128))
        G1 = 256
        xt = [A.alloc([D], F32)] * 2
        hnT2 = [A.alloc([KC, G1], BF16) for _ in range(2)]
        tabs2 = [A.alloc([5, G1], F32) for _ in range(2)]
        posb2 = [A.alloc([G1], I32) for _ in range(2)]
        tmpbs = [A.alloc([G1], BF16) for _ in range(2)]
        tmpfs = [A.alloc([2, G1], F32) for _ in range(2)]
        lnb = A.alloc([3, G1], F32)
        def a1_prep(sg):
            s0 = sg * G1
            hnT = hnT2[sg % 2]
            tabs = tabs2[sg % 2]
            posb = posb2[sg % 2]
            dma(SP, posb, pos_s[0, s0:s0 + G1].partition_broadcast(128))
            rope_tables(posb, G1, tabs, tmpfs[sg % 2])
            for ti in range(2):
                xb = xt[ti % 2]
                dma(SP, xb, xs[s0 + ti * 128:s0 + (ti + 1) * 128, :])
                norm_T(xb, hnT, ti * 128, 0, 16 * (ti % 2))

        def a1_compute(sg):
            s0 = sg * G1
            hnT = hnT2[sg % 2]
            tabs = tabs2[sg % 2]
            pks = []
            for g in range(4):
                pk = bank(g)[:, 0:G1]
                for kc in range(KC):
                    mm(pk, wkv[:, kc, g * 128:(g + 1) * 128], hnT[:, kc, :], kc == 0, kc == KC - 1)
                pks.append(pk)
            pki = bank(4)[:, 0:G1]
            for kc in range(KC):
                mm(pki, wkv[:, kc, 1024:1152], hnT[:, kc, :], kc == 0, kc == KC - 1)
            cp(ACT, lnb[:, 0, :], pki)
            pm_ = bank(5)[:, 0:G1]
            mm(pm_, ones_f, lnb[:, 0, :], True, True)
            for ti in range(2):
                pv = bank(6 + ti)
                for kc in range(KC):
                    mm(pv, hnT[:, kc, ti * 128:(ti + 1) * 128], wkv[:, kc, 512:1024], kc == 0, kc == KC - 1)
                cp(ACT if ti % 2 else DVE, V[:, sg * 2 + ti, :], pv)
            tt(DVE, lnb[:, 1, :], lnb[:, 0, :], pm_, ALU.subtract)
            act(lnb[:, 2, :], lnb[:, 1, :], AF.Square)
            mm(pm_, ones_f, lnb[:, 2, :], True, True)
            for g in range(4):
                rope_apply(KT[:, g, s0:s0 + G1], pks[g], G1, tabs[:, 0, :], tabs[:, 1, :], psw128, tmpbs, tmpfs2,
                           [bank(6), bank(7)])
            ts(DVE, lnb[:, 0, :], pm_, 1e-5, None, ALU.add)
            recip(lnb[:, 2, :], lnb[:, 0, :])
            act(lnb[:, 0, :], lnb[:, 2, :], AF.Sqrt)
            tt(DVE, lnb[:, 1, :], lnb[:, 1, :], lnb[:, 0, :], ALU.mult)
            ts(DVE, lnb[:, 2, :], lnb[:, 1, :], kig[:, 0:1], kig[:, 1:2], ALU.mult, ALU.add)
            rope_apply(kiT[:, s0:s0 + G1], lnb[:, 2, :], G1, tabs[:, 2, :], tabs[:, 3, :], psw64, tmpbs, tmpfs2,
                       [bank(6), bank(7)])

        tmpfs2 = tmpfs
        NG1 = S // G1
        a1_prep(0)
        for sg in range(NG1):
            if sg + 1 < NG1:
                a1_prep(sg + 1)
            a1_compute(sg)
        early('a1', [KT[:, 0, :].bitcast(F32), KT[:, 3, :].bitcast(F32), kiT.bitcast(F32), V[:, 0:8, :].rearrange('p a b -> p (a b)').bitcast(F32), V[:, 24:32, :].rearrange('p a b -> p (a b)').bitcast(F32)])
        A.release(m_attn)

        attnT = A.alloc([KC, T0], BF16)
        m_ws = A.mark()
        hnTg = A.alloc([KC, 384], BF16)
        tabs_o = A.alloc([5, 384], F32)
        posb_o = A.alloc([384], I32)
        tmpb_os = [A.alloc([384], BF16) for _ in range(2)]
        tmpf_os = [A.alloc([2, 384], F32) for _ in range(2)]
        tmpf_o = tmpf_os[0]
        stage = [A.alloc([384], BF16) for _ in range(2)]
        wwi = A.alloc([KC, 16], BF16)
        wist = A.alloc([3, 16], F32)
        xt2 = [A.alloc([D], F32)] * 2
        for kb in range(2):
            dma(POOL, wwi[:, kb * 8:(kb + 1) * 8, :],
                a_w_in[kb * 1024:(kb + 1) * 1024, 5248:5264].rearrange("(kc p) n -> p kc n", p=128))
        WSCALE = (16 ** -0.5) * (128 ** -0.5)
        ASCALE = 128 ** -0.5
        NBIS = 26
        RANGE = 64.0
        wr_gids = [[] for _ in range(NT)]
        sti = 0
        for grp in range(3):
            t0g = grp * 384
            dma(SP, posb_o, pos_o[0, t0g:t0g + 384].partition_broadcast(128))
            rope_tables(posb_o, 384, tabs_o, tmpf_o)
            for ti in range(3):
                xb = xt2[ti % 2]
                dma(SP, xb, xo[t0g + ti * 128:t0g + (ti + 1) * 128, :])
                norm_T(xb, hnTg, ti * 128, 0, 16 * (ti % 2))
            jobs = [(scr, c_base, Ct, St, psw, hp)
                    for (scr, c_base, Ct, St, psw) in ((qscr, 0, 0, 1, psw128), (qiscr, 3072, 2, 3, psw64))
                    for hp in range(8)]

            def a2_proj(k):
                scr, c_base, Ct, St, psw, hp = jobs[k]
                pA = load_panel(a_w_in, 0, 8, c_base + hp * 256)
                pB = load_panel(a_w_in, 1024, 8, c_base + hp * 256)
                for n in range(2):
                    pk = bank(2 * (k % 2) + n)
                    for kc in range(KC):
                        pan = pA if kc < 8 else pB
                        mm(pk[:, 0:384], pan[:, kc % 8, n * 128:(n + 1) * 128], hnTg[:, kc, :], kc == 0, kc == KC - 1)

            def a2_rope(k):
                nonlocal sti
                scr, c_base, Ct, St, psw, hp = jobs[k]
                for n in range(2):
                    hd = hp * 2 + n
                    st_ = stage[sti % 2]
                    sti += 1
                    rope_apply(st_, bank(2 * (k % 2) + n)[:, 0:384], 384, tabs_o[:, Ct, :], tabs_o[:, St, :],
                               psw, tmpb_os, tmpf_os, [bank(6), bank(7)])
                    gd = dma(SP, scr[grp * 3:(grp + 1) * 3, :, hd * 128:(hd + 1) * 128].rearrange("t p q -> p t q"),
                             st_.rearrange("p (t q) -> p t q", t=3))
                    for k3 in range(3):
                        wr_gids[grp * 3 + k3].append(gd)

            a2_proj(0)
            for k in range(len(jobs)):
                if k + 1 < len(jobs):
                    a2_proj(k + 1)
                a2_rope(k)
            for ti in range(3):
                pw = bank(ti % 2)
                for kc in range(KC):
                    mm(pw[:, 0:16], hnTg[:, kc, ti * 128:(ti + 1) * 128], wwi[:, kc, :], kc == 0, kc == KC - 1)
                ts(DVE, wist[:, ti, :], pw[:, 0:16], WSCALE, None, ALU.mult)
                gd = dma(SP, wiscr[grp * 3 + ti], wist[:, ti, :])
                wr_gids[grp * 3 + ti].append(gd)
        early('a2', [tabs_o.rearrange('p a b -> p (a b)'), hnTg[:, 0:4, :].rearrange('p a b -> p (a b)').bitcast(F32)])
        A.release(m_ws)

        Qb = A.alloc([16, 128], BF16)
        qib = A.alloc([16, 128], BF16)
        wi_sb = A.alloc([16], F32)
        cntb = A.alloc([32], F32)
        maskT = A.alloc([32, 128], BF16)
        score = A.alloc([S], F32)
        rbreg = A.alloc([2048], F32)
        rbuf = [rbreg[:, k * 256:(k + 1) * 256].bitcast(BF16) for k in range(4)]
        dgw = A.alloc([16, 128], BF16)
        maskb = rbreg.bitcast(BF16)
        PT = [score[:, k * 512:(k + 1) * 512].bitcast(BF16) for k in range(3)]
        rcp = score[:, 1536:2048]
        sgnb = A.alloc([32], F32)
        zcol = stats[:, 35:36]
        for tile_i in range(NT):
            nch = 24 + tile_i
            NK = nch * 128
            dma(SP, Qb.rearrange("p h q -> p (h q)"), qscr[tile_i], extra=wr_gids[tile_i])
            dma(SP, qib.rearrange("p h q -> p (h q)"), qiscr[tile_i], extra=wr_gids[tile_i])
            dma(SP, wi_sb, wiscr[tile_i], extra=wr_gids[tile_i])
            sc3 = score[:, 0:NK].rearrange("p (c k) -> p c k", k=64)
            ts(DVE, sc3, chunkid[:, 0:2 * nch].unsqueeze(2).to_broadcast([128, 2 * nch, 64]),
               qinfo_sb[:, tile_i:tile_i + 1], -1e30, ALU.is_gt, ALU.mult)
            tt(DVE, dgw, ident.unsqueeze(1).to_broadcast([128, 16, 128]),
               wi_sb.unsqueeze(2).to_broadcast([128, 16, 128]), ALU.mult)
            for kb in range((NK + 511) // 512):
                wd = min(512, NK - kb * 512)
                pS = bank(4 + kb % 2)[:, 0:wd]
                kis = kiT[:, kb * 512:kb * 512 + wd]

                def dots(hh, wd=wd, kis=kis):
                    pA = bank(hh % 4)[:, 0:wd]
                    mm(pA, qib[:, hh, :], kis, True, True)
                    act(rbuf[hh % 4][:, 0:wd], pA, AF.Relu)

                def hsum(hh, wd=wd, pS=pS):
                    mm(pS, dgw[:, hh, :], rbuf[hh % 4][:, 0:wd], hh == 0, hh == 15)

                dots(0)
                dots(1)
                for hh in range(16):
                    if hh + 2 < 16:
                        dots(hh + 2)
                    hsum(hh)
                seg = score[:, kb * 512:kb * 512 + wd]
                tt(DVE, seg, pS, seg, ALU.add)
            mid = stats[:, 32:33]
            tcol = stats[:, 33:34]
            nd = ((NK * 5 // 8) // 128) * 128
            n_act = NK - nd
            memset(DVE, mid, 0.0)
            memset(DVE, cntb, 0.0)
            memset(DVE, sgnb, 0.0)
            w = RANGE
            for itb in range(NBIS):
                cnt = cntb[:, itb:itb + 1]
                sgc = sgnb[:, itb:itb + 1]
                ts(DVE, maskb[:, 0:nd], score[:, 0:nd], mid, 0.0, ALU.is_ge, ALU.add, accum=cnt)
                act(maskb[:, nd:NK], score[:, nd:NK], AF.Sign, bias=mid, scale=-1.0, accum=sgc)
                stt(DVE, zcol, cnt, 2.0, sgc, ALU.mult, ALU.subtract)
                ts(DVE, tcol, zcol, 512.0 - n_act, w, ALU.is_ge, ALU.mult)
                stt(DVE, mid, tcol, -0.5 * w, mid, ALU.add, ALU.add)
                w *= 0.5
            ts(DVE, tcol, mid, -w, None, ALU.add)
            ts(DVE, maskb[:, 0:NK], score[:, 0:NK], tcol, None, ALU.is_ge)
            for c4 in range((nch + 7) // 8):
                ncc = min(8, nch - c4 * 8)
                pb = bankb(4 + c4 % 2 * 2, 2)[:, 0:ncc * 128]
                for cc in range(ncc):
                    ch = c4 * 8 + cc
                    tr(pb[:, cc * 128:(cc + 1) * 128], maskb[:, ch * 128:(ch + 1) * 128], ident)
                cp(ACT if c4 % 2 else DVE, maskT[:, c4 * 8:c4 * 8 + ncc, :], pb.rearrange("p (c q) -> p c q", c=ncc))
            njp = (nch + 1) // 2
            for g in range(4):
                po = bank(6)
                pd = bank(7)
                qsl = Qb[:, 4 * g:4 * g + 4, :].rearrange("p h q -> p (h q)")

                def produce(jp, g=g, qsl=qsl):
                    nh = min(2, nch - 2 * jp)
                    pst = bank(2 * (jp % 3), 2)
                    for half in range(nh):
                        j = jp * 2 + half
                        mm(pst[:, half * 512:(half + 1) * 512], KT[:, g, j * 128:(j + 1) * 128], qsl, True, True)
                    pt = PT[jp % 3]
                    act(pt[:, 0:nh * 512], pst[:, 0:nh * 512], AF.Exp, scale=ASCALE)
                    pt4 = pt[:, 0:nh * 512].rearrange("p (j h q) -> p j h q", j=nh, h=4)
                    mk = maskT[:, jp * 2:jp * 2 + nh, :].unsqueeze(2).to_broadcast([128, nh, 4, 128])
                    tt(DVE, pt4, pt4, mk, ALU.mult)

                def consume(jp, g=g, po=po, pd=pd):
                    nh = min(2, nch - 2 * jp)
                    pt = PT[jp % 3]
                    for half in range(nh):
                        j = jp * 2 + half
                        mm(po, V[:, j, g * 128:(g + 1) * 128], pt[:, half * 512:(half + 1) * 512], j == 0, j == nch - 1)
                        mm(pd, ones_bf, pt[:, half * 512:(half + 1) * 512], j == 0, j == nch - 1)

                produce(0)
                produce(1)
                for jp in range(njp):
                    if jp + 2 < njp:
                        produce(jp + 2)
                    consume(jp)
                recip(rcp, pd)
                tt(DVE, attnT[:, 4 * g:4 * g + 4, tile_i * 128:(tile_i + 1) * 128],
                   po.rearrange("p (h q) -> p h q", h=4), rcp.rearrange("p (h q) -> p h q", h=4), ALU.mult)
        A.release(m_ws)

        A.release(m_kv)
        h = A.alloc([NT, D], F32)
        assert A.mark() == m_attn
        A.release(m_ws)
        for ti in range(NT):
            dma(SP, h[:, ti, :], xo[ti * 128:(ti + 1) * 128, :])
        bi = 0
        for n in range(8):
            pA = load_panel(a_w_out, 0, 8, n * 256)
            pB = load_panel(a_w_out, 1024, 8, n * 256)
            for ti in range(NT):
                pk = bank(bi % 8)[:, 0:256]
                bi += 1
                for kc in range(KC):
                    pan = pA if kc < 8 else pB
                    mm(pk, attnT[:, kc, ti * 128:(ti + 1) * 128], pan[:, kc % 8, :], kc == 0, kc == KC - 1)
                hs = h[:, ti, n * 256:(n + 1) * 256]
                tt(DVE, hs, pk, hs, ALU.add)
        A.release(m_attn)

        def dump(tag):
            if dbg == tag:
                gl = []
                for ti in range(NT):
                    gl.append(dma(SP, dbg_out[ti * 128:(ti + 1) * 128, :], h[:, ti, :]))
                P.wait_for(SP, gl)

        dump("mix0")

        def mlp_ple(layer, tiles, p_dram, p_row0):
            mk0 = A.mark()
            ntl = len(tiles)
            T = ntl * 128
            hT = A.alloc([KC, T], BF16)
            aT = A.alloc([8, T], BF16)
            rtmp = [A.alloc([512], BF16) for _ in range(2)]
            for k, ti in enumerate(tiles):
                norm_T(h[:, ti, :], hT, k * 128, 1 + 2 * layer, 16 * (k % 2))
            gw = 384 if T % 384 == 0 else 512
            ngr = T // gw
            w1 = mlp_w1[layer]
            w2 = mlp_w2[layer]
            bsel = 0
            ri = 0
            for fb in range(8):
                for pp in range(4):
                    c0 = fb * 1024 + pp * 256
                    pA = load_panel(w1, 0, 8, c0)
                    pB = load_panel(w1, 1024, 8, c0)
                    for gi in range(ngr):
                        base = 2 * (bsel % 4)
                        bsel += 1
                        for n in range(2):
                            pk = bank(base + n)
                            for kc in range(KC):
                                pan = pA if kc < 8 else pB
                                mm(pk[:, 0:gw], pan[:, kc % 8, n * 128:(n + 1) * 128], hT[:, kc, gi * gw:(gi + 1) * gw],
                                   kc == 0, kc == KC - 1)
                        for n in range(2):
                            pk = bank(base + n)
                            rt = rtmp[ri % 2]
                            ri += 1
                            act(rt[:, 0:gw], pk[:, 0:gw], AF.Relu)
                            act(aT[:, pp * 2 + n, gi * gw:(gi + 1) * gw], rt[:, 0:gw], AF.Square)
                for n in range(8):
                    pW = load_panel(w2, fb * 1024, 8, n * 256)
                    for k, ti in enumerate(tiles):
                        pk = bank(bsel % 8)[:, 0:256]
                        bsel += 1
                        for fc in range(8):
                            mm(pk, aT[:, fc, k * 128:(k + 1) * 128], pW[:, fc, :], fc == 0, fc == 7)
                        hs = h[:, ti, n * 256:(n + 1) * 256]
                        tt(DVE, hs, pk, hs, ALU.add)
            dump(f"mlp{layer}")
            for k, ti in enumerate(tiles):
                norm_T(h[:, ti, :], hT, k * 128, None, 0)
            pT = aT
            pld = [A.alloc([256], F32) for _ in range(2)]
            plb = [A.alloc([256], BF16) for _ in range(2)]
            sg = [A.alloc([256], F32) for _ in range(2)]
            for k, ti in enumerate(tiles):
                dma(SP, pld[k % 2], p_dram[p_row0 + k * 128:p_row0 + (k + 1) * 128, :])
                cp(ACT, plb[k % 2], pld[k % 2])
                pb = bankb(4 + 2 * (k % 2), 1)[:, 0:256]
                for kc in range(2):
                    tr(pb[:, kc * 128:(kc + 1) * 128], plb[k % 2][:, kc * 128:(kc + 1) * 128], ident)
                cp(DVE, pT[:, 0:2, k * 128:(k + 1) * 128], pb.rearrange("p (kc c) -> p kc c", kc=2))
            wg = ple_wg[layer]
            wp = ple_wp[layer]
            for n in range(8):
                pA = load_panel(wg, 0, 8, n * 256)
                pB = load_panel(wg, 1024, 8, n * 256)
                pP = load_panel(wp, 0, 2, n * 256)
                for k, ti in enumerate(tiles):
                    pg = bank(2 * (k % 2))[:, 0:256]
                    pe = bank(2 * (k % 2) + 1)[:, 0:256]
                    for kc in range(KC):
                        pan = pA if kc < 8 else pB
                        mm(pg, hT[:, kc, k * 128:(k + 1) * 128], pan[:, kc % 8, :], kc == 0, kc == KC - 1)
                    for kc in range(2):
                        mm(pe, pT[:, kc, k * 128:(k + 1) * 128], pP[:, kc, :], kc == 0, kc == 1)
                    s_ = sg[k % 2]
                    act(s_, pg, AF.Sigmoid)
                    tt(DVE, s_, s_, pe, ALU.mult)
                    hs = h[:, ti, n * 256:(n + 1) * 256]
                    tt(DVE, hs, hs, s_, ALU.add)
            A.release(mk0)

        mlp_ple(0, list(range(NT)), p0, 0)
        dump("l0")

        mk1 = A.mark()
        cb = A.alloc([8, KC], F32)
        dma(SP, cb[:, 0, :], b_pw1[0:1, 0:D].rearrange("o (kc p) -> p (o kc)", p=128))
        dma(SP, cb[:, 1, :], b_pw1[0:1, D:2 * D].rearrange("o (kc p) -> p (o kc)", p=128))
        dma(SP, cb[:, 2, :], b_dw.rearrange("o (kc p) -> p (o kc)", p=128))
        dma(SP, cb[:, 3, :], ln_g.rearrange("o (kc p) -> p (o kc)", p=128))
        dma(SP, cb[:, 4, :], ln_b.rearrange("o (kc p) -> p (o kc)", p=128))
        wdw = A.alloc([KC, 31], F32)
        for c in range(KC):
            dma(SP, wdw[:, c, :], w_dw[:, c * 128:(c + 1) * 128].rearrange("j p -> p j"))
        hTh = A.alloc([KC, 640], BF16)
        b2row = A.alloc([D], BF16)
        dma(POOL, b2row[0:1, :], b_pw2)
        vbuf = A.alloc([KC, 512], F32)
        ubuf = [A.alloc([640], BF16) for _ in range(2)]
        sgb = [A.alloc([320], F32) for _ in range(2)]
        sqb = [A.alloc([512], F32)] * 2
        dgb = [A.alloc([16, 128], BF16) for _ in range(2)]
        dgi = 0
        lnm = A.alloc([3, 512], F32)
        for half in (1, 0):
            tiles = [half * 4 + k for k in range(5)]
            for k, ti in enumerate(tiles):
                norm_T(h[:, ti, :], hTh, k * 128, 2, 16 * (k % 2))
            for cq in range(8):
                pans = []
                for part in range(2):
                    pans.append((load_panel(w_pw1, 0, 8, part * D + cq * 256),
                                 load_panel(w_pw1, 1024, 8, part * D + cq * 256)))
                for cc in range(2):
                    c = cq * 2 + cc
                    ub = ubuf[c % 2]
                    for gi in range(2):
                        pa = bank(2 * gi)
                        pg = bank(2 * gi + 1)
                        for part, pk in ((0, pa), (1, pg)):
                            pA, pB = pans[part]
                            for kc in range(KC):
                                pan = pA if kc < 8 else pB
                                mm(pk[:, 0:320], pan[:, kc % 8, cc * 128:(cc + 1) * 128], hTh[:, kc, gi * 320:(gi + 1) * 320],
                                   kc == 0, kc == KC - 1)
                        s_ = sgb[gi]
                        act(s_, pg[:, 0:320], AF.Sigmoid, bias=cb[:, 1, c:c + 1])
                        stt(DVE, ub[:, gi * 320:(gi + 1) * 320], pa[:, 0:320], cb[:, 0, c:c + 1], s_, ALU.add, ALU.mult)
                    if half == 0:
                        ts(DVE, ub[:, 0:128], ub[:, 0:128], hflag_sb[:, 0:1], None, ALU.mult)
                    pacc = bank(4 + c % 2)
                    for (j0, nj) in ((0, 16), (16, 15)):
                        dg = dgb[dgi % 2]
                        dgi += 1
                        tt(DVE, dg[:, 0:nj, :], ident.unsqueeze(1).to_broadcast([128, nj, 128]),
                           wdw[:, c, j0:j0 + nj].unsqueeze(2).to_broadcast([128, nj, 128]), ALU.mult)
                        for jj in range(nj):
                            j = j0 + jj
                            mm(pacc, dg[:, jj, :], ub[:, 98 + j:98 + j + 512], j == 0, j == 30)
                    act(vbuf[:, c, :], pacc, AF.Identity, bias=cb[:, 2, c:c + 1])
            pm = bank(4)
            pq = bank(5)
            for c in range(KC):
                mm(pm, ones_d, vbuf[:, c, :], c == 0, c == KC - 1)
            for c in range(KC):
                sq = sqb[c % 2]
                act(sq, vbuf[:, c, :], AF.Square)
                mm(pq, ones_d, sq, c == 0, c == KC - 1)
            cp(ACT, lnm[:, 0, :], pm)
            tt(DVE, lnm[:, 1, :], lnm[:, 0, :], lnm[:, 0, :], ALU.mult)
            tt(DVE, lnm[:, 1, :], pq, lnm[:, 1, :], ALU.subtract)
            ts(DVE, lnm[:, 1, :], lnm[:, 1, :], 1e-5, None, ALU.add)
            recip(lnm[:, 2, :], lnm[:, 1, :])
            act(lnm[:, 1, :], lnm[:, 2, :], AF.Sqrt)
            zT = hTh
            for c in range(KC):
                sq = sqb[c % 2]
                eng = DVE
                tt(eng, sq, vbuf[:, c, :], lnm[:, 0, :], ALU.subtract)
                tt(eng, sq, sq, lnm[:, 1, :], ALU.mult)
                act(zT[:, c, 0:512], sq, AF.Silu, bias=cb[:, 4, c:c + 1], scale=cb[:, 3, c:c + 1])
            bi = 0
            for n in range(8):
                pA = load_panel(w_pw2, 0, 8, n * 256)
                pB = load_panel(w_pw2, 1024, 8, n * 256)
                for k in range(4):
                    ti = 1 + half * 4 + k
                    pk = bank(bi % 4)[:, 0:256]
                    bi += 1
                    for kc in range(KC):
                        pan = pA if kc < 8 else pB
                        mm(pk, zT[:, kc, k * 128:(k + 1) * 128], pan[:, kc % 8, :], kc == 0, False)
                    mm(pk, ones_bf[0:1, :], b2row[0:1, n * 256:(n + 1) * 256], False, True)
                    hs = h[:, ti, n * 256:(n + 1) * 256]
                    tt(DVE, hs, pk, hs, ALU.add)
        A.release(mk1)
        dump("mix1")

        mlp_ple(1, list(range(1, NT)), p1, 0)
        dump("l1")

        gbc = A.alloc([D], F32)
        dma(SP, gbc, g_fin[0].partition_broadcast(128))
        ob = [A.alloc([D], F32) for _ in range(2)]
        outs = []
        for k in range(8):
            ti = 1 + k
            o = ob[k % 2]
            c0 = 48 + 4 * (k % 2)
            memset(DVE, stats[:, c0:c0 + 1], 0.0)
            act(o, h[:, ti, :], AF.Square, accum=stats[:, c0:c0 + 1])
            ts(DVE, stats[:, c0 + 1:c0 + 2], stats[:, c0:c0 + 1], 1.0 / D, 1e-6, ALU.mult, ALU.add)
            recip(stats[:, c0 + 2:c0 + 3], stats[:, c0 + 1:c0 + 2])
            act(stats[:, c0 + 3:c0 + 4], stats[:, c0 + 2:c0 + 3], AF.Sqrt)
            stt(DVE, o, h[:, ti, :], stats[:, c0 + 3:c0 + 4], gbc, ALU.mult, ALU.mult)
            outs.append(dma(SP, y[k * 128:(k + 1) * 128, :], o))
        P.wait_for(SP, outs)
        P.emit()
        build_program.stats = (len(P.ops), P.nwaits)
    return nc


def _consts():
    d = np.arange(128)
    cst = np.zeros((128, 16), np.float32)
    inv128 = (10000.0 ** (-(d % 64).astype(np.float32) * np.float32(2.0 / 128))).astype(np.float32)
    inv64 = (10000.0 ** (-(d % 32).astype(np.float32) * np.float32(2.0 / 64))).astype(np.float32)
    inv64[64:] = 0.0
    cst[:, 0] = inv128
    cst[:, 1] = inv64
    cst[:, 2] = math.pi / 2
    cst[:, 3] = np.where(d < 64, math.pi, 0.0)
    cst[:, 4] = math.pi / 2
    cst[:, 5] = np.where(d < 32, math.pi, 0.0)
    ident = np.eye(128, dtype=np.float32)
    psw128 = np.zeros((128, 128), np.float32)
    for m in range(128):
        psw128[(m + 64) % 128, m] = 1.0
    psw64 = np.zeros((128, 128), np.float32)
    for m in range(64):
        psw64[(m + 32) % 64, m] = 1.0
    chunkid = np.tile(np.arange(64, dtype=np.float32)[None, :], (128, 1))
    return cst, ident, psw128, psw64, chunkid


_CACHE = {}


def make_in_maps(inputs):
    x = np.ascontiguousarray(inputs["x"], dtype=np.float32)
    p = np.ascontiguousarray(inputs["p"], dtype=np.float32)
    pos = np.ascontiguousarray(inputs["positions"], dtype=np.int32)
    cst, ident, psw128, psw64, chunkid = _consts()
    shared = {
        "cst": cst, "ident": ident, "psw128": psw128, "psw64": psw64, "chunkid": chunkid,
        "norm_mix_g": np.ascontiguousarray(inputs["norm_mix_g"], np.float32),
        "norm_mlp_g": np.ascontiguousarray(inputs["norm_mlp_g"], np.float32),
        "final_g": np.ascontiguousarray(inputs["final_g"], np.float32).reshape(1, D),
        "a_w_in": np.ascontiguousarray(inputs["a_w_in"][0], np.float32),
        "a_w_out": np.ascontiguousarray(inputs["a_w_out"][0], np.float32),
        "a_kidx_g": np.ascontiguousarray(inputs["a_kidx_g"][0], np.float32).reshape(128, 1),
        "a_kidx_b": np.ascontiguousarray(inputs["a_kidx_b"][0], np.float32).reshape(128, 1),
        "b_w_pw1": np.ascontiguousarray(inputs["b_w_pw1"][0], np.float32),
        "b_b_pw1": np.ascontiguousarray(inputs["b_b_pw1"][0], np.float32).reshape(1, 2 * D),
        "b_w_dw": np.ascontiguousarray(inputs["b_w_dw"][0], np.float32),
        "b_b_dw": np.ascontiguousarray(inputs["b_b_dw"][0], np.float32).reshape(1, D),
        "b_ln_g": np.ascontiguousarray(inputs["b_ln_g"][0], np.float32).reshape(1, D),
        "b_ln_b": np.ascontiguousarray(inputs["b_ln_b"][0], np.float32).reshape(1, D),
        "b_w_pw2": np.ascontiguousarray(inputs["b_w_pw2"][0], np.float32),
        "b_b_pw2": np.ascontiguousarray(inputs["b_b_pw2"][0], np.float32).reshape(1, D),
        "mlp_w1": np.ascontiguousarray(inputs["mlp_w1"], np.float32),
        "mlp_w2": np.ascontiguousarray(inputs["mlp_w2"], np.float32),
        "ple_w_proj": np.ascontiguousarray(inputs["ple_w_proj"], np.float32),
        "ple_w_gate": np.ascontiguousarray(inputs["ple_w_gate"], np.float32),
    }
    maps = []
    for c in range(8):
        b = c // 4
        t0 = (c % 4) * 1024
        first = (c % 4) == 0
        rows = np.arange(t0 - 128, t0 + 1024)
        if first:
            rows[:128] = np.arange(0, 128)
        m = dict(shared)
        m["xs"] = x[b]
        m["xo"] = np.ascontiguousarray(x[b][rows])
        m["pos_s"] = pos[b].reshape(1, S)
        m["pos_o"] = np.ascontiguousarray(pos[b][rows]).reshape(1, T0)
        m["p0"] = np.ascontiguousarray(p[0, b][rows])
        m["p1"] = np.ascontiguousarray(p[1, b][t0:t0 + 1024])
        m["qinfo"] = np.ascontiguousarray((rows // 64).astype(np.float32).reshape(NT, 128).T)
        m["hflag"] = np.full((128, 1), 0.0 if first else 1.0, np.float32)
        maps.append(m)
    return maps


def kernel(**inputs):
    if "nc" not in _CACHE:
        _CACHE["nc"] = build_program()
    nc = _CACHE["nc"]
    maps = make_in_maps(inputs)
    res = run_bass_kernel_spmd(nc, maps, core_ids=list(range(8)))
    out = np.empty((2, S, D), np.float32)
    for c in range(8):
        b = c // 4
        t0 = (c % 4) * 1024
        out[b, t0:t0 + 1024] = res.results[c]["y"]
    return out
```

```python
import math
import numpy as np
from contextlib import ExitStack
import concourse.bass as bass
import concourse.mybir as mybir
from concourse.bass_utils import run_bass_kernel_spmd

F32 = mybir.dt.float32
BF16 = mybir.dt.bfloat16
I32 = mybir.dt.int32
AF = mybir.ActivationFunctionType
ALU = mybir.AluOpType
AX = mybir.AxisListType

PE, ACT, DVE, POOL, SP = range(5)
NENG = 5
DSIZE = {F32: 4, BF16: 2, I32: 4}

D = 2048
S = 4096
NT = 9
T0 = NT * 128
KC = 16
DFF = 8192
IN_A = 5264
SB_BYTES = 176 * 1024
TWO_PI = 2.0 * math.pi


class Op:
    __slots__ = ("gid", "eng", "fn", "deps", "is_dma", "signal", "sem", "val")


class Prog:
    PAGE = 64
    NDMA_SEMS = 32

    def __init__(self, nc, sb_bytes, ps_bytes=16384):
        self.nc = nc
        self.ops = []
        self.np_sb = sb_bytes // self.PAGE
        self.np_ps = ps_bytes // self.PAGE
        self.lw = {"sb": np.full(self.np_sb, -1, np.int64), "ps": np.full(self.np_ps, -1, np.int64)}
        self.rd = {
            "sb": np.full((NENG + 1, self.np_sb), -1, np.int64),
            "ps": np.full((NENG + 1, self.np_ps), -1, np.int64),
        }
        self.bases = {}
        self.bank_last = np.full((8, NENG), -1, np.int64)

    def rng(self, ap):
        name = ap.tensor.name
        if name not in self.bases:
            return None
        space, base = self.bases[name]
        pstep = ap.ap[0][0]
        ds = DSIZE[ap.dtype]
        off = ap.offset % pstep if pstep > 0 else ap.offset
        ext = 1
        for st, cnt in ap.ap[1:]:
            ext += abs(st) * (cnt - 1)
        lo = base + off * ds
        hi = lo + ext * ds
        return space, lo // self.PAGE, (hi + self.PAGE - 1) // self.PAGE

    def add(self, eng, fn, reads=(), writes=(), dma=False, extra=()):
        op = Op()
        op.gid = len(self.ops)
        op.eng = eng
        op.fn = fn
        op.is_dma = dma
        op.signal = dma
        op.sem = None
        op.val = 0
        deps = set()
        ridx = NENG if dma else eng
        rr_ = [self.rng(a) for a in reads]
        ww_ = [self.rng(a) for a in writes]
        for r in rr_:
            if r is None:
                continue
            sp, a, b = r
            w = self.lw[sp][a:b]
            deps.update(np.unique(w[w >= 0]).tolist())
        for r in ww_:
            if r is None:
                continue
            sp, a, b = r
            w = self.lw[sp][a:b]
            deps.update(np.unique(w[w >= 0]).tolist())
            rr = self.rd[sp][:, a:b]
            for e in range(NENG + 1):
                if e == eng and not dma and e != NENG:
                    continue
                x = rr[e]
                deps.update(np.unique(x[x >= 0]).tolist())
        for r in rr_:
            if r is None:
                continue
            sp, a, b = r
            self.rd[sp][ridx, a:b] = op.gid
        for r in ww_:
            if r is None:
                continue
            sp, a, b = r
            self.lw[sp][a:b] = op.gid
            self.rd[sp][:, a:b] = -1
        if not dma:
            bpp = 2048 // self.PAGE
            for r in rr_ + ww_:
                if r is None or r[0] != "ps":
                    continue
                for bk in range(r[1] // bpp, (r[2] - 1) // bpp + 1):
                    for e in range(NENG):
                        if e != eng and self.bank_last[bk, e] >= 0:
                            deps.add(int(self.bank_last[bk, e]))
                    self.bank_last[bk, eng] = op.gid
        deps.update(extra)
        deps.discard(op.gid)
        best = {}
        out = set()
        for d in deps:
            o = self.ops[d]
            if o.is_dma:
                out.add(d)
            else:
                if o.eng == PE and eng == PE and not dma:
                    continue
                if o.eng not in best or best[o.eng] < d:
                    best[o.eng] = d
        out.update(best.values())
        op.deps = out
        self.ops.append(op)
        return op.gid

    def wait_for(self, eng, gids):
        op = Op()
        op.gid = len(self.ops)
        op.eng = eng
        op.fn = None
        op.is_dma = False
        op.signal = False
        op.sem = None
        op.val = 0
        op.deps = set(gids)
        self.ops.append(op)
        return op.gid

    def emit(self):
        nc = self.nc
        with ExitStack() as es:
            esems = [es.enter_context(nc.semaphore(f"e{i}")) for i in range(NENG)]
            dsems = [es.enter_context(nc.semaphore(f"d{i}")) for i in range(self.NDMA_SEMS)]
            dcount = [0] * self.NDMA_SEMS
            dlast = [None] * self.NDMA_SEMS
            k = 0
            for op in self.ops:
                if op.is_dma:
                    s = k % self.NDMA_SEMS
                    k += 1
                    if dlast[s] is not None:
                        op.deps.add(dlast[s])
                    dcount[s] += 1
                    op.sem = dsems[s]
                    op.val = 16 * dcount[s]
                    dlast[s] = op.gid
            for op in self.ops:
                for d in op.deps:
                    self.ops[d].signal = True
            ecount = [0] * NENG
            for op in self.ops:
                if op.is_dma or op.fn is None:
                    continue
                if op.signal:
                    ecount[op.eng] += 1
                    op.sem = esems[op.eng]
                    op.val = ecount[op.eng]
            per = [[] for _ in range(NENG)]
            for op in self.ops:
                per[op.eng].append(op)
            ops = self.ops
            self.nwaits = 0

            def run(eng_idx, e):
                waited = {}
                for op in per[eng_idx]:
                    for d in sorted(op.deps):
                        o = ops[d]
                        key = id(o.sem)
                        if waited.get(key, 0) < o.val:
                            e.wait_ge(o.sem, o.val)
                            waited[key] = o.val
                            self.nwaits += 1
                    if op.fn is None:
                        continue
                    ins = op.fn(e)
                    if op.signal:
                        ins.then_inc(op.sem, 16 if op.is_dma else 1)

            with nc.Block() as block:
                @block.tensor
                def _(e):
                    run(PE, e)

                @block.scalar
                def _(e):
                    run(ACT, e)

                @block.vector
                def _(e):
                    run(DVE, e)

                @block.gpsimd
                def _(e):
                    run(POOL, e)

                @block.sync
                def _(e):
                    run(SP, e)


class Arena:
    def __init__(self, ar):
        self.ar = ar
        self.top = 0

    def mark(self):
        return self.top

    def release(self, m):
        self.top = m

    def alloc(self, shape, dt):
        n = 1
        for s in shape:
            n *= s
        nb = (n * DSIZE[dt] + 63) // 64 * 64
        assert self.top + nb <= SB_BYTES, f"SBUF arena overflow {self.top + nb}"
        v = self.ar[:, self.top // 4:(self.top + nb) // 4]
        self.top += nb
        if dt != F32:
            v = v.bitcast(dt)
        v = v[:, :n]
        if len(shape) == 2:
            v = v.rearrange("p (a b) -> p a b", a=shape[0])
        elif len(shape) == 3:
            v = v.rearrange("p (a b c) -> p a b c", a=shape[0], b=shape[1])
        return v


class _Stop(Exception):
    pass


def build_program(dbg=None, stop=None):
    try:
        return _build_program(dbg, stop)
    except _Stop as e:
        return e.args[0]


def _build_program(dbg=None, stop=None):
    nc = bass.Bass("TRN2", target_bir_lowering=False)
    dram = {}

    def din(name, shape, dt=F32):
        dram[name] = nc.dram_tensor(name, list(shape), dt, kind="ExternalInput").ap()
        return dram[name]

    xs = din("xs", [S, D])
    xo = din("xo", [T0, D])
    pos_s = din("pos_s", [1, S], I32)
    pos_o = din("pos_o", [1, T0], I32)
    p0 = din("p0", [T0, 256])
    p1 = din("p1", [1024, 256])
    qinfo = din("qinfo", [128, NT])
    hflag = din("hflag", [128, 1])
    cst = din("cst", [128, 16])
    ident_d = din("ident", [128, 128])
    psw128_d = din("psw128", [128, 128])
    psw64_d = din("psw64", [128, 128])
    chunkid_d = din("chunkid", [128, 64])
    g_mix = din("norm_mix_g", [2, D])
    g_mlp = din("norm_mlp_g", [2, D])
    g_fin = din("final_g", [1, D])
    a_w_in = din("a_w_in", [D, IN_A])
    a_w_out = din("a_w_out", [D, D])
    kidx_g = din("a_kidx_g", [128, 1])
    kidx_b = din("a_kidx_b", [128, 1])
    w_pw1 = din("b_w_pw1", [D, 2 * D])
    b_pw1 = din("b_b_pw1", [1, 2 * D])
    w_dw = din("b_w_dw", [31, D])
    b_dw = din("b_b_dw", [1, D])
    ln_g = din("b_ln_g", [1, D])
    ln_b = din("b_ln_b", [1, D])
    w_pw2 = din("b_w_pw2", [D, D])
    b_pw2 = din("b_b_pw2", [1, D])
    mlp_w1 = din("mlp_w1", [2, D, DFF])
    mlp_w2 = din("mlp_w2", [2, DFF, D])
    ple_wp = din("ple_w_proj", [2, 256, D])
    ple_wg = din("ple_w_gate", [2, D, D])
    y = nc.dram_tensor("y", [1024, D], F32, kind="ExternalOutput").ap()
    dbg_out = None
    if dbg:
        dbg_out = nc.dram_tensor("dbg", [T0, D], F32, kind="ExternalOutput").ap()

    with ExitStack() as es:
        ar = es.enter_context(nc.sbuf_tensor("arena", [128, SB_BYTES // 4], F32))
        ps = es.enter_context(nc.psum_tensor("ps", [128, 4096], F32))
        P = Prog(nc, SB_BYTES)
        P.bases["arena"] = ("sb", 0)
        P.bases["ps"] = ("ps", 0)
        A = Arena(ar)
        es.enter_context(nc.allow_non_contiguous_dma(reason="small per-partition constant loads"))

        def bank(i, n=1):
            return ps[:, i * 512:(i + n) * 512]

        def bankb(i, n=1):
            return ps[:, i * 512:(i + n) * 512].bitcast(BF16)

        def mm(out, lhsT, rhs, start, stop):
            P.add(PE, lambda e: e.matmul(out, lhsT=lhsT, rhs=rhs, start=start, stop=stop),
                  reads=[lhsT, rhs], writes=[out])

        def tr(out, in_, ident):
            P.add(PE, lambda e: e.transpose(out, in_, ident), reads=[in_, ident], writes=[out])

        def act(out, in_, func, bias=None, scale=None, accum=None, eng=ACT):
            kw = {}
            rd = [in_]
            wr = [out]
            if bias is not None:
                kw["bias"] = bias
                if not isinstance(bias, float):
                    rd.append(bias)
            if scale is not None:
                kw["scale"] = scale
                if not isinstance(scale, float):
                    rd.append(scale)
            if accum is not None:
                kw["accum_out"] = accum
                wr.append(accum)
            P.add(ACT, lambda e: e.activation(out=out, in_=in_, func=func, **kw), reads=rd, writes=wr)

        def ts(eng, out, in0, s1, s2, op0, op1=None, accum=None):
            rd = [in0]
            wr = [out]
            if not isinstance(s1, float):
                rd.append(s1)
            if s2 is not None and not isinstance(s2, float):
                rd.append(s2)
            kw = {}
            if op1 is not None:
                kw["op1"] = op1
            if accum is not None:
                kw["accum_out"] = accum
                wr.append(accum)
            P.add(eng, lambda e: e.tensor_scalar(out=out, in0=in0, scalar1=s1, scalar2=s2, op0=op0, **kw),
                  reads=rd, writes=wr)

        def tt(eng, out, in0, in1, op):
            P.add(eng, lambda e: e.tensor_tensor(out=out, in0=in0, in1=in1, op=op), reads=[in0, in1], writes=[out])

        def stt(eng, out, in0, sc, in1, op0, op1):
            rd = [in0, in1]
            if not isinstance(sc, float):
                rd.append(sc)
            P.add(eng, lambda e: e.scalar_tensor_tensor(out=out, in0=in0, scalar=sc, in1=in1, op0=op0, op1=op1),
                  reads=rd, writes=[out])

        def cp(eng, out, in_):
            if eng == ACT:
                act(out, in_, AF.Copy)
            else:
                P.add(eng, lambda e: e.tensor_copy(out=out, in_=in_), reads=[in_], writes=[out])

        def recip(out, in_):
            P.add(DVE, lambda e: e.reciprocal(out=out, in_=in_), reads=[in_], writes=[out])

        def memset(eng, out, v):
            P.add(eng, lambda e: e.memset(out, v), writes=[out])

        def dma(eng, out, in_, extra=()):
            return P.add(eng, lambda e: e.dma_start(out=out, in_=in_), reads=[in_], writes=[out], dma=True, extra=extra)

        def early(tag, views):
            if stop != tag:
                return
            gl = []
            for i, v in enumerate(views):
                ncol = v.shape[-1]
                gl.append(dma(SP, y[i * 128:(i + 1) * 128, 0:ncol], v))
            P.wait_for(SP, gl)
            P.emit()
            build_program.stats = (len(P.ops), P.nwaits)
            raise _Stop(nc)

        ident = A.alloc([128], BF16)
        psw128 = A.alloc([128], BF16)
        psw64 = A.alloc([128], BF16)
        ones_bf = A.alloc([128], BF16)
        ones_f = A.alloc([128], F32)
        ones_d = A.alloc([128], F32)
        cst_sb = A.alloc([16], F32)
        qinfo_sb = A.alloc([NT], F32)
        hflag_sb = A.alloc([1], F32)
        chunkid = A.alloc([64], F32)
        gfm = A.alloc([4, KC], F32)
        kig = A.alloc([2], F32)
        stats = A.alloc([64], F32)
        dma(POOL, ident, ident_d)
        dma(POOL, psw128, psw128_d)
        dma(POOL, psw64, psw64_d)
        dma(SP, cst_sb, cst)
        dma(SP, qinfo_sb, qinfo)
        dma(SP, hflag_sb, hflag)
        dma(SP, chunkid, chunkid_d)
        dma(SP, kig[:, 0:1], kidx_g)
        dma(SP, kig[:, 1:2], kidx_b)
        for i, gsrc in enumerate([g_mix[0:1, :], g_mlp[0:1, :], g_mix[1:2, :], g_mlp[1:2, :]]):
            dma(SP, gfm[:, i, :], gsrc.rearrange("o (kc p) -> p (o kc)", p=128))
        memset(DVE, ones_bf, 1.0)
        memset(DVE, ones_f, 1.0 / 128.0)
        memset(DVE, ones_d, 1.0 / D)

        early('consts', [cst_sb, chunkid, gfm.rearrange('p a b -> p (a b)'), ident.bitcast(F32)])
        NPB = 4
        panels = [A.alloc([8, 256], BF16) for _ in range(NPB)]
        pstate = {"i": 0}

        def load_panel(w2d, r0, nk, c0, ncols=256):
            buf = panels[pstate["i"] % NPB]
            pstate["i"] += 1
            src = w2d[r0:r0 + nk * 128, c0:c0 + ncols].rearrange("(kc p) n -> p kc n", p=128)
            dma(POOL, buf[:, 0:nk, 0:ncols], src)
            return buf

        hn_tm = [A.alloc([D], BF16)] * 2
        nstate = {"i": 0}

        def norm_T(src, dstT, col0, gidx, st_col):
            i = nstate["i"]
            nstate["i"] += 1
            hb = hn_tm[i % 2]
            if gidx is not None:
                ss = stats[:, st_col:st_col + 1]
                memset(DVE, ss, 0.0)
                act(hb, src, AF.Square, accum=ss)
                ts(DVE, stats[:, st_col + 1:st_col + 2], ss, 1.0 / D, 1e-6, ALU.mult, ALU.add)
                recip(stats[:, st_col + 2:st_col + 3], stats[:, st_col + 1:st_col + 2])
                act(stats[:, st_col + 3:st_col + 4], stats[:, st_col + 2:st_col + 3], AF.Sqrt)
                ts(DVE, hb, src, stats[:, st_col + 3:st_col + 4], None, ALU.mult)
            else:
                cp(ACT, hb, src)
            pb = bankb(4 + 2 * (i % 2), 2)
            for kc in range(KC):
                tr(pb[:, kc * 128:(kc + 1) * 128], hb[:, kc * 128:(kc + 1) * 128], ident)
            if gidx is not None:
                for kc in range(KC):
                    o = dstT[:, kc, col0:col0 + 128]
                    i_ = pb[:, kc * 128:(kc + 1) * 128]
                    if kc < 8:
                        act(o, i_, AF.Copy, scale=gfm[:, gidx, kc:kc + 1])
                    else:
                        ts(DVE, o, i_, gfm[:, gidx, kc:kc + 1], None, ALU.mult)
            else:
                o = dstT[:, :, col0:col0 + 128]
                i_ = pb.rearrange("p (kc c) -> p kc c", kc=KC)
                cp(DVE if i % 2 else ACT, o, i_)

        def rope_tables(posb_i, n, tabs, tf):
            posf = tabs[:, 4, :]
            cp(DVE, posf, posb_i)
            x = tf[:, 0, 0:n]
            kf = tf[:, 1, 0:n]
            for (ti, inv_c, ph_c) in ((0, 0, 2), (1, 0, 3), (2, 1, 4), (3, 1, 5)):
                ts(DVE, x, posf, cst_sb[:, inv_c:inv_c + 1], cst_sb[:, ph_c:ph_c + 1], ALU.mult, ALU.add)
                ts(DVE, posb_i, x, 1.0 / TWO_PI, None, ALU.mult)
                cp(DVE, kf, posb_i)
                stt(DVE, x, kf, -TWO_PI, x, ALU.mult, ALU.add)
                ts(DVE, kf, x, math.pi, -TWO_PI, ALU.is_gt, ALU.mult)
                tt(DVE, x, x, kf, ALU.add)
                ts(DVE, x, x, math.pi, -math.pi, ALU.min, ALU.max)
                act(tabs[:, ti, :], x, AF.Sin)

        rstate = {"i": 0}

        def rope_apply(dst, src_ps, n, Ct, St, psw, tmpbs, tmpfs, pbanks):
            i = rstate["i"]
            rstate["i"] += 1
            tmpb = tmpbs[i % len(tmpbs)]
            tmpf = tmpfs[i % len(tmpfs)]
            pbank = pbanks[i % len(pbanks)]
            cp(ACT, tmpb[:, 0:n], src_ps)
            mm(pbank[:, 0:n], psw, tmpb[:, 0:n], True, True)
            tt(DVE, tmpf[:, 0, 0:n], tmpb[:, 0:n], Ct, ALU.mult)
            tt(DVE, tmpf[:, 1, 0:n], pbank[:, 0:n], St, ALU.mult)
            tt(DVE, dst, tmpf[:, 0, 0:n], tmpf[:, 1, 0:n], ALU.add)

        qscr = nc.dram_tensor("qscr", [NT, 128, 2048], BF16).ap()
        qiscr = nc.dram_tensor("qiscr", [NT, 128, 2048], BF16).ap()
        wiscr = nc.dram_tensor("wiscr", [NT, 128, 16], F32).ap()
        m_kv = A.mark()
        KT = A.alloc([4, S], BF16)
        V = A.alloc([32, 512], BF16)
        kiT = A.alloc([S], BF16)
        m_attn = A.mark()
        assert m_attn - m_kv == NT * D * 4
        wkv = A.alloc([KC, 1152], BF16)
        for kb in range(2):
            dma(POOL, wkv[:, kb * 8:(kb + 1) * 8, 0:1024],
                a_w_in[kb * 1024:(kb + 1) * 1024, 2048:3072].rearrange("(kc p) n -> p kc n", p=128))
            dma(POOL, wkv[:, kb * 8:(kb + 1) * 8, 1024:1152],
                a_w_in[kb * 1024:(kb + 1) * 1024, 5120:5248].rearrange("(kc p) n -> p kc n", p=128))
        G1 = 256
        xt = [A.alloc([D], F32)] * 2
        hnT2 = [A.alloc([KC, G1], BF16) for _ in range(2)]
        tabs2 = [A.alloc([5, G1], F32) for _ in range(2)]
        posb2 = [A.alloc([G1], I32) for _ in range(2)]
        tmpbs = [A.alloc([G1], BF16) for _ in range(2)]
        tmpfs = [A.alloc([2, G1], F32) for _ in range(2)]
        lnb = A.alloc([3, G1], F32)
        def a1_tables(sg):
            s0 = sg * G1
            posb = posb2[sg % 2]
            dma(SP, posb, pos_s[0, s0:s0 + G1].partition_broadcast(128))
            rope_tables(posb, G1, tabs2[sg % 2], tmpfs[sg % 2])

        def a1_norm(sg, ti):
            s0 = sg * G1
            xb = xt[ti % 2]
            dma(SP, xb, xs[s0 + ti * 128:s0 + (ti + 1) * 128, :])
            norm_T(xb, hnT2[sg % 2], ti * 128, 0, 16 * (ti % 2))

        def a1_K(sg):
            s0 = sg * G1
            hnT = hnT2[sg % 2]
            tabs = tabs2[sg % 2]
            pks = []
            for g in range(4):
                pk = bank(g)[:, 0:G1]
                for kc in range(KC):
                    mm(pk, wkv[:, kc, g * 128:(g + 1) * 128], hnT[:, kc, :], kc == 0, kc == KC - 1)
                pks.append(pk)
            for g in range(4):
                rope_apply(KT[:, g, s0:s0 + G1], pks[g], G1, tabs[:, 0, :], tabs[:, 1, :], psw128, tmpbs, tmpfs,
                           [bank(6), bank(7)])

        def a1_Vki(sg):
            s0 = sg * G1
            hnT = hnT2[sg % 2]
            tabs = tabs2[sg % 2]
            pki = bank(0)[:, 0:G1]
            for kc in range(KC):
                mm(pki, wkv[:, kc, 1024:1152], hnT[:, kc, :], kc == 0, kc == KC - 1)
            cp(ACT, lnb[:, 0, :], pki)
            pm_ = bank(1)[:, 0:G1]
            mm(pm_, ones_f, lnb[:, 0, :], True, True)
            for ti in range(2):
                pv = bank(2 + ti)
                for kc in range(KC):
                    mm(pv, hnT[:, kc, ti * 128:(ti + 1) * 128], wkv[:, kc, 512:1024], kc == 0, kc == KC - 1)
                cp(ACT if ti % 2 else DVE, V[:, sg * 2 + ti, :], pv)
            tt(DVE, lnb[:, 1, :], lnb[:, 0, :], pm_, ALU.subtract)
            act(lnb[:, 2, :], lnb[:, 1, :], AF.Square)
            mm(pm_, ones_f, lnb[:, 2, :], True, True)
            ts(DVE, lnb[:, 0, :], pm_, 1e-5, None, ALU.add)
            recip(lnb[:, 2, :], lnb[:, 0, :])
            act(lnb[:, 0, :], lnb[:, 2, :], AF.Sqrt)
            tt(DVE, lnb[:, 1, :], lnb[:, 1, :], lnb[:, 0, :], ALU.mult)
            ts(DVE, lnb[:, 2, :], lnb[:, 1, :], kig[:, 0:1], kig[:, 1:2], ALU.mult, ALU.add)
            rope_apply(kiT[:, s0:s0 + G1], lnb[:, 2, :], G1, tabs[:, 2, :], tabs[:, 3, :], psw64, tmpbs, tmpfs,
                       [bank(6), bank(7)])

        NG1 = S // G1
        a1_tables(0)
        a1_norm(0, 0)
        a1_norm(0, 1)
        for sg in range(NG1):
            nxt = sg + 1 < NG1
            if nxt:
                a1_tables(sg + 1)
            a1_K(sg)
            if nxt:
                a1_norm(sg + 1, 0)
            a1_Vki(sg)
            if nxt:
                a1_norm(sg + 1, 1)
        early('a1', [KT[:, 0, :].bitcast(F32), KT[:, 3, :].bitcast(F32), kiT.bitcast(F32), V[:, 0:8, :].rearrange('p a b -> p (a b)').bitcast(F32), V[:, 24:32, :].rearrange('p a b -> p (a b)').bitcast(F32)])
        A.release(m_attn)

        attnT = A.alloc([KC, T0], BF16)
        m_ws = A.mark()
        hnTg = A.alloc([KC, 384], BF16)
        tabs_o = A.alloc([5, 384], F32)
        posb_o = A.alloc([384], I32)
        tmpb_os = [A.alloc([384], BF16) for _ in range(2)]
        tmpf_os = [A.alloc([2, 384], F32) for _ in range(2)]
        tmpf_o = tmpf_os[0]
        stage = [A.alloc([384], BF16) for _ in range(2)]
        wwi = A.alloc([KC, 16], BF16)
        wist = A.alloc([3, 16], F32)
        xt2 = [A.alloc([D], F32)] * 2
        for kb in range(2):
            dma(POOL, wwi[:, kb * 8:(kb + 1) * 8, :],
                a_w_in[kb * 1024:(kb + 1) * 1024, 5248:5264].rearrange("(kc p) n -> p kc n", p=128))
        WSCALE = (16 ** -0.5) * (128 ** -0.5)
        ASCALE = 128 ** -0.5
        NBIS = 26
        RANGE = 64.0
        wr_gids = [[] for _ in range(NT)]
        sti = 0
        for grp in range(3):
            t0g = grp * 384
            dma(SP, posb_o, pos_o[0, t0g:t0g + 384].partition_broadcast(128))
            rope_tables(posb_o, 384, tabs_o, tmpf_o)
            for ti in range(3):
                xb = xt2[ti % 2]
                dma(SP, xb, xo[t0g + ti * 128:t0g + (ti + 1) * 128, :])
                norm_T(xb, hnTg, ti * 128, 0, 16 * (ti % 2))
            jobs = [(scr, c_base, Ct, St, psw, hp)
                    for (scr, c_base, Ct, St, psw) in ((qscr, 0, 0, 1, psw128), (qiscr, 3072, 2, 3, psw64))
                    for hp in range(8)]

            def a2_proj(k):
                scr, c_base, Ct, St, psw, hp = jobs[k]
                pA = load_panel(a_w_in, 0, 8, c_base + hp * 256)
                pB = load_panel(a_w_in, 1024, 8, c_base + hp * 256)
                for n in range(2):
                    pk = bank(2 * (k % 2) + n)
                    for kc in range(KC):
                        pan = pA if kc < 8 else pB
                        mm(pk[:, 0:384], pan[:, kc % 8, n * 128:(n + 1) * 128], hnTg[:, kc, :], kc == 0, kc == KC - 1)

            def a2_rope(k):
                nonlocal sti
                scr, c_base, Ct, St, psw, hp = jobs[k]
                for n in range(2):
                    hd = hp * 2 + n
                    st_ = stage[sti % 2]
                    sti += 1
                    rope_apply(st_, bank(2 * (k % 2) + n)[:, 0:384], 384, tabs_o[:, Ct, :], tabs_o[:, St, :],
                               psw, tmpb_os, tmpf_os, [bank(6), bank(7)])
                    gd = dma(SP, scr[grp * 3:(grp + 1) * 3, :, hd * 128:(hd + 1) * 128].rearrange("t p q -> p t q"),
                             st_.rearrange("p (t q) -> p t q", t=3))
                    for k3 in range(3):
                        wr_gids[grp * 3 + k3].append(gd)

            a2_proj(0)
            for k in range(len(jobs)):
                if k + 1 < len(jobs):
                    a2_proj(k + 1)
                a2_rope(k)
            for ti in range(3):
                pw = bank(ti % 2)
                for kc in range(KC):
                    mm(pw[:, 0:16], hnTg[:, kc, ti * 128:(ti + 1) * 128], wwi[:, kc, :], kc == 0, kc == KC - 1)
                ts(DVE, wist[:, ti, :], pw[:, 0:16], WSCALE, None, ALU.mult)
                gd = dma(SP, wiscr[grp * 3 + ti], wist[:, ti, :])
                wr_gids[grp * 3 + ti].append(gd)
        early('a2', [tabs_o.rearrange('p a b -> p (a b)'), hnTg[:, 0:4, :].rearrange('p a b -> p (a b)').bitcast(F32)])
        A.release(m_ws)

        Qb = A.alloc([16, 128], BF16)
        qib = A.alloc([16, 128], BF16)
        wi_sb = A.alloc([16], F32)
        cntb = A.alloc([32], F32)
        maskT = A.alloc([32, 128], BF16)
        score = A.alloc([S], F32)
        rbreg = A.alloc([2048], F32)
        rbuf = [rbreg[:, k * 256:(k + 1) * 256].bitcast(BF16) for k in range(4)]
        dgw = A.alloc([16, 128], BF16)
        maskb = rbreg.bitcast(BF16)
        PT = [score[:, k * 512:(k + 1) * 512].bitcast(BF16) for k in range(3)]
        rcp = score[:, 1536:2048]
        sgnb = A.alloc([32], F32)
        zcol = stats[:, 35:36]
        for tile_i in range(NT):
            nch = 24 + tile_i
            NK = nch * 128
            dma(SP, Qb.rearrange("p h q -> p (h q)"), qscr[tile_i], extra=wr_gids[tile_i])
            dma(SP, qib.rearrange("p h q -> p (h q)"), qiscr[tile_i], extra=wr_gids[tile_i])
            dma(SP, wi_sb, wiscr[tile_i], extra=wr_gids[tile_i])
            sc3 = score[:, 0:NK].rearrange("p (c k) -> p c k", k=64)
            ts(DVE, sc3, chunkid[:, 0:2 * nch].unsqueeze(2).to_broadcast([128, 2 * nch, 64]),
               qinfo_sb[:, tile_i:tile_i + 1], -1e30, ALU.is_gt, ALU.mult)
            tt(DVE, dgw, ident.unsqueeze(1).to_broadcast([128, 16, 128]),
               wi_sb.unsqueeze(2).to_broadcast([128, 16, 128]), ALU.mult)
            for kb in range((NK + 511) // 512):
                wd = min(512, NK - kb * 512)
                pS = bank(4 + kb % 2)[:, 0:wd]
                kis = kiT[:, kb * 512:kb * 512 + wd]

                def dots(hh, wd=wd, kis=kis):
                    pA = bank(hh % 4)[:, 0:wd]
                    mm(pA, qib[:, hh, :], kis, True, True)
                    act(rbuf[hh % 4][:, 0:wd], pA, AF.Relu)

                def hsum(hh, wd=wd, pS=pS):
                    mm(pS, dgw[:, hh, :], rbuf[hh % 4][:, 0:wd], hh == 0, hh == 15)

                dots(0)
                dots(1)
                for hh in range(16):
                    if hh + 2 < 16:
                        dots(hh + 2)
                    hsum(hh)
                seg = score[:, kb * 512:kb * 512 + wd]
                tt(DVE, seg, pS, seg, ALU.add)
            mid = stats[:, 32:33]
            tcol = stats[:, 33:34]
            nd = ((NK * 9 // 16) // 128) * 128
            n_act = NK - nd
            memset(DVE, mid, 0.0)
            memset(DVE, cntb, 0.0)
            memset(DVE, sgnb, 0.0)
            w = RANGE
            for itb in range(NBIS):
                cnt = cntb[:, itb:itb + 1]
                sgc = sgnb[:, itb:itb + 1]
                ts(DVE, maskb[:, 0:nd], score[:, 0:nd], mid, 0.0, ALU.is_ge, ALU.add, accum=cnt)
                act(maskb[:, nd:NK], score[:, nd:NK], AF.Sign, bias=mid, scale=-1.0, accum=sgc)
                stt(DVE, zcol, cnt, 2.0, sgc, ALU.mult, ALU.subtract)
                ts(DVE, tcol, zcol, 512.0 - n_act, w, ALU.is_ge, ALU.mult)
                stt(DVE, mid, tcol, -0.5 * w, mid, ALU.add, ALU.add)
                w *= 0.5
            ts(DVE, tcol, mid, -w, None, ALU.add)
            ts(DVE, maskb[:, 0:NK], score[:, 0:NK], tcol, None, ALU.is_ge)
            for c4 in range((nch + 7) // 8):
                ncc = min(8, nch - c4 * 8)
                pb = bankb(4 + c4 % 2 * 2, 2)[:, 0:ncc * 128]
                for cc in range(ncc):
                    ch = c4 * 8 + cc
                    tr(pb[:, cc * 128:(cc + 1) * 128], maskb[:, ch * 128:(ch + 1) * 128], ident)
                cp(ACT if c4 % 2 else DVE, maskT[:, c4 * 8:c4 * 8 + ncc, :], pb.rearrange("p (c q) -> p c q", c=ncc))
            njp = (nch + 1) // 2
            for g in range(4):
                po = bank(6)
                pd = bank(7)
                qsl = Qb[:, 4 * g:4 * g + 4, :].rearrange("p h q -> p (h q)")

                def produce(jp, g=g, qsl=qsl):
                    nh = min(2, nch - 2 * jp)
                    pst = bank(2 * (jp % 3), 2)
                    for half in range(nh):
                        j = jp * 2 + half
                        mm(pst[:, half * 512:(half + 1) * 512], KT[:, g, j * 128:(j + 1) * 128], qsl, True, True)
                    pt = PT[jp % 3]
                    act(pt[:, 0:nh * 512], pst[:, 0:nh * 512], AF.Exp, scale=ASCALE)
                    pt4 = pt[:, 0:nh * 512].rearrange("p (j h q) -> p j h q", j=nh, h=4)
                    mk = maskT[:, jp * 2:jp * 2 + nh, :].unsqueeze(2).to_broadcast([128, nh, 4, 128])
                    tt(DVE, pt4, pt4, mk, ALU.mult)

                def consume(jp, g=g, po=po, pd=pd):
                    nh = min(2, nch - 2 * jp)
                    pt = PT[jp % 3]
                    for half in range(nh):
                        j = jp * 2 + half
                        mm(po, V[:, j, g * 128:(g + 1) * 128], pt[:, half * 512:(half + 1) * 512], j == 0, j == nch - 1)
                        mm(pd, ones_bf, pt[:, half * 512:(half + 1) * 512], j == 0, j == nch - 1)

                produce(0)
                produce(1)
                for jp in range(njp):
                    if jp + 2 < njp:
                        produce(jp + 2)
                    consume(jp)
                recip(rcp, pd)
                tt(DVE, attnT[:, 4 * g:4 * g + 4, tile_i * 128:(tile_i + 1) * 128],
                   po.rearrange("p (h q) -> p h q", h=4), rcp.rearrange("p (h q) -> p h q", h=4), ALU.mult)
        A.release(m_ws)

        A.release(m_kv)
        h = A.alloc([NT, D], F32)
        assert A.mark() == m_attn
        A.release(m_ws)
        for ti in range(NT):
            dma(SP, h[:, ti, :], xo[ti * 128:(ti + 1) * 128, :])
        bi = 0
        for n in range(8):
            pA = load_panel(a_w_out, 0, 8, n * 256)
            pB = load_panel(a_w_out, 1024, 8, n * 256)
            for ti in range(NT):
                pk = bank(bi % 8)[:, 0:256]
                bi += 1
                for kc in range(KC):
                    pan = pA if kc < 8 else pB
                    mm(pk, attnT[:, kc, ti * 128:(ti + 1) * 128], pan[:, kc % 8, :], kc == 0, kc == KC - 1)
                hs = h[:, ti, n * 256:(n + 1) * 256]
                tt(DVE, hs, pk, hs, ALU.add)
        A.release(m_attn)

        def dump(tag):
            if dbg == tag:
                gl = []
                for ti in range(NT):
                    gl.append(dma(SP, dbg_out[ti * 128:(ti + 1) * 128, :], h[:, ti, :]))
                P.wait_for(SP, gl)

        dump("mix0")

        def mlp_ple(layer, tiles, p_dram, p_row0):
            mk0 = A.mark()
            ntl = len(tiles)
            T = ntl * 128
            hT = A.alloc([KC, T], BF16)
            aT = A.alloc([8, T], BF16)
            rtmp = [A.alloc([512], BF16) for _ in range(2)]
            for k, ti in enumerate(tiles):
                norm_T(h[:, ti, :], hT, k * 128, 1 + 2 * layer, 16 * (k % 2))
            gw = 384 if T % 384 == 0 else 512
            ngr = T // gw
            w1 = mlp_w1[layer]
            w2 = mlp_w2[layer]
            bsel = 0
            ri = 0
            for fb in range(8):
                for pp in range(4):
                    c0 = fb * 1024 + pp * 256
                    pA = load_panel(w1, 0, 8, c0)
                    pB = load_panel(w1, 1024, 8, c0)
                    for gi in range(ngr):
                        base = 2 * (bsel % 4)
                        bsel += 1
                        for n in range(2):
                            pk = bank(base + n)
                            for kc in range(KC):
                                pan = pA if kc < 8 else pB
                                mm(pk[:, 0:gw], pan[:, kc % 8, n * 128:(n + 1) * 128], hT[:, kc, gi * gw:(gi + 1) * gw],
                                   kc == 0, kc == KC - 1)
                        for n in range(2):
                            pk = bank(base + n)
                            rt = rtmp[ri % 2]
                            ri += 1
                            act(rt[:, 0:gw], pk[:, 0:gw], AF.Relu)
                            act(aT[:, pp * 2 + n, gi * gw:(gi + 1) * gw], rt[:, 0:gw], AF.Square)
                for n in range(8):
                    pW = load_panel(w2, fb * 1024, 8, n * 256)
                    for k, ti in enumerate(tiles):
                        pk = bank(bsel % 8)[:, 0:256]
                        bsel += 1
                        for fc in range(8):
                            mm(pk, aT[:, fc, k * 128:(k + 1) * 128], pW[:, fc, :], fc == 0, fc == 7)
                        hs = h[:, ti, n * 256:(n + 1) * 256]
                        tt(DVE, hs, pk, hs, ALU.add)
            dump(f"mlp{layer}")
            for k, ti in enumerate(tiles):
                norm_T(h[:, ti, :], hT, k * 128, None, 0)
            pT = aT
            pld = [A.alloc([256], F32) for _ in range(2)]
            plb = [A.alloc([256], BF16) for _ in range(2)]
            sg = [A.alloc([256], F32) for _ in range(2)]
            for k, ti in enumerate(tiles):
                dma(SP, pld[k % 2], p_dram[p_row0 + k * 128:p_row0 + (k + 1) * 128, :])
                cp(ACT, plb[k % 2], pld[k % 2])
                pb = bankb(4 + 2 * (k % 2), 1)[:, 0:256]
                for kc in range(2):
                    tr(pb[:, kc * 128:(kc + 1) * 128], plb[k % 2][:, kc * 128:(kc + 1) * 128], ident)
                cp(DVE, pT[:, 0:2, k * 128:(k + 1) * 128], pb.rearrange("p (kc c) -> p kc c", kc=2))
            wg = ple_wg[layer]
            wp = ple_wp[layer]
            for n in range(8):
                pA = load_panel(wg, 0, 8, n * 256)
                pB = load_panel(wg, 1024, 8, n * 256)
                pP = load_panel(wp, 0, 2, n * 256)
                for k, ti in enumerate(tiles):
                    pg = bank(2 * (k % 2))[:, 0:256]
                    pe = bank(2 * (k % 2) + 1)[:, 0:256]
                    for kc in range(KC):
                        pan = pA if kc < 8 else pB
                        mm(pg, hT[:, kc, k * 128:(k + 1) * 128], pan[:, kc % 8, :], kc == 0, kc == KC - 1)
                    for kc in range(2):
                        mm(pe, pT[:, kc, k * 128:(k + 1) * 128], pP[:, kc, :], kc == 0, kc == 1)
                    s_ = sg[k % 2]
                    act(s_, pg, AF.Sigmoid)
                    tt(DVE, s_, s_, pe, ALU.mult)
                    hs = h[:, ti, n * 256:(n + 1) * 256]
                    tt(DVE, hs, hs, s_, ALU.add)
            A.release(mk0)

        mlp_ple(0, list(range(NT)), p0, 0)
        dump("l0")

        mk1 = A.mark()
        cb = A.alloc([8, KC], F32)
        dma(SP, cb[:, 0, :], b_pw1[0:1, 0:D].rearrange("o (kc p) -> p (o kc)", p=128))
        dma(SP, cb[:, 1, :], b_pw1[0:1, D:2 * D].rearrange("o (kc p) -> p (o kc)", p=128))
        dma(SP, cb[:, 2, :], b_dw.rearrange("o (kc p) -> p (o kc)", p=128))
        dma(SP, cb[:, 3, :], ln_g.rearrange("o (kc p) -> p (o kc)", p=128))
        dma(SP, cb[:, 4, :], ln_b.rearrange("o (kc p) -> p (o kc)", p=128))
        wdw = A.alloc([KC, 31], F32)
        for c in range(KC):
            dma(SP, wdw[:, c, :], w_dw[:, c * 128:(c + 1) * 128].rearrange("j p -> p j"))
        hTh = A.alloc([KC, 640], BF16)
        b2row = A.alloc([D], BF16)
        dma(POOL, b2row[0:1, :], b_pw2)
        vbuf = A.alloc([KC, 512], F32)
        ubuf = [A.alloc([640], BF16) for _ in range(2)]
        sgb = [A.alloc([320], F32) for _ in range(2)]
        sqb = [A.alloc([512], F32)] * 2
        dgb = [A.alloc([16, 128], BF16) for _ in range(2)]
        dgi = 0
        lnm = A.alloc([3, 512], F32)
        for half in (1, 0):
            tiles = [half * 4 + k for k in range(5)]
            for k, ti in enumerate(tiles):
                norm_T(h[:, ti, :], hTh, k * 128, 2, 16 * (k % 2))
            for cq in range(8):
                pans = []
                for part in range(2):
                    pans.append((load_panel(w_pw1, 0, 8, part * D + cq * 256),
                                 load_panel(w_pw1, 1024, 8, part * D + cq * 256)))
                for cc in range(2):
                    c = cq * 2 + cc
                    ub = ubuf[c % 2]
                    for gi in range(2):
                        pa = bank(2 * gi)
                        pg = bank(2 * gi + 1)
                        for part, pk in ((0, pa), (1, pg)):
                            pA, pB = pans[part]
                            for kc in range(KC):
                                pan = pA if kc < 8 else pB
                                mm(pk[:, 0:320], pan[:, kc % 8, cc * 128:(cc + 1) * 128], hTh[:, kc, gi * 320:(gi + 1) * 320],
                                   kc == 0, kc == KC - 1)
                        s_ = sgb[gi]
                        act(s_, pg[:, 0:320], AF.Sigmoid, bias=cb[:, 1, c:c + 1])
                        stt(DVE, ub[:, gi * 320:(gi + 1) * 320], pa[:, 0:320], cb[:, 0, c:c + 1], s_, ALU.add, ALU.mult)
                    if half == 0:
                        ts(DVE, ub[:, 0:128], ub[:, 0:128], hflag_sb[:, 0:1], None, ALU.mult)
                    pacc = bank(4 + c % 2)
                    for (j0, nj) in ((0, 16), (16, 15)):
                        dg = dgb[dgi % 2]
                        dgi += 1
                        tt(DVE, dg[:, 0:nj, :], ident.unsqueeze(1).to_broadcast([128, nj, 128]),
                           wdw[:, c, j0:j0 + nj].unsqueeze(2).to_broadcast([128, nj, 128]), ALU.mult)
                        for jj in range(nj):
                            j = j0 + jj
                            mm(pacc, dg[:, jj, :], ub[:, 98 + j:98 + j + 512], j == 0, j == 30)
                    act(vbuf[:, c, :], pacc, AF.Identity, bias=cb[:, 2, c:c + 1])
            pm = bank(4)
            pq = bank(5)
            for c in range(KC):
                mm(pm, ones_d, vbuf[:, c, :], c == 0, c == KC - 1)
            for c in range(KC):
                sq = sqb[c % 2]
                act(sq, vbuf[:, c, :], AF.Square)
                mm(pq, ones_d, sq, c == 0, c == KC - 1)
            cp(ACT, lnm[:, 0, :], pm)
            tt(DVE, lnm[:, 1, :], lnm[:, 0, :], lnm[:, 0, :], ALU.mult)
            tt(DVE, lnm[:, 1, :], pq, lnm[:, 1, :], ALU.subtract)
            ts(DVE, lnm[:, 1, :], lnm[:, 1, :], 1e-5, None, ALU.add)
            recip(lnm[:, 2, :], lnm[:, 1, :])
            act(lnm[:, 1, :], lnm[:, 2, :], AF.Sqrt)
            zT = hTh
            for c in range(KC):
                sq = sqb[c % 2]
                eng = DVE
                tt(eng, sq, vbuf[:, c, :], lnm[:, 0, :], ALU.subtract)
                tt(eng, sq, sq, lnm[:, 1, :], ALU.mult)
                act(zT[:, c, 0:512], sq, AF.Silu, bias=cb[:, 4, c:c + 1], scale=cb[:, 3, c:c + 1])
            bi = 0
            for n in range(8):
                pA = load_panel(w_pw2, 0, 8, n * 256)
                pB = load_panel(w_pw2, 1024, 8, n * 256)
                for k in range(4):
                    ti = 1 + half * 4 + k
                    pk = bank(bi % 4)[:, 0:256]
                    bi += 1
                    for kc in range(KC):
                        pan = pA if kc < 8 else pB
                        mm(pk, zT[:, kc, k * 128:(k + 1) * 128], pan[:, kc % 8, :], kc == 0, False)
                    mm(pk, ones_bf[0:1, :], b2row[0:1, n * 256:(n + 1) * 256], False, True)
                    hs = h[:, ti, n * 256:(n + 1) * 256]
                    tt(DVE, hs, pk, hs, ALU.add)
        A.release(mk1)
        dump("mix1")

        mlp_ple(1, list(range(1, NT)), p1, 0)
        dump("l1")

        gbc = A.alloc([D], F32)
        dma(SP, gbc, g_fin[0].partition_broadcast(128))
        ob = [A.alloc([D], F32) for _ in range(2)]
        outs = []
        for k in range(8):
            ti = 1 + k
            o = ob[k % 2]
            c0 = 48 + 4 * (k % 2)
            memset(DVE, stats[:, c0:c0 + 1], 0.0)
            act(o, h[:, ti, :], AF.Square, accum=stats[:, c0:c0 + 1])
            ts(DVE, stats[:, c0 + 1:c0 + 2], stats[:, c0:c0 + 1], 1.0 / D, 1e-6, ALU.mult, ALU.add)
            recip(stats[:, c0 + 2:c0 + 3], stats[:, c0 + 1:c0 + 2])
            act(stats[:, c0 + 3:c0 + 4], stats[:, c0 + 2:c0 + 3], AF.Sqrt)
            stt(DVE, o, h[:, ti, :], stats[:, c0 + 3:c0 + 4], gbc, ALU.mult, ALU.mult)
            outs.append(dma(SP, y[k * 128:(k + 1) * 128, :], o))
        P.wait_for(SP, outs)
        P.emit()
        build_program.stats = (len(P.ops), P.nwaits)
    return nc


def _consts():
    d = np.arange(128)
    cst = np.zeros((128, 16), np.float32)
    inv128 = (10000.0 ** (-(d % 64).astype(np.float32) * np.float32(2.0 / 128))).astype(np.float32)
    inv64 = (10000.0 ** (-(d % 32).astype(np.float32) * np.float32(2.0 / 64))).astype(np.float32)
    inv64[64:] = 0.0
    cst[:, 0] = inv128
    cst[:, 1] = inv64
    cst[:, 2] = math.pi / 2
    cst[:, 3] = np.where(d < 64, math.pi, 0.0)
    cst[:, 4] = math.pi / 2
    cst[:, 5] = np.where(d < 32, math.pi, 0.0)
    ident = np.eye(128, dtype=np.float32)
    psw128 = np.zeros((128, 128), np.float32)
    for m in range(128):
        psw128[(m + 64) % 128, m] = 1.0
    psw64 = np.zeros((128, 128), np.float32)
    for m in range(64):
        psw64[(m + 32) % 64, m] = 1.0
    chunkid = np.tile(np.arange(64, dtype=np.float32)[None, :], (128, 1))
    return cst, ident, psw128, psw64, chunkid


_CACHE = {}


def make_in_maps(inputs):
    x = np.ascontiguousarray(inputs["x"], dtype=np.float32)
    p = np.ascontiguousarray(inputs["p"], dtype=np.float32)
    pos = np.ascontiguousarray(inputs["positions"], dtype=np.int32)
    cst, ident, psw128, psw64, chunkid = _consts()
    shared = {
        "cst": cst, "ident": ident, "psw128": psw128, "psw64": psw64, "chunkid": chunkid,
        "norm_mix_g": np.ascontiguousarray(inputs["norm_mix_g"], np.float32),
        "norm_mlp_g": np.ascontiguousarray(inputs["norm_mlp_g"], np.float32),
        "final_g": np.ascontiguousarray(inputs["final_g"], np.float32).reshape(1, D),
        "a_w_in": np.ascontiguousarray(inputs["a_w_in"][0], np.float32),
        "a_w_out": np.ascontiguousarray(inputs["a_w_out"][0], np.float32),
        "a_kidx_g": np.ascontiguousarray(inputs["a_kidx_g"][0], np.float32).reshape(128, 1),
        "a_kidx_b": np.ascontiguousarray(inputs["a_kidx_b"][0], np.float32).reshape(128, 1),
        "b_w_pw1": np.ascontiguousarray(inputs["b_w_pw1"][0], np.float32),
        "b_b_pw1": np.ascontiguousarray(inputs["b_b_pw1"][0], np.float32).reshape(1, 2 * D),
        "b_w_dw": np.ascontiguousarray(inputs["b_w_dw"][0], np.float32),
        "b_b_dw": np.ascontiguousarray(inputs["b_b_dw"][0], np.float32).reshape(1, D),
        "b_ln_g": np.ascontiguousarray(inputs["b_ln_g"][0], np.float32).reshape(1, D),
        "b_ln_b": np.ascontiguousarray(inputs["b_ln_b"][0], np.float32).reshape(1, D),
        "b_w_pw2": np.ascontiguousarray(inputs["b_w_pw2"][0], np.float32),
        "b_b_pw2": np.ascontiguousarray(inputs["b_b_pw2"][0], np.float32).reshape(1, D),
        "mlp_w1": np.ascontiguousarray(inputs["mlp_w1"], np.float32),
        "mlp_w2": np.ascontiguousarray(inputs["mlp_w2"], np.float32),
        "ple_w_proj": np.ascontiguousarray(inputs["ple_w_proj"], np.float32),
        "ple_w_gate": np.ascontiguousarray(inputs["ple_w_gate"], np.float32),
    }
    maps = []
    for c in range(8):
        b = c // 4
        t0 = (c % 4) * 1024
        first = (c % 4) == 0
        rows = np.arange(t0 - 128, t0 + 1024)
        if first:
            rows[:128] = np.arange(0, 128)
        m = dict(shared)
        m["xs"] = x[b]
        m["xo"] = np.ascontiguousarray(x[b][rows])
        m["pos_s"] = pos[b].reshape(1, S)
        m["pos_o"] = np.ascontiguousarray(pos[b][rows]).reshape(1, T0)
        m["p0"] = np.ascontiguousarray(p[0, b][rows])
        m["p1"] = np.ascontiguousarray(p[1, b][t0:t0 + 1024])
        m["qinfo"] = np.ascontiguousarray((rows // 64).astype(np.float32).reshape(NT, 128).T)
        m["hflag"] = np.full((128, 1), 0.0 if first else 1.0, np.float32)
        maps.append(m)
    return maps


def kernel(**inputs):
    if "nc" not in _CACHE:
        _CACHE["nc"] = build_program()
    nc = _CACHE["nc"]
    maps = make_in_maps(inputs)
    res = run_bass_kernel_spmd(nc, maps, core_ids=list(range(8)))
    out = np.empty((2, S, D), np.float32)
    for c in range(8):
        b = c // 4
        t0 = (c % 4) * 1024
        out[b, t0:t0 + 1024] = res.results[c]["y"]
    return out
```
